# Optimizing a Trainium2 kernel written in Bass

```python
import jax
import jax.numpy as jnp
from jax import lax
import numpy as np

D_MODEL = 1024
BATCH = 8
SEQ = 4096
DEPTH = 2

HEAD_DIM = 64
ROPE_THETA = 10000.0
LN_EPS = 1e-5
DEEPNORM_ALPHA = (2 * DEPTH) ** 0.25
DEEPNORM_BETA = (8 * DEPTH) ** -0.25
N_ADA = 6
MAX_POS_OFFSET = 4096

A_HEADS = 4
MOBA_BLOCK = 256
MOBA_TOPK = 3
MOBA_Q_CHUNK = 64

B_HEADS = 6
B_KV_GROUPS = 2
B_REP = B_HEADS // B_KV_GROUPS
CMP_BLOCK = 32
CMP_STRIDE = 16
CMP_HIDDEN = 128
SLC_BLOCK = 64
SLC_TOPK = 16
NSA_WINDOW = 512
NSA_Q_CHUNK = 64
N_NSA_BRANCHES = 3
FORCE_SCORE = 1e6

DILATED_PAIRS = ((128, 1), (512, 4), (2048, 16))
C_HEADS_PER_GROUP = 2
C_HEADS = C_HEADS_PER_GROUP * len(DILATED_PAIRS)
BAND_Q = 128

N_BRANCHES = 3

N_EXPERTS = 32
TOP_K = 4
D_EXPERT = D_MODEL
SWIGLU_ALPHA = 1.702
SWIGLU_LIMIT = 7.0
EXPERT_ROW_BLOCK = 256

IN_LAYOUT = (
    ("a_q", A_HEADS * HEAD_DIM), ("a_k", A_HEADS * HEAD_DIM), ("a_v", A_HEADS * HEAD_DIM),
    ("b_q", B_HEADS * HEAD_DIM),
    ("b_kc", B_KV_GROUPS * HEAD_DIM), ("b_vc", B_KV_GROUPS * HEAD_DIM),
    ("b_ks", B_KV_GROUPS * HEAD_DIM), ("b_vs", B_KV_GROUPS * HEAD_DIM),
    ("b_kw", B_KV_GROUPS * HEAD_DIM), ("b_vw", B_KV_GROUPS * HEAD_DIM),
    ("b_gate", B_HEADS * N_NSA_BRANCHES),
    ("c_q", C_HEADS * HEAD_DIM), ("c_k", C_HEADS * HEAD_DIM), ("c_v", C_HEADS * HEAD_DIM),
    ("merge_gate", N_BRANCHES * D_MODEL),
)
IN_WIDTH = sum(w for _, w in IN_LAYOUT)
V_COLUMNS = ("a_v", "b_vc", "b_vs", "b_vw", "c_v")

kernel_name = "hybrid_moba_nsa_dilated_moe_deepnorm"


def _layer_norm(x):
    xf = x.astype(jnp.float32)
    mu = xf.mean(-1, keepdims=True)
    var = jnp.mean(jnp.square(xf - mu), -1, keepdims=True)
    return ((xf - mu) * lax.rsqrt(var + LN_EPS)).astype(x.dtype)


def _affine_ln(x, g, b):
    return _layer_norm(x) * g + b


def _rope_tables(positions):
    inv = ROPE_THETA ** (-jnp.arange(0, HEAD_DIM, 2, dtype=jnp.float32) / HEAD_DIM)
    ang = positions.astype(jnp.float32)[..., None] * inv
    return jnp.cos(ang)[:, :, None, :], jnp.sin(ang)[:, :, None, :]


def _rope(x, cos, sin):
    x1, x2 = jnp.split(x, 2, axis=-1)
    cos = cos.astype(x.dtype)
    sin = sin.astype(x.dtype)
    return jnp.concatenate([x1 * cos - x2 * sin, x2 * cos + x1 * sin], axis=-1)


def _split_in(proj):
    offs = [int(o) for o in np.cumsum([w for _, w in IN_LAYOUT])[:-1]]
    parts = jnp.split(proj, offs, axis=-1)
    return {name: p for (name, _), p in zip(IN_LAYOUT, parts)}


def _gather_blocks(blocks, idx):
    return jax.vmap(jax.vmap(lambda a, i: a[i]))(blocks, idx)


def _banded_attention(q, k, v, window):
    b, s, g, r, dh = q.shape
    span = window - 1
    pad = ((0, 0), (span, 0), (0, 0), (0, 0))
    kp = jnp.pad(k, pad)
    vp = jnp.pad(v, pad)
    nq = s // BAND_Q
    qb = q.reshape(b, nq, BAND_Q, g, r, dh).transpose(1, 0, 3, 4, 2, 5)
    rel = jnp.arange(BAND_Q)[:, None] + span - jnp.arange(BAND_Q + span)[None, :]
    band = (rel >= 0) & (rel < window)
    scale = dh ** -0.5

    def block(args):
        q_blk, ci = args
        start = ci * BAND_Q
        kw = lax.dynamic_slice_in_dim(kp, start, BAND_Q + span, axis=1)
        vw = lax.dynamic_slice_in_dim(vp, start, BAND_Q + span, axis=1)
        key_pos = start - span + jnp.arange(BAND_Q + span)
        ok = band & (key_pos >= 0)[None, :]
        sc = jnp.einsum("bgrqd,bkgd->bgrqk", q_blk, kw).astype(jnp.float32) * scale
        sc = jnp.where(ok, sc, -jnp.inf)
        m = jnp.max(sc, -1, keepdims=True)
        e = jnp.exp(sc - m)
        den = e.sum(-1, keepdims=True)
        o = jnp.einsum("bgrqk,bkgd->bgrqd", (e / den).astype(v.dtype), vw)
        return o, (m + jnp.log(den))[..., 0]

    o, lse = lax.map(block, (qb, jnp.arange(nq)))
    o = o.transpose(1, 0, 4, 2, 3, 5).reshape(b, s, g, r, dh)
    lse = lse.transpose(1, 0, 4, 2, 3).reshape(b, s, g, r)
    return o, lse


def _moba_attention(q, k, v):
    b, s, h, dh = q.shape
    nb = -(-s // MOBA_BLOCK)
    pad = ((0, 0), (0, nb * MOBA_BLOCK - s), (0, 0), (0, 0))
    kb = jnp.pad(k, pad).reshape(b, nb, MOBA_BLOCK, h, dh).transpose(0, 3, 1, 2, 4)
    vb = jnp.pad(v, pad).reshape(b, nb, MOBA_BLOCK, h, dh).transpose(0, 3, 1, 2, 4)
    k_mean = kb.astype(jnp.float32).mean(axis=3)
    n_sel = min(MOBA_TOPK, nb - 1)
    n_chunks = s // MOBA_Q_CHUNK
    qc = q.reshape(b, n_chunks, MOBA_Q_CHUNK, h, dh).transpose(1, 0, 3, 2, 4)
    scale = dh ** -0.5
    blk_ids = jnp.arange(nb)
    key_off = jnp.arange(MOBA_BLOCK)

    def chunk(args):
        q_blk, ci = args
        t = ci * MOBA_Q_CHUNK + jnp.arange(MOBA_Q_CHUNK)
        own = (ci * MOBA_Q_CHUNK) // MOBA_BLOCK
        k_own = lax.dynamic_index_in_dim(kb, own, axis=2, keepdims=False)
        v_own = lax.dynamic_index_in_dim(vb, own, axis=2, keepdims=False)
        s_own = jnp.einsum("bhqd,bhld->bhql", q_blk, k_own).astype(jnp.float32) * scale
        s_own = jnp.where((own * MOBA_BLOCK + key_off)[None, :] <= t[:, None], s_own, -jnp.inf)
        if n_sel == 0:
            p = jax.nn.softmax(s_own, axis=-1).astype(v.dtype)
            return jnp.einsum("bhql,bhld->bhqd", p, v_own)
        gate = jnp.einsum("bhqd,bhnd->bhqn", q_blk.astype(jnp.float32), k_mean)
        gate = jnp.where(blk_ids < own, gate, -jnp.inf)
        _, sel = lax.top_k(gate, n_sel)
        valid = sel < own
        k_sel = _gather_blocks(kb, sel)
        v_sel = _gather_blocks(vb, sel)
        s_sel = jnp.einsum("bhqd,bhqnld->bhqnl", q_blk, k_sel).astype(jnp.float32) * scale
        s_sel = jnp.where(valid[..., None], s_sel, -jnp.inf)
        s_sel = s_sel.reshape(b, h, MOBA_Q_CHUNK, n_sel * MOBA_BLOCK)
        p = jax.nn.softmax(jnp.concatenate([s_sel, s_own], -1), axis=-1).astype(v.dtype)
        p_sel = p[..., : n_sel * MOBA_BLOCK].reshape(b, h, MOBA_Q_CHUNK, n_sel, MOBA_BLOCK)
        p_own = p[..., n_sel * MOBA_BLOCK:]
        return (jnp.einsum("bhqnl,bhqnld->bhqd", p_sel, v_sel)
                + jnp.einsum("bhql,bhld->bhqd", p_own, v_own))

    out = lax.map(chunk, (qc, jnp.arange(n_chunks)))
    return out.transpose(1, 0, 3, 2, 4).reshape(b, s, h, dh)


def _nsa_compress(x, w1, w2, pe):
    b, s, g, dh = x.shape
    nc = (s - CMP_BLOCK) // CMP_STRIDE + 1
    idx = jnp.arange(nc)[:, None] * CMP_STRIDE + jnp.arange(CMP_BLOCK)[None, :]
    blocks = x[:, idx] + pe[None, None, :, None, :]
    flat = blocks.transpose(0, 1, 3, 2, 4).reshape(b, nc, g, CMP_BLOCK * dh)
    return jax.nn.silu(flat @ w1) @ w2


def _nsa_attention(q, k_cmp, v_cmp, k_slc, v_slc, k_win, v_win, gate_logits):
    b, s, g, r, dh = q.shape
    nc = k_cmp.shape[1]
    ns = s // SLC_BLOCK
    n_sel = min(SLC_TOPK, ns)
    scale = dh ** -0.5
    cmp_start = jnp.arange(nc) * CMP_STRIDE
    cmp_end = cmp_start + CMP_BLOCK - 1
    slc_start = jnp.arange(ns) * SLC_BLOCK
    overlap = ((cmp_start[:, None] < slc_start[None, :] + SLC_BLOCK)
               & (cmp_start[:, None] + CMP_BLOCK > slc_start[None, :])).astype(jnp.float32)
    ksb = k_slc.reshape(b, ns, SLC_BLOCK, g, dh).transpose(0, 3, 1, 2, 4)
    vsb = v_slc.reshape(b, ns, SLC_BLOCK, g, dh).transpose(0, 3, 1, 2, 4)
    nq = s // NSA_Q_CHUNK
    qc = q.reshape(b, nq, NSA_Q_CHUNK, g, r, dh).transpose(1, 0, 3, 4, 2, 5)
    blk_ids = jnp.arange(ns)
    key_off = jnp.arange(SLC_BLOCK)

    def chunk(args):
        q_blk, ci = args
        t = ci * NSA_Q_CHUNK + jnp.arange(NSA_Q_CHUNK)
        sc = jnp.einsum("bgrqd,bngd->bgrqn", q_blk, k_cmp).astype(jnp.float32) * scale
        vis = cmp_end[None, :] <= t[:, None]
        sc = jnp.where(vis, sc, -jnp.inf)
        m = jnp.max(sc, -1, keepdims=True)
        m = jnp.where(jnp.isfinite(m), m, 0.0)
        e = jnp.where(vis, jnp.exp(sc - m), 0.0)
        p_cmp = e / jnp.maximum(e.sum(-1, keepdims=True), 1e-30)
        o_cmp = jnp.einsum("bgrqn,bngd->bgrqd", p_cmp.astype(v_cmp.dtype), v_cmp)
        imp = jnp.einsum("bgrqn,nm->bgqm", p_cmp, overlap)
        q_blk_id = (t // SLC_BLOCK)[:, None]
        forced = (blk_ids[None, :] == 0) | (blk_ids[None, :] == q_blk_id) | (blk_ids[None, :] == q_blk_id - 1)
        imp = jnp.where(forced, FORCE_SCORE, imp)
        imp = jnp.where(blk_ids[None, :] <= q_blk_id, imp, -jnp.inf)
        _, sel = lax.top_k(imp, n_sel)
        k_sel = _gather_blocks(ksb, sel)
        v_sel = _gather_blocks(vsb, sel)
        pos = sel[..., None] * SLC_BLOCK + key_off
        ok = pos <= t[None, None, :, None, None]
        ss = jnp.einsum("bgrqd,bgqnld->bgrqnl", q_blk, k_sel).astype(jnp.float32) * scale
        ss = jnp.where(ok[:, :, None], ss, -jnp.inf).reshape(b, g, r, NSA_Q_CHUNK, n_sel * SLC_BLOCK)
        p = jax.nn.softmax(ss, axis=-1).reshape(b, g, r, NSA_Q_CHUNK, n_sel, SLC_BLOCK)
        o_slc = jnp.einsum("bgrqnl,bgqnld->bgrqd", p.astype(v_slc.dtype), v_sel)
        return o_cmp, o_slc

    o_cmp, o_slc = lax.map(chunk, (qc, jnp.arange(nq)))
    o_cmp = o_cmp.transpose(1, 0, 4, 2, 3, 5).reshape(b, s, g, r, dh)
    o_slc = o_slc.transpose(1, 0, 4, 2, 3, 5).reshape(b, s, g, r, dh)
    o_win, _ = _banded_attention(q, k_win, v_win, NSA_WINDOW)
    gw = jax.nn.sigmoid(gate_logits)
    return gw[..., 0:1] * o_cmp + gw[..., 1:2] * o_slc + gw[..., 2:3] * o_win


def _dilated_attention(q, k, v):
    b, s, _, dh = q.shape
    outs, lses = [], []
    for gi, (window, dil) in enumerate(DILATED_PAIRS):
        hs = slice(gi * C_HEADS_PER_GROUP, (gi + 1) * C_HEADS_PER_GROUP)
        n_res = -(-s // dil)
        n_res = -(-n_res // BAND_Q) * BAND_Q
        pad = ((0, 0), (0, n_res * dil - s), (0, 0), (0, 0))
        qr = jnp.pad(q[:, :, hs], pad).reshape(b, n_res, dil * C_HEADS_PER_GROUP, dh)
        kr = jnp.pad(k[:, :, hs], pad).reshape(b, n_res, dil * C_HEADS_PER_GROUP, dh)
        vr = jnp.pad(v[:, :, hs], pad).reshape(b, n_res, dil * C_HEADS_PER_GROUP, dh)
        o, lse = _banded_attention(qr[:, :, :, None], kr, vr, window // dil + 1)
        outs.append(o[:, :, :, 0].reshape(b, n_res * dil, C_HEADS_PER_GROUP, dh)[:, :s])
        lses.append(lse[..., 0].reshape(b, n_res * dil, C_HEADS_PER_GROUP)[:, :s])
    wts = jax.nn.softmax(jnp.stack(lses, 0), axis=0)
    return jnp.sum(wts[..., None].astype(q.dtype) * jnp.stack(outs, 0), axis=0)


def _clamped_swiglu(gu):
    x_glu = jnp.minimum(gu[..., ::2], SWIGLU_LIMIT)
    x_lin = jnp.clip(gu[..., 1::2], -SWIGLU_LIMIT, SWIGLU_LIMIT)
    return x_glu * jax.nn.sigmoid(SWIGLU_ALPHA * x_glu) * (x_lin + 1.0)


def _moe_ffn(h, router_w, router_b, w_gu, b_gu, w_dn, b_dn):
    n, d = h.shape
    logits = (h @ router_w + router_b).astype(jnp.float32)
    top_val, top_idx = lax.top_k(logits, TOP_K)
    gate = jax.nn.softmax(top_val, axis=-1)
    nk = n * TOP_K
    flat_e = top_idx.reshape(nk)
    flat_tok = jnp.repeat(jnp.arange(n, dtype=jnp.int32), TOP_K)
    flat_gate = gate.reshape(nk)
    order = jnp.argsort(flat_e, stable=True)
    e_sorted = flat_e[order]
    counts = jnp.bincount(flat_e, length=N_EXPERTS)
    padded = -(-counts // EXPERT_ROW_BLOCK) * EXPERT_ROW_BLOCK
    start = jnp.cumsum(counts) - counts
    pend = jnp.cumsum(padded)
    pstart = pend - padded
    dest = pstart[e_sorted] + jnp.arange(nk) - start[e_sorted]
    n_blocks = -(-(nk + N_EXPERTS * (EXPERT_ROW_BLOCK - 1)) // EXPERT_ROW_BLOCK)
    rows = n_blocks * EXPERT_ROW_BLOCK
    row_tok = jnp.zeros((rows,), jnp.int32).at[dest].set(flat_tok[order])
    row_gate = jnp.zeros((rows,), jnp.float32).at[dest].set(flat_gate[order])
    block_exp = jnp.minimum(
        jnp.searchsorted(pend, jnp.arange(n_blocks) * EXPERT_ROW_BLOCK, side="right"), N_EXPERTS - 1)

    def block(args):
        tok, g, e = args
        xb = h[tok]
        y = _clamped_swiglu(xb @ w_gu[e] + b_gu[e]) @ w_dn[e] + b_dn[e]
        return y * g[:, None].astype(y.dtype)

    y_rows = lax.map(block, (row_tok.reshape(n_blocks, EXPERT_ROW_BLOCK),
                             row_gate.reshape(n_blocks, EXPERT_ROW_BLOCK), block_exp))
    return jax.ops.segment_sum(y_rows.reshape(rows, d), row_tok, num_segments=n)


def _mixer(h, cos, sin, w_in, cmp_w1_k, cmp_w2_k, cmp_pe_k, cmp_w1_v, cmp_w2_v, cmp_pe_v,
           w_branch_a, w_branch_b, w_branch_c, w_out):
    b, s, _ = h.shape
    p = _split_in(h @ w_in)

    def heads(t, nh):
        return t.reshape(b, s, nh, HEAD_DIM)

    o_a = _moba_attention(_rope(heads(p["a_q"], A_HEADS), cos, sin),
                          _rope(heads(p["a_k"], A_HEADS), cos, sin),
                          heads(p["a_v"], A_HEADS))
    q_b = _rope(heads(p["b_q"], B_HEADS), cos, sin).reshape(b, s, B_KV_GROUPS, B_REP, HEAD_DIM)
    k_c = _nsa_compress(_rope(heads(p["b_kc"], B_KV_GROUPS), cos, sin), cmp_w1_k, cmp_w2_k, cmp_pe_k)
    v_c = _nsa_compress(heads(p["b_vc"], B_KV_GROUPS), cmp_w1_v, cmp_w2_v, cmp_pe_v)
    o_b = _nsa_attention(q_b, k_c, v_c,
                         _rope(heads(p["b_ks"], B_KV_GROUPS), cos, sin), heads(p["b_vs"], B_KV_GROUPS),
                         _rope(heads(p["b_kw"], B_KV_GROUPS), cos, sin), heads(p["b_vw"], B_KV_GROUPS),
                         p["b_gate"].reshape(b, s, B_KV_GROUPS, B_REP, N_NSA_BRANCHES))
    o_c = _dilated_attention(_rope(heads(p["c_q"], C_HEADS), cos, sin),
                             _rope(heads(p["c_k"], C_HEADS), cos, sin),
                             heads(p["c_v"], C_HEADS))
    g_a, g_b, g_c = jnp.split(jax.nn.sigmoid(p["merge_gate"]), N_BRANCHES, axis=-1)
    merged = (g_a * (o_a.reshape(b, s, -1) @ w_branch_a)
              + g_b * (o_b.reshape(b, s, -1) @ w_branch_b)
              + g_c * (o_c.reshape(b, s, -1) @ w_branch_c))
    return merged @ w_out


def setup_inputs(seed: int = 0) -> dict:
    key = jax.random.key(seed)
    ks = jax.random.split(key, 26)
    f32 = jnp.float32

    def nrm(k, shape, std):
        return jax.random.normal(k, shape, f32) * std

    col_scale = jnp.asarray(np.concatenate(
        [np.full((w,), DEEPNORM_BETA if name in V_COLUMNS else 1.0, np.float32) for name, w in IN_LAYOUT]))
    cmp_in = CMP_BLOCK * HEAD_DIM
    wa = A_HEADS * HEAD_DIM
    wb = B_HEADS * HEAD_DIM
    wc = C_HEADS_PER_GROUP * HEAD_DIM
    positions = (jax.random.randint(ks[2], (BATCH, 1), 0, MAX_POS_OFFSET)
                 + jnp.arange(SEQ)[None, :]).astype(jnp.int32)
    return {
        "x": nrm(ks[0], (BATCH, SEQ, D_MODEL), 1.0),
        "c": nrm(ks[1], (BATCH, D_MODEL), 1.0),
        "positions": positions,
        "w_ada": nrm(ks[3], (DEPTH, D_MODEL, N_ADA * D_MODEL), 0.5 * D_MODEL ** -0.5),
        "b_ada": nrm(ks[4], (DEPTH, N_ADA * D_MODEL), 0.01),
        "w_in": nrm(ks[5], (DEPTH, D_MODEL, IN_WIDTH), D_MODEL ** -0.5) * col_scale,
        "cmp_w1_k": nrm(ks[6], (DEPTH, cmp_in, CMP_HIDDEN), cmp_in ** -0.5),
        "cmp_w2_k": nrm(ks[7], (DEPTH, CMP_HIDDEN, HEAD_DIM), CMP_HIDDEN ** -0.5),
        "cmp_pe_k": nrm(ks[8], (DEPTH, CMP_BLOCK, HEAD_DIM), 0.1),
        "cmp_w1_v": nrm(ks[9], (DEPTH, cmp_in, CMP_HIDDEN), cmp_in ** -0.5),
        "cmp_w2_v": nrm(ks[10], (DEPTH, CMP_HIDDEN, HEAD_DIM), CMP_HIDDEN ** -0.5),
        "cmp_pe_v": nrm(ks[11], (DEPTH, CMP_BLOCK, HEAD_DIM), 0.1),
        "w_branch_a": nrm(ks[12], (DEPTH, wa, D_MODEL), wa ** -0.5),
        "w_branch_b": nrm(ks[13], (DEPTH, wb, D_MODEL), wb ** -0.5),
        "w_branch_c": nrm(ks[14], (DEPTH, wc, D_MODEL), wc ** -0.5),
        "w_out": nrm(ks[15], (DEPTH, D_MODEL, D_MODEL), DEEPNORM_BETA * D_MODEL ** -0.5),
        "ln1_g": 1.0 + nrm(ks[16], (DEPTH, D_MODEL), 0.02),
        "ln1_b": nrm(ks[17], (DEPTH, D_MODEL), 0.02),
        "router_w": nrm(ks[18], (DEPTH, D_MODEL, N_EXPERTS), D_MODEL ** -0.5),
        "router_b": nrm(ks[19], (DEPTH, N_EXPERTS), 0.01),
        "w_gate_up": nrm(ks[20], (DEPTH, N_EXPERTS, D_MODEL, 2 * D_EXPERT), D_MODEL ** -0.5),
        "b_gate_up": nrm(ks[21], (DEPTH, N_EXPERTS, 2 * D_EXPERT), 0.01),
        "w_down": nrm(ks[22], (DEPTH, N_EXPERTS, D_EXPERT, D_MODEL), DEEPNORM_BETA * D_EXPERT ** -0.5),
        "b_down": nrm(ks[23], (DEPTH, N_EXPERTS, D_MODEL), 0.01),
        "ln2_g": 1.0 + nrm(ks[24], (DEPTH, D_MODEL), 0.02),
        "ln2_b": nrm(ks[25], (DEPTH, D_MODEL), 0.02),
    }


def reference(x, c, positions, w_ada, b_ada, w_in, cmp_w1_k, cmp_w2_k, cmp_pe_k,
              cmp_w1_v, cmp_w2_v, cmp_pe_v, w_branch_a, w_branch_b, w_branch_c, w_out,
              ln1_g, ln1_b, router_w, router_b, w_gate_up, b_gate_up, w_down, b_down,
              ln2_g, ln2_b):
    b, s, d = x.shape
    cos, sin = _rope_tables(positions)
    cond = jax.nn.silu(c)
    for l in range(DEPTH):
        mod = (cond @ w_ada[l] + b_ada[l])[:, None, :]
        sh1, sc1, g1, sh2, sc2, g2 = jnp.split(mod, N_ADA, axis=-1)
        h = _layer_norm(x) * (1.0 + sc1) + sh1
        y = _mixer(h, cos, sin, w_in[l], cmp_w1_k[l], cmp_w2_k[l], cmp_pe_k[l],
                   cmp_w1_v[l], cmp_w2_v[l], cmp_pe_v[l],
                   w_branch_a[l], w_branch_b[l], w_branch_c[l], w_out[l])
        x = _affine_ln(DEEPNORM_ALPHA * x + g1 * y, ln1_g[l], ln1_b[l])
        h = _layer_norm(x) * (1.0 + sc2) + sh2
        y = _moe_ffn(h.reshape(b * s, d), router_w[l], router_b[l], w_gate_up[l],
                     b_gate_up[l], w_down[l], b_down[l]).reshape(b, s, d)
        x = _affine_ln(DEEPNORM_ALPHA * x + g2 * y, ln2_g[l], ln2_b[l])
    return x
```

```python
import numpy as np
import ml_dtypes
from contextlib import ExitStack
import concourse.bass as bass
import concourse.mybir as mybir
from concourse.bass_utils import run_bass_kernel_spmd

F32 = mybir.dt.float32
BF16 = mybir.dt.bfloat16
I32 = mybir.dt.int32
AF = mybir.ActivationFunctionType
ALU = mybir.AluOpType
AX = mybir.AxisListType

D = 1024
S = 4096
NT = S // 128
DEPTH = 2
NCORES = 8
LN_EPS = 1e-5
ALPHA = (2 * DEPTH) ** 0.25
NEG = -30000.0
BIG = 1.0e30
SCALE = 64 ** -0.5
NEXP = 32
RB = 512
NBLK = 64
C_AQ, C_AK, C_AV, C_BQ, C_BKC, C_BVC, C_BKS, C_BVS, C_BKW, C_BVW, C_BG, C_CQ, C_CK, C_CV, C_MG = (
    0, 256, 512, 768, 1152, 1280, 1408, 1536, 1664, 1792, 1920, 1938, 2322, 2706, 3090)
IN_W = 6162
FB_AQ, FB_AK, FB_BQ, FB_KC, FB_KS, FB_KW, FB_CQ, FB_CK, FB_VC = 0, 2, 4, 7, 8, 9, 10, 13, 16
NFB = 17


class Res:
    __slots__ = ("lw", "rd")

    def __init__(self):
        self.lw = None
        self.rd = {}


class Sched:
    def __init__(self, nc, es):
        self.nc = nc
        self.eng = {"pe": nc.tensor, "act": nc.scalar, "dve": nc.vector, "pool": nc.gpsimd, "sp": nc.sync}
        self.sem = {k: es.enter_context(nc.semaphore("prog_" + k)) for k in self.eng}
        self.cnt = {k: 0 for k in self.eng}
        self.seen = {k: {} for k in self.eng}
        self.own = {self.sem[k]: k for k in self.eng}
        self.dq = {}
        for q, n in (("sp", 20), ("pool", 12), ("act", 4)):
            self.dq[q] = {"sems": [es.enter_context(nc.semaphore(f"dma_{q}_{i}")) for i in range(n)],
                          "val": [0] * n, "i": 0}
        self.n_ins = 0

    def _wait(self, e, tok, raw):
        sem, val = tok
        if self.seen[e].get(sem, 0) >= val:
            return
        o = self.own.get(sem)
        if o == e:
            if e == "pe" or not raw or self.cnt[e] - val >= 2:
                return
        self.eng[e].wait_ge(sem, val)
        self.seen[e][sem] = val
        self.n_ins += 1

    def _deps(self, e, R, W):
        for r in R:
            if r.lw is not None:
                self._wait(e, r.lw, True)
        for w in W:
            if w.lw is not None:
                self._wait(e, w.lw, False)
            for sem, val in w.rd.items():
                self._wait(e, (sem, val), False)

    def _commit(self, tok, R, W):
        sem, val = tok
        for r in R:
            if r.rd.get(sem, 0) < val:
                r.rd[sem] = val
        for w in W:
            w.lw = tok
            w.rd = {}

    def op(self, e, R, W, name, *a, **k):
        self._deps(e, R, W)
        ins = getattr(self.eng[e], name)(*a, **k)
        self.cnt[e] += 1
        ins.then_inc(self.sem[e], 1)
        self._commit((self.sem[e], self.cnt[e]), R, W)
        self.n_ins += 1
        return ins

    def dma(self, q, R, W, out, in_, **k):
        d = self.dq[q]
        i = d["i"] % len(d["sems"])
        d["i"] += 1
        sem = d["sems"][i]
        if d["val"][i] > 0:
            self._wait(q, (sem, d["val"][i]), False)
        self._deps(q, R, W)
        ins = self.eng[q].dma_start(out=out, in_=in_, **k)
        ins.then_inc(sem, 16)
        d["val"][i] += 16
        self._commit((sem, d["val"][i]), R, W)
        self.n_ins += 1

    def idma(self, R, W, **k):
        q = "pool"
        d = self.dq[q]
        i = d["i"] % len(d["sems"])
        d["i"] += 1
        sem = d["sems"][i]
        if d["val"][i] > 0:
            self._wait(q, (sem, d["val"][i]), False)
        self._deps(q, R, W)
        ins = self.eng[q].indirect_dma_start(**k)
        ins.then_inc(sem, 16)
        d["val"][i] += 16
        self._commit((sem, d["val"][i]), R, W)
        self.n_ins += 1

    def barrier(self):
        toks = [(self.sem[f], self.cnt[f]) for f in self.eng if self.cnt[f] > 0]
        for q in self.dq.values():
            for s_, v in zip(q["sems"], q["val"]):
                if v > 0:
                    toks.append((s_, v))
        for e in self.eng:
            for (sem, val) in toks:
                if self.own.get(sem) == e:
                    continue
                if self.seen[e].get(sem, 0) >= val:
                    continue
                self.eng[e].wait_ge(sem, val)
                self.seen[e][sem] = val
                self.n_ins += 1


def round_robin(gens, width):
    it = iter(gens)
    active = []
    more = True
    while True:
        while more and len(active) < width:
            try:
                active.append(next(it))
            except StopIteration:
                more = False
        if not active:
            break
        for g in list(active):
            try:
                next(g)
            except StopIteration:
                active.remove(g)


def bf16_np(a):
    return np.asarray(a, np.float32).astype(ml_dtypes.bfloat16)


def make_consts():
    c = {}
    c["identb"] = bf16_np(np.eye(128))
    c["identf"] = np.eye(128, dtype=np.float32)
    k = np.arange(128)[:, None]
    q = np.arange(128)[None, :]
    masks = np.zeros((128, 3, 128), np.float32)
    masks[:, 0] = np.where(k <= q, 0.0, NEG)
    masks[:, 1] = np.where(k > q, 0.0, NEG)
    masks[:, 2] = np.where(k >= q, 0.0, NEG)
    c["masks"] = bf16_np(masks)
    e16 = np.zeros((16, 16, 128), np.float32)
    for n in range(16):
        e16[n, n, :] = 1.0
    c["esel16"] = bf16_np(e16)
    e64 = np.zeros((64, 32, 128), np.float32)
    for kt in range(32):
        e64[2 * kt, kt, :64] = 1.0
        e64[2 * kt + 1, kt, 64:] = 1.0
    c["esel64"] = bf16_np(e64)
    inv = (10000.0 ** (-np.arange(0, 64, 2, dtype=np.float32) / 64)).astype(np.float32)
    rp = np.zeros((128, 2), np.float32)
    for f in range(128):
        rp[f, 0] = inv[f % 32]
        rp[f, 1] = -1.0 if (f % 64) < 32 else 1.0
    c["ropec"] = rp
    n = (np.arange(2)[None, :, None] * 128 + np.arange(128)[:, None, None])
    t = np.arange(S)[None, None, :]
    c["cmpmask"] = bf16_np(np.where(16 * n + 31 <= t, 0.0, NEG))
    ov = np.zeros((128, 2, 64), np.float32)
    for nt in range(2):
        for kk in range(128):
            nn = nt * 128 + kk
            if nn >= 255:
                continue
            for m in range(64):
                if 16 * nn < 64 * m + 64 and 16 * nn + 32 > 64 * m:
                    ov[kk, nt, m] = 1.0
    c["overlap"] = bf16_np(ov)
    add = np.zeros((128, 32, 64), np.float32)
    mn = np.zeros((128, 32, 64), np.float32)
    for qt in range(32):
        for p in range(128):
            qb = 2 * qt + (1 if p >= 64 else 0)
            for m in range(64):
                forced = (m == 0) or (m == qb) or (m == qb - 1)
                add[p, qt, m] = 1.0e6 if forced else 0.0
                mn[p, qt, m] = BIG if m <= qb else -BIG
    c["nsa_add"] = add
    c["nsa_min"] = mn
    c["tri"] = bf16_np((np.arange(128)[:, None] < np.arange(128)[None, :]).astype(np.float32))
    c["pidx"] = np.arange(128, dtype=np.float32).reshape(128, 1)
    return c


WNAMES = ["w_ada", "b_ada", "w_in", "cmp_w1_k", "cmp_w2_k", "cmp_peT_k", "cmp_w1_v", "cmp_w2_v", "cmp_peT_v",
          "w_branch_a", "w_branch_b", "w_branch_c", "w_out", "ln1_g", "ln1_b", "router_w", "router_b",
          "w_gu_l", "b_gu_l", "w_dn_l", "b_down", "ln2_g", "ln2_b"]
WSHAPES = {
    "w_ada": [DEPTH, D, 6 * D], "b_ada": [DEPTH, 6 * D], "w_in": [DEPTH, D, IN_W],
    "cmp_w1_k": [DEPTH, 2048, 128], "cmp_w2_k": [DEPTH, 128, 64], "cmp_peT_k": [DEPTH, 64, 32],
    "cmp_w1_v": [DEPTH, 2048, 128], "cmp_w2_v": [DEPTH, 128, 64], "cmp_peT_v": [DEPTH, 64, 32],
    "w_branch_a": [DEPTH, 256, D], "w_branch_b": [DEPTH, 384, D], "w_branch_c": [DEPTH, 128, D],
    "w_out": [DEPTH, D, D], "ln1_g": [DEPTH, D], "ln1_b": [DEPTH, D], "router_w": [DEPTH, D, NEXP],
    "router_b": [DEPTH, NEXP], "w_gu_l": [DEPTH, NEXP * 128, 16 * D], "b_gu_l": [DEPTH, NEXP * 128, 16],
    "w_dn_l": [DEPTH, NEXP * 128, 8 * D], "b_down": [DEPTH, NEXP, D], "ln2_g": [DEPTH, D], "ln2_b": [DEPTH, D],
}
CONST_DT = {"identb": BF16, "identf": F32, "masks": BF16, "esel16": BF16, "esel64": BF16, "ropec": F32,
            "cmpmask": BF16, "overlap": BF16, "nsa_add": F32, "nsa_min": F32, "tri": BF16, "pidx": F32}


class Prog:
    def __init__(self, consts, debug=False, stop_after=None, nlayers=DEPTH):
        self.debug = debug
        self.stop_after = stop_after
        self.nlayers = nlayers
        nc = self.nc = bass.Bass("TRN2", target_bir_lowering=False)
        self.es = ExitStack()
        self.I = {}
        self.I["x"] = nc.dram_tensor("x", [S, D], F32, kind="ExternalInput").ap()
        self.I["cT"] = nc.dram_tensor("cT", [128, 8], F32, kind="ExternalInput").ap()
        self.I["pos"] = nc.dram_tensor("pos", [1, S], I32, kind="ExternalInput").ap()
        for n in WNAMES:
            self.I[n] = nc.dram_tensor(n, WSHAPES[n], F32, kind="ExternalInput").ap()
        self.C = {}
        for n, a in consts.items():
            self.C[n] = nc.dram_tensor("k_" + n, list(a.shape), CONST_DT[n], kind="ExternalInput").ap()
        self.out = nc.dram_tensor("out", [S, D], F32, kind="ExternalOutput").ap()
        sk = "ExternalOutput" if debug else "Internal"
        self.scr = {}

        def scr(name, shape, dt):
            self.scr[name] = nc.dram_tensor("s_" + name, shape, dt, kind=sk).ap()
        scr("rope", [2, 128, S], F32)
        scr("mod", [DEPTH, 6 * D], F32)
        scr("ft", [NFB, 128, S], BF16)
        scr("tv", [S, 896], BF16)
        scr("mg", [S, 3072], BF16)
        scr("o", [S, 768], BF16)
        scr("oc", [3, S, 130], F32)
        scr("xs", [2, S, D], F32)
        scr("h2t", [128, 8, S], BF16)
        scr("h2", [S, D], BF16)
        scr("xs2", [NBLK * RB, D], BF16)
        scr("ys2", [NBLK * RB, D], F32)
        if debug:
            scr("dbg1", [128, 256], BF16)
            scr("dbg2", [128, 2 * 2 * 129], BF16)

    def sb(self, es, name, shape, dt):
        self._uid = getattr(self, "_uid", 0) + 1
        return es.enter_context(self.nc.sbuf_tensor(f"{name}_{self._uid}", shape, dt))

    def build(self):
        nc = self.nc
        with self.es as es:
            self.S_ = S_ = Sched(nc, es)
            self.ps = [es.enter_context(nc.psum_tensor(f"ps{i}", [128, 512], F32)) for i in range(8)]
            self.psr = [Res() for _ in range(8)]
            self.identb = self.sb(es, "identb", [128, 128], BF16)
            self.identf = self.sb(es, "identf", [128, 128], F32)
            self.masks = self.sb(es, "masks", [128, 3, 128], BF16)
            self.Gall = self.sb(es, "Gall", [128, NT, NEXP], F32)
            self.BGs = self.sb(es, "BGs", [128, NT, 18], F32)
            self.rc = Res()
            self.epsc = self.sb(es, "epsc", [128, 1], F32)
            self.zeros = self.sb(es, "zeros", [128, 512], BF16)
            S_.op("dve", [], [self.rc], "memset", self.zeros[:], 0.0)
            S_.op("dve", [], [self.rc], "memset", self.epsc[:], LN_EPS)
            for n_, t_ in (("identb", self.identb), ("identf", self.identf), ("masks", self.masks)):
                S_.dma("sp", [], [self.rc], t_[:], self.C[n_])
            self.rG = Res()
            self.rBG = Res()
            self.Sall = self.sb(es, "Sall", [128, NT, NEXP], F32)
            self.Di = self.sb(es, "Di", [128, NT, 4], I32)
            self.g4 = self.sb(es, "g4", [128, NT, 4], F32)
            self.Iw = self.sb(es, "Iw", [128, NBLK], I32)
            self.rS, self.rDi, self.rg4, self.rIw = Res(), Res(), Res(), Res()
            self.phase_rope()
            S_.barrier()
            xin = self.I["x"]
            for l in range(self.nlayers):
                self.l = l
                for pn in ("phase_ada", "phase_ln1", "phase_proj", "phase_moba", "phase_nsa",
                           "phase_dil", "phase_merge", "phase_route", "phase_ffn", "phase_ln2"):
                    ph = getattr(self, pn)
                    if pn == "phase_ln1":
                        ph(xin)
                    elif pn == "phase_merge":
                        ph(xin, self.scr["xs"][0])
                    elif pn == "phase_ln2":
                        dst = self.out if l == self.nlayers - 1 else self.scr["xs"][1]
                        ph(self.scr["xs"][0], dst)
                    else:
                        ph()
                    S_.barrier()
                    if self.stop_after == (l, pn):
                        return nc
                xin = self.scr["xs"][1]
            S_.barrier()
        return nc

    def phase_rope(self):
        import math
        S_ = self.S_
        with ExitStack() as es:
            posb = self.sb(es, "posb", [128, S], I32)
            ang = self.sb(es, "ang", [128, S], F32)
            arg = self.sb(es, "arg", [128, S], F32)
            res_ = self.sb(es, "rres", [128, S], F32)
            rpc = self.sb(es, "rpc", [128, 2], F32)
            npi = self.sb(es, "npi", [128, 1], F32)
            r1, r2, r3, r4, r5 = Res(), Res(), Res(), Res(), Res()
            S_.dma("sp", [], [r1], posb[:], self.I["pos"].partition_broadcast(128))
            S_.dma("sp", [], [r5], rpc[:], self.C["ropec"])
            S_.op("dve", [], [r5], "memset", npi[:], -math.pi)
            S_.op("dve", [r1], [r2], "tensor_copy", out=ang[:], in_=posb[:])
            S_.op("dve", [r2, r5], [r2], "tensor_scalar", out=ang[:], in0=ang[:], scalar1=rpc[:, 0:1], scalar2=None,
                  op0=ALU.mult)
            ki = posb
            for i, off in enumerate((0.5 * math.pi, 0.0)):
                S_.op("dve", [r2], [r3], "tensor_scalar", out=arg[:], in0=ang[:], scalar1=off, scalar2=None,
                      op0=ALU.add)
                S_.op("dve", [r3], [r4], "tensor_scalar", out=res_[:], in0=arg[:], scalar1=1.0 / (2 * math.pi),
                      scalar2=None, op0=ALU.mult)
                S_.op("dve", [r4], [r1], "tensor_copy", out=ki[:], in_=res_[:])
                S_.op("dve", [r1], [r4], "tensor_copy", out=res_[:], in_=ki[:])
                S_.op("dve", [r4, r3], [r3], "scalar_tensor_tensor", out=arg[:], in0=res_[:], scalar=-2 * math.pi,
                      in1=arg[:], op0=ALU.mult, op1=ALU.add)
                S_.op("dve", [r3], [r4], "tensor_scalar", out=res_[:], in0=arg[:], scalar1=math.pi,
                      scalar2=-2 * math.pi, op0=ALU.is_gt, op1=ALU.mult)
                S_.op("dve", [r3, r4], [r3], "tensor_tensor", out=arg[:], in0=arg[:], in1=res_[:], op=ALU.add)
                S_.op("dve", [r3], [r3], "tensor_scalar", out=arg[:], in0=arg[:], scalar1=-math.pi, scalar2=math.pi,
                      op0=ALU.max, op1=ALU.min)
                S_.op("act", [r3], [r4], "activation", out=res_[:], in_=arg[:], func=AF.Sin)
                if i == 1:
                    S_.op("dve", [r4, r5], [r4], "tensor_scalar", out=res_[:], in0=res_[:], scalar1=rpc[:, 1:2],
                          scalar2=None, op0=ALU.mult)
                S_.dma("pool", [r4], [], self.scr["rope"][i], res_[:])
            S_.barrier()

    def phase_ada(self):
        S_ = self.S_
        l = self.l
        with ExitStack() as es:
            cs = self.sb(es, "cs", [128, 8], F32)
            brow = self.sb(es, "brow", [1, 6 * D], F32)
            mrow = self.sb(es, "mrow", [1, 6 * D], F32)
            wa = [self.sb(es, f"wa{i}", [128, 8, 512], F32) for i in range(2)]
            rcs, rb, rm = Res(), Res(), Res()
            rwa = [Res(), Res()]
            S_.dma("sp", [], [rcs], cs[:], self.I["cT"])
            S_.dma("sp", [], [rb], brow[:], self.I["b_ada"][l:l + 1, :])
            S_.op("act", [rcs], [rcs], "activation", out=cs[:], in_=cs[:], func=AF.Silu)
            for nb in range(12):
                w = wa[nb % 2]
                S_.dma("sp", [], [rwa[nb % 2]], w[:],
                       self.I["w_ada"][l][:, nb * 512:(nb + 1) * 512].rearrange("(c p) n -> p c n", p=128))
                pr = self.psr[nb % 2]
                for c in range(8):
                    S_.op("pe", [rcs, rwa[nb % 2]], [pr], "matmul", self.ps[nb % 2][0:1, :], lhsT=cs[:, c:c + 1],
                          rhs=w[:, c, :], start=(c == 0), stop=(c == 7))
                S_.op("dve", [pr, rb], [rm], "tensor_tensor", out=mrow[:, nb * 512:(nb + 1) * 512],
                      in0=self.ps[nb % 2][0:1, :], in1=brow[:, nb * 512:(nb + 1) * 512], op=ALU.add)
            for seg in (1, 4):
                S_.op("dve", [rm], [rm], "tensor_scalar", out=mrow[:, seg * D:(seg + 1) * D],
                      in0=mrow[:, seg * D:(seg + 1) * D], scalar1=1.0, scalar2=None, op0=ALU.add)
            S_.dma("pool", [rm], [], self.scr["mod"][l:l + 1, :], mrow[:])

    def bload(self, es, name, src_row, r):
        t = self.sb(es, name, [128, D], F32)
        self.S_.dma("sp", [], [r], t[:], src_row.partition_broadcast(128))
        return t

    def ln_tile(self, xt, rx, tmp, rtmp, stat, rstat, split=False):
        S_ = self.S_
        st6 = stat[:, 4:16].rearrange("p (a b) -> p a b", a=2)
        for a in range(2):
            S_.op("dve", [rx], [rstat], "bn_stats", out=st6[:, a, :], in_=xt[:, a * 512:(a + 1) * 512])
        S_.op("dve", [rstat], [rstat], "bn_aggr", out=stat[:, 2:4], in_=st6)
        S_.op("act", [rstat, self.rc], [rstat], "activation", out=stat[:, 0:1], in_=stat[:, 3:4], func=AF.Sqrt,
              bias=self.epsc[:, 0:1], scale=1.0)
        if split:
            return
        self.ln_tile2(stat, rstat)

    def ln_tile2(self, stat, rstat):
        S_ = self.S_
        S_.op("dve", [rstat], [rstat], "reciprocal", out=stat[:, 0:1], in_=stat[:, 0:1])
        S_.op("dve", [rstat], [rstat], "scalar_tensor_tensor", out=stat[:, 1:2], in0=stat[:, 2:3], scalar=-1.0,
              in1=stat[:, 0:1], op0=ALU.mult, op1=ALU.mult)

    def phase_ln1(self, xin):
        S_ = self.S_
        l = self.l
        es = self.es_h = ExitStack()
        self.hT = self.sb(es, "hT", [128, 8, S], BF16)
        self.rhT = [Res() for _ in range(NT)]
        with ExitStack() as e2:
            rmod = Res()
            scp = self.bload(e2, "scp", self.scr["mod"][l:l + 1, D:2 * D], rmod)
            shf = self.bload(e2, "shf", self.scr["mod"][l:l + 1, 0:D], rmod)
            xt = [self.sb(e2, f"xt{i}", [128, D], F32) for i in range(2)]
            xn = [self.sb(e2, f"xn{i}", [128, D], F32) for i in range(2)]
            hb = [self.sb(e2, f"hb{i}", [128, D], BF16) for i in range(2)]
            stt = [self.sb(e2, f"stt{i}", [128, 16], F32) for i in range(2)]
            rx, rxn, rhb, rst = [Res(), Res()], [Res(), Res()], [Res(), Res()], [Res(), Res()]
            def tile_body(t):
                b = t % 2
                S_.dma("sp", [], [rx[b]], xt[b][:], xin[t * 128:(t + 1) * 128, :])
                self.ln_tile(xt[b], rx[b], None, None, stt[b], rst[b], split=True)
                yield
                self.ln_tile2(stt[b], rst[b])
                S_.op("act", [rx[b], rst[b]], [rxn[b]], "activation", out=xn[b][:], in_=xt[b][:], func=AF.Identity,
                      bias=stt[b][:, 1:2], scale=stt[b][:, 0:1])
                S_.op("pool", [rxn[b], rmod], [rxn[b]], "tensor_tensor", out=xn[b][:], in0=xn[b][:], in1=scp[:],
                      op=ALU.mult)
                yield
                S_.op("dve", [rxn[b], rmod], [rhb[b]], "tensor_tensor", out=hb[b][:], in0=xn[b][:], in1=shf[:],
                      op=ALU.add)
                pb = 6 + b
                pst = self.ps[pb][:].bitcast(BF16)
                for c in range(8):
                    S_.op("pe", [rhb[b], self.rc], [self.psr[pb]], "transpose", out=pst[:, c * 128:(c + 1) * 128],
                          in_=hb[b][:, c * 128:(c + 1) * 128], identity=self.identb[:])
                S_.op("act", [self.psr[pb]], [self.rhT[t]], "copy", out=self.hT[:, :, t * 128:(t + 1) * 128],
                      in_=pst.rearrange("p (c n) -> p c n", c=8))
            round_robin((tile_body(t) for t in range(NT)), 2)

    def phase_proj(self):
        S_ = self.S_
        l = self.l
        win = self.I["w_in"][l]

        def wsrc(c0, n):
            return win[:, c0:c0 + n].rearrange("(c p) n -> p c n", p=128)
        with ExitStack() as es:
            Wb = self.sb(es, "Wb", [128, 8, 3090], BF16)
            Wr = self.sb(es, "Wr", [128, 8, 2048], BF16)
            rW = Res()
            segs = [(0, C_AQ, 512), (896, C_BKC, 128), (1024, C_BKS, 128), (1152, C_BKW, 128), (1280, C_CQ, 768),
                    (2048, C_BVC, 128), (2176, C_AV, 256), (2432, C_BVS, 128), (2560, C_BVW, 128),
                    (2688, C_CV, 384), (3072, C_BG, 18)]
            for r in range(3):
                for g in range(2):
                    segs.append((512 + r * 128 + g * 64, C_BQ + (3 * g + r) * 64, 64))
            for (o, c0, n) in segs:
                S_.dma("pool", [], [rW], Wb[:, :, o:o + n], wsrc(c0, n))
            rWr = Res()
            for c in range(8):
                src = Wb[:, c, 0:2048].rearrange("p (h two d) -> p h two d", two=2, d=32)
                dst = Wr[:, c, :].rearrange("p (h two d) -> p h two d", two=2, d=32)
                S_.op("pool", [rW], [rWr], "tensor_copy", out=dst[:, :, 0, :], in_=src[:, :, 1, :])
                S_.op("pool", [rW], [rWr], "tensor_copy", out=dst[:, :, 1, :], in_=src[:, :, 0, :])
            cs = [self.sb(es, f"cosc{i}", [128, 2, 512], F32) for i in range(2)]
            rcs = [Res(), Res()]
            t1 = [self.sb(es, f"t1_{i}", [128, 512], F32) for i in range(2)]
            t2 = [self.sb(es, f"t2_{i}", [128, 512], F32) for i in range(2)]
            ob = [self.sb(es, f"ob{i}", [128, 512], BF16) for i in range(4)]
            rt1, rt2, rob = [Res(), Res()], [Res(), Res()], [Res() for _ in range(4)]
            k = 0
            for tc in range(8):
                cb = tc % 2
                S_.dma("sp", [], [rcs[cb]], cs[cb][:],
                       self.scr["rope"][:, :, tc * 512:(tc + 1) * 512].rearrange("a p n -> p a n"))
                rh = self.rhT[tc * 4:(tc + 1) * 4]
                for fb in range(NFB):
                    pa, pb = (0, 1) if k % 2 == 0 else (2, 3)
                    for c in range(8):
                        S_.op("pe", [rW] + rh, [self.psr[pa]], "matmul", self.ps[pa][:, :],
                              lhsT=Wb[:, c, fb * 128:(fb + 1) * 128], rhs=self.hT[:, c, tc * 512:(tc + 1) * 512],
                              start=(c == 0), stop=(c == 7))
                    o_ = k % 4
                    if fb < 16:
                        for c in range(8):
                            S_.op("pe", [rWr] + rh, [self.psr[pb]], "matmul", self.ps[pb][:, :],
                                  lhsT=Wr[:, c, fb * 128:(fb + 1) * 128], rhs=self.hT[:, c, tc * 512:(tc + 1) * 512],
                                  start=(c == 0), stop=(c == 7))
                        b2 = k % 2
                        S_.op("dve", [self.psr[pa], rcs[cb]], [rt1[b2]], "tensor_tensor", out=t1[b2][:],
                              in0=self.ps[pa][:, :], in1=cs[cb][:, 0, :], op=ALU.mult)
                        S_.op("dve", [self.psr[pb], rcs[cb]], [rt2[b2]], "tensor_tensor", out=t2[b2][:],
                              in0=self.ps[pb][:, :], in1=cs[cb][:, 1, :], op=ALU.mult)
                        S_.op("pool", [rt1[b2], rt2[b2]], [rob[o_]], "tensor_tensor", out=ob[o_][:], in0=t1[b2][:],
                              in1=t2[b2][:], op=ALU.add)
                    else:
                        S_.op("act", [self.psr[pa]], [rob[o_]], "copy", out=ob[o_][:], in_=self.ps[pa][:, :])
                    S_.dma("pool", [rob[o_]], [], self.scr["ft"][fb][:, tc * 512:(tc + 1) * 512], ob[o_][:])
                    k += 1
            tvb = [self.sb(es, f"tvb{i}", [128, 896], BF16) for i in range(2)]
            rtv = [Res(), Res()]
            for t in range(NT):
                b = t % 2
                pa, pb = (4, 5) if b == 0 else (6, 7)
                for (pp, c0, n) in ((pa, 2176, 512), (pb, 2688, 402)):
                    for c in range(8):
                        S_.op("pe", [rW, self.rhT[t]], [self.psr[pp]], "matmul", self.ps[pp][:, 0:n],
                              lhsT=self.hT[:, c, t * 128:(t + 1) * 128], rhs=Wb[:, c, c0:c0 + n],
                              start=(c == 0), stop=(c == 7))
                S_.op("act", [self.psr[pa]], [rtv[b]], "copy", out=tvb[b][:, 0:512], in_=self.ps[pa][:, :])
                S_.op("act", [self.psr[pb]], [rtv[b]], "copy", out=tvb[b][:, 512:896], in_=self.ps[pb][:, 0:384])
                S_.op("act", [self.psr[pb]], [self.rBG], "activation", out=self.BGs[:, t, :],
                      in_=self.ps[pb][:, 384:402], func=AF.Sigmoid)
                S_.dma("pool", [rtv[b]], [], self.scr["tv"][t * 128:(t + 1) * 128, :], tvb[b][:])
            S_.barrier()
        with ExitStack() as es:
            Wm = self.sb(es, "Wm", [128, 8, 3072], BF16)
            rW = Res()
            for j in range(6):
                S_.dma("pool", [], [rW], Wm[:, :, j * 512:(j + 1) * 512], wsrc(C_MG + j * 512, 512))
            mgb = [self.sb(es, f"mgb{i}", [128, 3072], BF16) for i in range(2)]
            rmg = [Res(), Res()]
            k = 0
            for t in range(NT):
                b = t % 2
                for j in range(6):
                    pp = k % 8
                    k += 1
                    for c in range(8):
                        S_.op("pe", [rW, self.rhT[t]], [self.psr[pp]], "matmul", self.ps[pp][:, :],
                              lhsT=self.hT[:, c, t * 128:(t + 1) * 128], rhs=Wm[:, c, j * 512:(j + 1) * 512],
                              start=(c == 0), stop=(c == 7))
                    S_.op("act", [self.psr[pp]], [rmg[b]], "activation", out=mgb[b][:, j * 512:(j + 1) * 512],
                          in_=self.ps[pp][:, :], func=AF.Sigmoid)
                S_.dma("pool", [rmg[b]], [], self.scr["mg"][t * 128:(t + 1) * 128, :], mgb[b][:])
            S_.barrier()
        self.es_h.close()


def host_shared(inp):
    w = np.asarray(inp["w_gate_up"], np.float32).reshape(DEPTH, NEXP, 8, 128, 2 * D)
    wg = np.ascontiguousarray(np.transpose(w, (0, 1, 3, 2, 4))).reshape(DEPTH, NEXP * 128, 16 * D)
    w = np.asarray(inp["w_down"], np.float32).reshape(DEPTH, NEXP, 8, 128, D)
    wd = np.ascontiguousarray(np.transpose(w, (0, 1, 3, 2, 4))).reshape(DEPTH, NEXP * 128, 8 * D)
    return {"w_gu_l": wg, "w_dn_l": wd}


def host_inputs(inp, consts, b, shared=None):
    if shared is None:
        shared = host_shared(inp)
    m = {"x": np.ascontiguousarray(inp["x"][b], dtype=np.float32),
         "cT": np.ascontiguousarray(np.asarray(inp["c"][b], np.float32).reshape(8, 128).T),
         "pos": np.ascontiguousarray(inp["positions"][b:b + 1]).astype(np.int32)}
    for n in WNAMES:
        if n == "cmp_peT_k":
            m[n] = np.ascontiguousarray(np.transpose(np.asarray(inp["cmp_pe_k"], np.float32), (0, 2, 1)))
        elif n == "cmp_peT_v":
            m[n] = np.ascontiguousarray(np.transpose(np.asarray(inp["cmp_pe_v"], np.float32), (0, 2, 1)))
        elif n == "b_gu_l":
            bg = np.asarray(inp["b_gate_up"], np.float32).reshape(DEPTH, NEXP, 8, 128, 2)
            m[n] = np.ascontiguousarray(np.transpose(bg, (0, 1, 3, 2, 4)).reshape(DEPTH, NEXP * 128, 16))
        elif n == "w_gu_l":
            m[n] = shared["w_gu_l"]
        elif n == "w_dn_l":
            m[n] = shared["w_dn_l"]
        else:
            m[n] = np.ascontiguousarray(inp[n], dtype=np.float32)
    for n, a in consts.items():
        m["k_" + n] = a
    return m


class AttnPipe:
    def __init__(self, prog, es, ncol, nbuf=3):
        self.p = prog
        self.S_ = prog.S_
        self.ncol = ncol
        self.G = 512 // ncol
        self.pt = [prog.sb(es, f"pt{i}", [128, 512], BF16) for i in range(nbuf)]
        self.rpt = [Res() for _ in range(nbuf)]
        self.k = 0
        self.pending = None
        self.sbanks = (0, 1, 2)

    def qtile(self, items, qrhs, nh, obank, oregs, rq, post):
        p, S_ = self.p, self.S_
        ncol = self.ncol
        n = len(items)
        for g0 in range(0, n, self.G):
            grp = items[g0:g0 + self.G]
            bank = self.sbanks[self.k % 3]
            buf = self.k % len(self.pt)
            self.k += 1
            for j, (kT, v, extras, rds) in enumerate(grp):
                reg = p.ps[bank][:, j * ncol:(j + 1) * ncol]
                S_.op("pe", rq + rds, [p.psr[bank]], "matmul", reg, lhsT=kT, rhs=qrhs, start=True,
                      stop=(len(extras) == 0))
                for xi, (xl, xr) in enumerate(extras):
                    for hh in range(nh):
                        S_.op("pe", rq + rds, [p.psr[bank]], "matmul", reg[:, hh * 128:(hh + 1) * 128], lhsT=xl,
                              rhs=xr, start=False, stop=(xi == len(extras) - 1))
            if self.pending is not None:
                self.pending()
            w = len(grp) * ncol
            S_.op("act", [p.psr[bank]], [self.rpt[buf]], "activation", out=self.pt[buf][:, 0:w],
                  in_=p.ps[bank][:, 0:w], func=AF.Exp, scale=SCALE)
            first = (g0 == 0)
            last = (g0 + self.G >= n)

            def pv(grp=grp, buf=buf, first=first, last=last, g0=g0):
                multi = nh > 1
                if multi and first:
                    wtot = oregs[-1][0] + oregs[-1][1]
                    S_.op("pe", [p.rc], [p.psr[obank]], "matmul", p.ps[obank][:, 0:wtot], lhsT=p.zeros[:, 0:128],
                          rhs=p.zeros[:, 0:wtot], start=True, stop=False)
                for j, (kT, v, extras, rds) in enumerate(grp):
                    for hh in range(nh):
                        c0, wd = oregs[hh]
                        S_.op("pe", [self.rpt[buf]] + rds, [p.psr[obank]], "matmul", p.ps[obank][:, c0:c0 + wd],
                              lhsT=self.pt[buf][:, j * ncol + hh * 128:j * ncol + (hh + 1) * 128], rhs=v,
                              start=(first and j == 0 and not multi),
                              stop=(last and j == len(grp) - 1 and hh == nh - 1))
                if last:
                    post()
            self.pending = pv

    def flush(self):
        if self.pending is not None:
            self.pending()
            self.pending = None


def _moba(self):
    S_ = self.S_
    for pr in range(2):
        with ExitStack() as es:
            QT = self.sb(es, "QT", [128, S], BF16)
            KT = self.sb(es, "KT", [128, S], BF16)
            V = self.sb(es, "V", [128, NT, 2, 65], BF16)
            km = self.sb(es, "km", [128, 16], F32)
            kmb = self.sb(es, "kmb", [128, 16], BF16)
            bT = [self.sb(es, f"bT{i}", [16, S], BF16) for i in range(2)]
            e16 = self.sb(es, "e16", [16, 16, 128], BF16)
            O = self.sb(es, "O", [128, NT, 128], BF16)
            wk = [self.sb(es, f"wk{i}", [128, 16], F32) for i in range(2)]
            m8 = [self.sb(es, f"m8{i}", [128, 8], F32) for i in range(2)]
            bs = [self.sb(es, f"bs{i}", [128, 16], F32) for i in range(2)]
            rd = [self.sb(es, f"rd{i}", [128, 1], F32) for i in range(2)]
            rQ, rK, rV, rkm, re, rO = Res(), Res(), Res(), Res(), Res(), Res()
            rbT = [[Res() for _ in range(NT)] for _ in range(2)]
            rwk, rm8, rbs, rrd = [Res(), Res()], [Res(), Res()], [Res(), Res()], [Res(), Res()]
            S_.dma("sp", [], [rQ], QT[:], self.scr["ft"][FB_AQ + pr])
            S_.dma("sp", [], [rK], KT[:], self.scr["ft"][FB_AK + pr])
            S_.dma("sp", [], [re], e16[:], self.C["esel16"])
            S_.op("pool", [], [rV], "memset", V[:, :, :, 64:65], 1.0)
            for hh in range(2):
                S_.dma("sp", [], [rV], V[:, :, hh, 0:64],
                       self.scr["tv"][:, pr * 128 + hh * 64:pr * 128 + (hh + 1) * 64].rearrange("(t p) d -> p t d", p=128))
            S_.op("dve", [rK], [rkm], "tensor_reduce", out=km[:], in_=KT[:].rearrange("p (n k) -> p n k", k=256),
                  axis=AX.X, op=ALU.add)
            S_.op("dve", [rkm], [rkm], "tensor_scalar", out=kmb[:], in0=km[:], scalar1=1.0 / 256, scalar2=None,
                  op0=ALU.mult)
            def bias_body(hh, qt, kk):
                h0 = 64 * hh
                own = qt // 2
                b = kk % 2
                pg = 5 + (kk % 2)
                S_.op("dve", [], [rwk[b]], "memset", wk[b][:, own:16], -BIG)
                S_.op("pe", [rQ, rkm], [self.psr[pg]], "matmul", self.ps[pg][:, 0:16],
                      lhsT=QT[h0:h0 + 64, qt * 128:(qt + 1) * 128], rhs=kmb[h0:h0 + 64, :], start=True, stop=True)
                S_.op("dve", [self.psr[pg]], [rwk[b]], "tensor_copy", out=wk[b][:, 0:own], in_=self.ps[pg][:, 0:own])
                S_.op("dve", [rwk[b]], [rm8[b]], "max", out=m8[b][:], in_=wk[b][:])
                S_.op("dve", [rwk[b], rm8[b]], [rbs[b]], "tensor_scalar", out=bs[b][:], in0=wk[b][:],
                      scalar1=m8[b][:, 2:3], scalar2=NEG, op0=ALU.is_lt, op1=ALU.mult)
                yield
                S_.op("pe", [rbs[b], self.rc], [self.psr[7]], "transpose", out=self.ps[7][0:16, 0:128],
                      in_=bs[b][:], identity=self.identf[:])
                S_.op("act", [self.psr[7]], [rbT[hh][qt]], "copy", out=bT[hh][:, qt * 128:(qt + 1) * 128],
                      in_=self.ps[7][0:16, 0:128])
            round_robin((bias_body(hh, qt, i) for i, (hh, qt) in
                         enumerate((hh, qt) for hh in range(2) for qt in range(8, NT))), 2)
            ap_ = AttnPipe(self, es, 128)
            for hh in range(2):
                h0 = 64 * hh
                for qt in range(NT):
                    own = qt // 2
                    items = []
                    for kt in range(qt + 1):
                        n = kt // 2
                        ex = []
                        rds = [rK, rV]
                        if n < own and qt >= 8:
                            ex = [(e16[:, n, :], bT[hh][:, qt * 128:(qt + 1) * 128])]
                            rds = rds + [re, rbT[hh][qt]]
                        elif kt == qt:
                            ex = [(self.identb[:], self.masks[:, 0, :])]
                        items.append((KT[h0:h0 + 64, kt * 128:(kt + 1) * 128], V[:, kt, hh, :], ex, rds))
                    ob = 3 + (qt % 2)

                    def post(ob=ob, qt=qt, hh=hh):
                        b = qt % 2
                        S_.op("dve", [self.psr[ob]], [rrd[b]], "reciprocal", out=rd[b][:], in_=self.ps[ob][:, 64:65])
                        S_.op("dve", [self.psr[ob], rrd[b]], [rO], "tensor_scalar", out=O[:, qt, hh * 64:(hh + 1) * 64],
                              in0=self.ps[ob][:, 0:64], scalar1=rd[b][:, 0:1], scalar2=None, op0=ALU.mult)
                    ap_.qtile(items, QT[h0:h0 + 64, qt * 128:(qt + 1) * 128], 1, ob, [(0, 65)], [rQ], post)
            ap_.flush()
            S_.dma("pool", [rO], [], self.scr["o"][:, pr * 128:(pr + 1) * 128].rearrange("(t p) c -> p t c", p=128), O[:])
            S_.barrier()


Prog.phase_moba = _moba


def _nsa(self):
    S_ = self.S_
    l = self.l
    with ExitStack() as es:
        QTb = self.sb(es, "QTb", [128, 3, S], BF16)
        KcP = self.sb(es, "KcP", [128, S], BF16)
        VcP = self.sb(es, "VcP", [128, S], BF16)
        KsT = self.sb(es, "KsT", [128, S], BF16)
        KwT = self.sb(es, "KwT", [128, S], BF16)
        Vs = self.sb(es, "Vs", [128, NT, 2, 65], BF16)
        Vw = self.sb(es, "Vw", [128, NT, 2, 65], BF16)
        cmpm = self.sb(es, "cmpm", [128, 2, S], BF16)
        nadd = self.sb(es, "nadd", [128, NT, 64], F32)
        nmin = self.sb(es, "nmin", [128, NT, 64], F32)
        e64 = self.sb(es, "e64", [128, 32, 128], BF16)
        w1 = {"k": self.sb(es, "w1k", [128, 32, 128], BF16), "v": self.sb(es, "w1v", [128, 32, 128], BF16)}
        pe_ = {"k": self.sb(es, "pek", [128, 32], BF16), "v": self.sb(es, "pev", [128, 32], BF16)}
        w2kp = self.sb(es, "w2kp", [128, 2, 128], BF16)
        w2v = self.sb(es, "w2v", [128, 64], BF16)
        ovl = self.sb(es, "ovl", [128, 2, 64], BF16)
        KcT = self.sb(es, "KcT", [128, 256], BF16)
        VcA = self.sb(es, "VcA", [128, 2, 2, 129], BF16)
        hid = [self.sb(es, f"hid{i}", [128, 256], BF16) for i in range(2)]
        peb = [self.sb(es, f"peb{i}", [128, 1], F32) for i in range(2)]
        Oall = self.sb(es, "Oall", [128, NT, 384], BF16)
        rld, rw, rV, rKc, rVc, rO = Res(), Res(), Res(), Res(), Res(), Res()
        for r in range(3):
            S_.dma("sp", [], [rld], QTb[:, r, :], self.scr["ft"][FB_BQ + r])
        for t_, fb in ((KcP, FB_KC), (VcP, FB_VC), (KsT, FB_KS), (KwT, FB_KW)):
            S_.dma("sp", [], [rld], t_[:], self.scr["ft"][fb])
        S_.op("pool", [], [rld], "memset", e64[64:128, :, :], 0.0)
        S_.dma("sp", [], [rld], e64[0:64, :, :], self.C["esel64"])
        for t_, n_ in ((cmpm, "cmpmask"), (nadd, "nsa_add"), (nmin, "nsa_min"), (ovl, "overlap")):
            S_.dma("sp", [], [rld], t_[:], self.C[n_])
        for vt, c0 in ((Vs, 256), (Vw, 384)):
            S_.op("pool", [], [rV], "memset", vt[:, :, :, 64:65], 1.0)
            for g in range(2):
                S_.dma("sp", [], [rV], vt[:, :, g, 0:64],
                       self.scr["tv"][:, c0 + g * 64:c0 + (g + 1) * 64].rearrange("(t p) d -> p t d", p=128))
        S_.op("pool", [], [rw], "memset", w2kp[:], 0.0)
        for kind in ("k", "v"):
            for g in range(2):
                S_.dma("pool", [], [rw], w1[kind][64 * g:64 * g + 64, :, :],
                       self.I["cmp_w1_" + kind][l].rearrange("(l d) h -> d l h", d=64))
                S_.dma("pool", [], [rw], pe_[kind][64 * g:64 * g + 64, :], self.I["cmp_peT_" + kind][l])
        for g in range(2):
            S_.dma("pool", [], [rw], w2kp[:, g, 64 * g:64 * g + 64], self.I["cmp_w2_k"][l])
        S_.dma("pool", [], [rw], w2v[:], self.I["cmp_w2_v"][l])
        S_.op("pool", [], [rVc], "memset", VcA[:, :, :, 64:65], 1.0)
        for nt in range(2):
            for g in range(2):
                S_.op("pool", [rld], [rVc], "tensor_copy", out=VcA[:, nt, g, 65:129], in_=ovl[:, nt, :])
        rhid, rpeb = [Res(), Res()], [Res(), Res()]
        kk = 0
        for kind, src in (("k", KcP), ("v", VcP)):
            s16 = src[:].rearrange("p (n s) -> p n s", s=16)
            for g in range(2):
                h0 = 64 * g
                b = kk % 2
                kk += 1
                for li in range(32):
                    S_.op("pe", [rw], [self.psr[5]], "matmul", self.ps[5][:, 0:1], lhsT=w1[kind][h0:h0 + 64, li, :],
                          rhs=pe_[kind][h0:h0 + 64, li:li + 1], start=(li == 0), stop=(li == 31))
                S_.op("dve", [self.psr[5]], [rpeb[b]], "tensor_copy", out=peb[b][:], in_=self.ps[5][:, 0:1])
                for li in range(32):
                    rhs = s16[h0:h0 + 64, 0:255, li] if li < 16 else s16[h0:h0 + 64, 1:256, li - 16]
                    S_.op("pe", [rw, rld], [self.psr[6]], "matmul", self.ps[6][:, 0:255], lhsT=w1[kind][h0:h0 + 64, li, :],
                          rhs=rhs, start=(li == 0), stop=(li == 31))
                S_.op("dve", [], [rhid[b]], "memset", hid[b][:, 255:256], 0.0)
                S_.op("act", [self.psr[6], rpeb[b]], [rhid[b]], "activation", out=hid[b][:, 0:255], in_=self.ps[6][:, 0:255],
                      func=AF.Silu, bias=peb[b][:, 0:1], scale=1.0)
                if kind == "k":
                    S_.op("pe", [rw, rhid[b]], [self.psr[7]], "matmul", self.ps[7][:, 0:256], lhsT=w2kp[:, g, :],
                          rhs=hid[b][:], start=(g == 0), stop=(g == 1))
                    if g == 1:
                        S_.op("act", [self.psr[7]], [rKc], "copy", out=KcT[:], in_=self.ps[7][:, 0:256])
                else:
                    for nt in range(2):
                        S_.op("pe", [rw, rhid[b]], [self.psr[7]], "matmul", self.ps[7][:, nt * 64:(nt + 1) * 64],
                              lhsT=hid[b][:, nt * 128:(nt + 1) * 128], rhs=w2v[:], start=True, stop=True)
                    S_.op("act", [self.psr[7]], [rVc], "copy", out=VcA[:, :, g, 0:64],
                          in_=self.ps[7][:, 0:128].rearrange("p (n d) -> p n d", d=64))
        import os
        if self.debug:
            S_.dma("pool", [rKc], [], self.scr["dbg1"], KcT[:])
            S_.dma("pool", [rVc], [], self.scr["dbg2"], VcA[:].rearrange("p a b c -> p (a b c)"))
        if os.environ.get("NSA_STOP") == "1":
            S_.barrier()
            return
        bT = [[self.sb(es, f"nbT{g}{b}", [128, 128], BF16) for b in range(2)] for g in range(2)]
        rbT = [[Res(), Res()], [Res(), Res()]]
        for g in range(2):
            for b in range(2):
                S_.op("pool", [], [rbT[g][b]], "memset", bT[g][b][:], 0.0)
        Ob = [self.sb(es, f"Ob{b}", [128, 6, 64], F32) for b in range(2)]
        rOb = [Res(), Res()]
        NS = 4
        rdn = [self.sb(es, f"rdn{i}", [128, 3], F32) for i in range(NS)]
        sc3 = [self.sb(es, f"sc3{i}", [128, 3], F32) for i in range(NS)]
        imp = [self.sb(es, f"imp{i}", [128, 64], F32) for i in range(2)]
        imp2 = [self.sb(es, f"imp2{i}", [128, 64], F32) for i in range(2)]
        m8a = [self.sb(es, f"m8a{i}", [128, 8], F32) for i in range(2)]
        m8b = [self.sb(es, f"m8b{i}", [128, 8], F32) for i in range(2)]
        bsf = [self.sb(es, f"bsf{i}", [128, 64], F32) for i in range(2)]
        rsm = [Res() for _ in range(NS)]
        rimp = [Res(), Res()]
        ap_ = AttnPipe(self, es, 384)
        cnt = {"o": 0, "s": 0, "i": 0}

        def gw(qt, g, br):
            return self.BGs[:, qt, :].rearrange("p (h k) -> p h k", k=3)[:, 3 * g:3 * g + 3, br]

        def post_common(ob, wd, qt, g, br, first):
            i = cnt["s"] % NS
            cnt["s"] += 1
            b = qt % 2
            den = self.ps[ob][:, 0:3 * wd].rearrange("p (r w) -> p r w", w=wd)[:, :, 64]
            S_.op("dve", [self.psr[ob]], [rsm[i]], "tensor_scalar", out=rdn[i][:], in0=den, scalar1=1e-30, scalar2=None,
                  op0=ALU.max)
            S_.op("dve", [rsm[i]], [rsm[i]], "reciprocal", out=rdn[i][:], in_=rdn[i][:])
            S_.op("dve", [rsm[i], self.rBG], [rsm[i]], "tensor_tensor", out=sc3[i][:], in0=rdn[i][:], in1=gw(qt, g, br),
                  op=ALU.mult)
            for r in range(3):
                src = self.ps[ob][:, r * wd:r * wd + 64]
                if first:
                    S_.op("dve", [self.psr[ob], rsm[i]], [rOb[b]], "tensor_scalar", out=Ob[b][:, 3 * g + r, :], in0=src,
                          scalar1=sc3[i][:, r:r + 1], scalar2=None, op0=ALU.mult)
                else:
                    S_.op("dve", [self.psr[ob], rsm[i]], [rOb[b]], "scalar_tensor_tensor", out=Ob[b][:, 3 * g + r, :],
                          in0=src, scalar=sc3[i][:, r:r + 1], in1=Ob[b][:, 3 * g + r, :], op0=ALU.mult, op1=ALU.add)
            return i

        nqt = int(os.environ.get("NSA_NQT", NT))
        brs = os.environ.get("NSA_BR", "csw")
        deferred = []

        def emit_cmp(qt):
            b = qt % 2
            qs = slice(qt * 128, (qt + 1) * 128)
            for g in range(2):
                h0 = 64 * g
                qrhs = QTb[h0:h0 + 64, :, qs]
                items = []
                for nt in ([0, 1] if qt >= 16 else [0]):
                    ex = []
                    if nt == 1 or qt < 17:
                        ex = [(self.identb[:], cmpm[:, nt, qs])]
                    items.append((KcT[h0:h0 + 64, nt * 128:(nt + 1) * 128], VcA[:, nt, g, :], ex, [rKc, rVc, rld]))
                ob = 3 + cnt["o"] % 3
                cnt["o"] += 1

                def post_cmp(ob=ob, qt=qt, g=g, b=b):
                    i = post_common(ob, 129, qt, g, 0, True)
                    j = cnt["i"] % 2
                    cnt["i"] += 1
                    for r in range(3):
                        src = self.ps[ob][:, r * 129 + 65:r * 129 + 129]
                        if r == 0:
                            S_.op("dve", [self.psr[ob], rsm[i]], [rimp[j]], "tensor_scalar", out=imp[j][:], in0=src,
                                  scalar1=rdn[i][:, 0:1], scalar2=None, op0=ALU.mult)
                        else:
                            S_.op("dve", [self.psr[ob], rsm[i]], [rimp[j]], "scalar_tensor_tensor", out=imp[j][:], in0=src,
                                  scalar=rdn[i][:, r:r + 1], in1=imp[j][:], op0=ALU.mult, op1=ALU.add)
                    S_.op("dve", [rld], [rimp[j]], "tensor_tensor", out=imp[j][:], in0=imp[j][:], in1=nadd[:, qt, :], op=ALU.add)
                    S_.op("dve", [rld], [rimp[j]], "tensor_tensor", out=imp[j][:], in0=imp[j][:], in1=nmin[:, qt, :], op=ALU.min)
                    S_.op("dve", [rimp[j]], [rimp[j]], "max", out=m8a[j][:], in_=imp[j][:])
                    S_.op("dve", [rimp[j]], [rimp[j]], "match_replace", out=imp2[j][:], in_to_replace=m8a[j][:],
                          in_values=imp[j][:], imm_value=-BIG)
                    S_.op("dve", [rimp[j]], [rimp[j]], "max", out=m8b[j][:], in_=imp2[j][:])
                    S_.op("dve", [rimp[j]], [rimp[j]], "tensor_scalar", out=bsf[j][:], in0=imp[j][:], scalar1=m8b[j][:, 7:8],
                          scalar2=NEG, op0=ALU.is_lt, op1=ALU.mult)
                    def tr(j=j, g=g, b=b):
                        S_.op("pe", [rimp[j], self.rc], [self.psr[7]], "transpose", out=self.ps[7][0:64, 0:128], in_=bsf[j][:],
                              identity=self.identf[:])
                        S_.op("act", [self.psr[7]], [rbT[g][b]], "copy", out=bT[g][b][0:64, :], in_=self.ps[7][0:64, 0:128])
                    deferred.append(tr)
                ap_.qtile(items, qrhs, 3, ob, [(r * 129, 129) for r in range(3)], [rld], post_cmp)

        def emit_sw(qt):
            b = qt % 2
            qs = slice(qt * 128, (qt + 1) * 128)
            for br, KT_, V_ in ((1, KsT, Vs), (2, KwT, Vw)):
                if "csw"[br] not in brs:
                    continue
                for g in range(2):
                    h0 = 64 * g
                    qrhs = QTb[h0:h0 + 64, :, qs]
                    items = []
                    k0 = 0 if br == 1 else max(0, qt - 4)
                    for kt in range(k0, qt + 1):
                        ex = []
                        rds = [rld, rV]
                        if br == 1:
                            ex.append((e64[:, kt, :], bT[g][b][:]))
                            rds = rds + [rbT[g][b]]
                        if kt == qt:
                            ex.append((self.identb[:], self.masks[:, 0, :]))
                        elif br == 2 and kt == qt - 4:
                            ex.append((self.identb[:], self.masks[:, 1, :]))
                        items.append((KT_[h0:h0 + 64, kt * 128:(kt + 1) * 128], V_[:, kt, g, :], ex, rds))
                    ob = 3 + cnt["o"] % 3
                    cnt["o"] += 1

                    def post_sw(ob=ob, qt=qt, g=g, br=br, b=b):
                        post_common(ob, 65, qt, g, br, False)
                        if br == 2 and g == 1:
                            S_.op("pool", [rOb[b]], [rO], "tensor_copy", out=Oall[:, qt, :],
                                  in_=Ob[b][:].rearrange("p h d -> p (h d)"))
                    ap_.qtile(items, qrhs, 3, ob, [(r * 65, 65) for r in range(3)], [rld], post_sw)

        for qt in range(nqt + 1):
            if qt < nqt:
                emit_cmp(qt)
            if qt >= 1:
                emit_sw(qt - 1)
            else:
                ap_.flush()
            while deferred:
                deferred.pop(0)()
        ap_.flush()
        S_.dma("pool", [rO], [], self.scr["o"][:, 256:640].rearrange("(t p) c -> p t c", p=128), Oall[:])
        S_.barrier()


Prog.phase_nsa = _nsa


def _dil(self):
    S_ = self.S_
    with ExitStack() as es:
        QTc = self.sb(es, "QTc", [128, 3, S], BF16)
        KTc = self.sb(es, "KTc", [128, 3, S], BF16)
        Vg = [self.sb(es, f"Vg{gi}", [128, NT, 2, 65], BF16) for gi in range(3)]
        rld, rV = Res(), Res()
        for gi in range(3):
            S_.dma("sp", [], [rld], QTc[:, gi, :], self.scr["ft"][FB_CQ + gi])
            S_.dma("sp", [], [rld], KTc[:, gi, :], self.scr["ft"][FB_CK + gi])
        for gi, dil in enumerate((1, 4, 16)):
            ntile = NT // dil
            S_.op("pool", [], [rV], "memset", Vg[gi][:, :, :, 64:65], 1.0)
            for r in range(dil):
                for hs in range(2):
                    c0 = 512 + gi * 128 + hs * 64
                    src = self.scr["tv"][:, c0:c0 + 64].rearrange("(l d) c -> d l c", d=dil)[r]
                    S_.dma("sp", [], [rV], Vg[gi][:, r * ntile:(r + 1) * ntile, hs, 0:64],
                           src.rearrange("(j p) c -> p j c", p=128))
        ocs = [self.sb(es, f"ocs{i}", [128, 65], F32) for i in range(4)]
        rocs = [Res() for _ in range(4)]
        ap_ = AttnPipe(self, es, 128)
        cnt = {"o": 0}
        for gi, dil in enumerate((1, 4, 16)):
            ntile = NT // dil
            ocv = self.scr["oc"][gi].rearrange("(l d) c -> d l c", d=dil)
            for hs in range(2):
                h0 = 64 * hs
                qv = QTc[h0:h0 + 64, gi, :].rearrange("p (l d) -> p d l", d=dil)
                kv = KTc[h0:h0 + 64, gi, :].rearrange("p (l d) -> p d l", d=dil)
                for r in range(dil):
                    for j in range(ntile):
                        items = []
                        for kt in ([j - 1, j] if j >= 1 else [j]):
                            mk = 0 if kt == j else 2
                            items.append((kv[:, r, kt * 128:(kt + 1) * 128], Vg[gi][:, r * ntile + kt, hs, :],
                                          [(self.identb[:], self.masks[:, mk, :])], [rld, rV]))
                        ob = 3 + cnt["o"] % 3
                        cnt["o"] += 1

                        def post(ob=ob, r=r, j=j, hs=hs, ocv=ocv, i=cnt["o"] % 4):
                            S_.op("dve", [self.psr[ob]], [rocs[i]], "tensor_copy", out=ocs[i][:], in_=self.ps[ob][:, 0:65])
                            S_.dma("pool", [rocs[i]], [], ocv[r][j * 128:(j + 1) * 128, hs * 65:(hs + 1) * 65], ocs[i][:])
                        ap_.qtile(items, qv[:, r, j * 128:(j + 1) * 128], 1, ob, [(0, 65)], [rld], post)
        ap_.flush()
        S_.barrier()
        Oc = self.sb(es, "Oc", [128, NT, 128], BF16)
        rOc = Res()
        acc = [[self.sb(es, f"acc{i}{gi}", [128, 8, 130], F32) for gi in range(3)] for i in range(2)]
        racc = [Res(), Res()]
        rdc = [self.sb(es, f"rdc{i}", [128, 8, 2], F32) for i in range(2)]
        for ch in range(4):
            i = ch % 2
            for gi in range(3):
                S_.dma("sp", [], [racc[i]], acc[i][gi][:],
                       self.scr["oc"][gi][ch * 1024:(ch + 1) * 1024, :].rearrange("(t p) c -> p t c", p=128))
            S_.op("dve", [racc[i]], [racc[i]], "tensor_tensor", out=acc[i][0][:], in0=acc[i][0][:], in1=acc[i][1][:], op=ALU.add)
            S_.op("dve", [racc[i]], [racc[i]], "tensor_tensor", out=acc[i][0][:], in0=acc[i][0][:], in1=acc[i][2][:], op=ALU.add)
            for hs in range(2):
                S_.op("dve", [racc[i]], [racc[i]], "reciprocal", out=rdc[i][:, :, hs:hs + 1],
                      in_=acc[i][0][:, :, hs * 65 + 64:hs * 65 + 65])
            for t in range(8):
                for hs in range(2):
                    S_.op("dve", [racc[i]], [rOc], "tensor_scalar", out=Oc[:, ch * 8 + t, hs * 64:(hs + 1) * 64],
                          in0=acc[i][0][:, t, hs * 65:hs * 65 + 64], scalar1=rdc[i][:, t, hs:hs + 1], scalar2=None,
                          op0=ALU.mult)
        S_.dma("pool", [rOc], [], self.scr["o"][:, 640:768].rearrange("(t p) c -> p t c", p=128), Oc[:])
        S_.barrier()


Prog.phase_dil = _dil


def _merge(self, xin, xout):
    S_ = self.S_
    l = self.l
    with ExitStack() as es:
        Wa = self.sb(es, "Wa", [128, 6, D], BF16)
        Wo = self.sb(es, "Wo", [128, 8, D], BF16)
        Wr = self.sb(es, "Wr", [128, 8, NEXP], BF16)
        rW, rB = Res(), Res()
        for nm, c0, nck in (("w_branch_a", 0, 2), ("w_branch_b", 2, 3), ("w_branch_c", 5, 1)):
            S_.dma("pool", [], [rW], Wa[:, c0:c0 + nck, :], self.I[nm][l].rearrange("(c p) n -> p c n", p=128))
        S_.dma("pool", [], [rW], Wo[:], self.I["w_out"][l].rearrange("(c p) n -> p c n", p=128))
        S_.dma("pool", [], [rW], Wr[:], self.I["router_w"][l].rearrange("(c p) n -> p c n", p=128))
        g1b = self.bload(es, "g1b", self.scr["mod"][l:l + 1, 2 * D:3 * D], rB)
        l1g = self.bload(es, "l1g", self.I["ln1_g"][l:l + 1, :], rB)
        l1b = self.bload(es, "l1b", self.I["ln1_b"][l:l + 1, :], rB)
        sc2 = self.bload(es, "sc2", self.scr["mod"][l:l + 1, 4 * D:5 * D], rB)
        sh2 = self.bload(es, "sh2", self.scr["mod"][l:l + 1, 3 * D:4 * D], rB)
        rbb = self.sb(es, "rbb", [128, NEXP], F32)
        S_.dma("sp", [], [rB], rbb[:], self.I["router_b"][l:l + 1, :].partition_broadcast(128))

        def dbl(name, shape, dt):
            return [self.sb(es, f"{name}{i}", shape, dt) for i in range(2)], [Res(), Res()]
        ot, rot = dbl("ot", [128, 768], BF16)
        gt, rgt = dbl("gt", [128, 3072], BF16)
        xt, rxt = dbl("mxt", [128, D], F32)
        oT, roT = dbl("oT", [128, 6, 128], BF16)
        m1, rm1 = dbl("m1", [128, 512], F32)
        m2, rm2 = dbl("m2", [128, 512], F32)
        m3, rm3 = dbl("m3", [128, 512], F32)
        mb, rmb = dbl("mb", [128, D], BF16)
        mT, rmT = dbl("mT", [128, 8, 128], BF16)
        yt, ryt = dbl("yt", [128, D], F32)
        zt, rzt = dbl("zt", [128, D], F32)
        x1, rx1 = dbl("x1", [128, D], F32)
        xn2, rxn2 = dbl("xn2", [128, D], F32)
        hb2, rhb2 = dbl("hb2", [128, D], BF16)
        hT2, rhT2 = dbl("hT2", [128, 8, 128], BF16)
        st1, rst1 = dbl("st1", [128, 16], F32)
        st2, rst2 = dbl("st2", [128, 16], F32)
        lg, rlg = dbl("lg", [128, NEXP], F32)
        ex, rex = dbl("ex", [128, NEXP], F32)
        m8, rm8 = dbl("rm8", [128, 8], F32)
        sm, rsm = dbl("rsm", [128, 4], F32)
        def tile_body(t):
            b = t % 2
            rows = slice(t * 128, (t + 1) * 128)
            S_.dma("sp", [], [rot[b]], ot[b][:], self.scr["o"][rows, :])
            S_.dma("sp", [], [rgt[b]], gt[b][:], self.scr["mg"][rows, :])
            S_.dma("sp", [], [rxt[b]], xt[b][:], xin[rows, :])
            pT = 6 + b
            pst = self.ps[pT][:].bitcast(BF16)
            for c in range(6):
                S_.op("pe", [rot[b], self.rc], [self.psr[pT]], "transpose", out=pst[:, c * 128:(c + 1) * 128],
                      in_=ot[b][:, c * 128:(c + 1) * 128], identity=self.identb[:])
            S_.op("act", [self.psr[pT]], [roT[b]], "copy", out=oT[b][:], in_=pst[:, 0:768].rearrange("p (c n) -> p c n", c=6))
            yield
            for half in range(2):
                hs_ = slice(half * 512, (half + 1) * 512)
                pbs = (0, 1, 2) if half == 0 else (3, 4, 5)
                for bi, (c0, nck) in enumerate(((0, 2), (2, 3), (5, 1))):
                    for c in range(nck):
                        S_.op("pe", [roT[b], rW], [self.psr[pbs[bi]]], "matmul", self.ps[pbs[bi]][:, :],
                              lhsT=oT[b][:, c0 + c, :], rhs=Wa[:, c0 + c, hs_], start=(c == 0), stop=(c == nck - 1))
                for bi, (mm_, rmm) in enumerate(((m1, rm1), (m2, rm2), (m3, rm3))):
                    S_.op("dve", [self.psr[pbs[bi]], rgt[b]], [rmm[b]], "tensor_tensor", out=mm_[b][:],
                          in0=self.ps[pbs[bi]][:, :], in1=gt[b][:, bi * 1024 + half * 512:bi * 1024 + (half + 1) * 512],
                          op=ALU.mult)
                S_.op("pool", [rm1[b], rm2[b]], [rm1[b]], "tensor_tensor", out=m1[b][:], in0=m1[b][:], in1=m2[b][:], op=ALU.add)
                S_.op("pool", [rm1[b], rm3[b]], [rmb[b]], "tensor_tensor", out=mb[b][:, hs_], in0=m1[b][:], in1=m3[b][:],
                      op=ALU.add)
                yield
            for c in range(8):
                S_.op("pe", [rmb[b], self.rc], [self.psr[pT]], "transpose", out=pst[:, c * 128:(c + 1) * 128],
                      in_=mb[b][:, c * 128:(c + 1) * 128], identity=self.identb[:])
            S_.op("act", [self.psr[pT]], [rmT[b]], "copy", out=mT[b][:], in_=pst.rearrange("p (c n) -> p c n", c=8))
            yield
            for half in range(2):
                hs_ = slice(half * 512, (half + 1) * 512)
                pb_ = half
                for c in range(8):
                    S_.op("pe", [rmT[b], rW], [self.psr[pb_]], "matmul", self.ps[pb_][:, :], lhsT=mT[b][:, c, :],
                          rhs=Wo[:, c, hs_], start=(c == 0), stop=(c == 7))
                S_.op("dve", [self.psr[pb_], rB], [ryt[b]], "tensor_tensor", out=yt[b][:, hs_], in0=self.ps[pb_][:, :],
                      in1=g1b[:, hs_], op=ALU.mult)
            yield
            S_.op("dve", [ryt[b], rxt[b]], [rzt[b]], "scalar_tensor_tensor", out=zt[b][:], in0=xt[b][:], scalar=ALPHA,
                  in1=yt[b][:], op0=ALU.mult, op1=ALU.add)
            self.ln_tile(zt[b], rzt[b], None, None, st1[b], rst1[b], split=True)
            yield
            self.ln_tile2(st1[b], rst1[b])
            S_.op("act", [rzt[b], rst1[b]], [rzt[b]], "activation", out=zt[b][:], in_=zt[b][:], func=AF.Identity,
                  bias=st1[b][:, 1:2], scale=st1[b][:, 0:1])
            S_.op("pool", [rzt[b], rB], [rzt[b]], "tensor_tensor", out=zt[b][:], in0=zt[b][:], in1=l1g[:], op=ALU.mult)
            yield
            S_.op("dve", [rzt[b], rB], [rx1[b]], "tensor_tensor", out=x1[b][:], in0=zt[b][:], in1=l1b[:], op=ALU.add)
            S_.dma("pool", [rx1[b]], [], xout[rows, :], x1[b][:])
            self.ln_tile(x1[b], rx1[b], None, None, st2[b], rst2[b], split=True)
            yield
            self.ln_tile2(st2[b], rst2[b])
            S_.op("act", [rx1[b], rst2[b]], [rxn2[b]], "activation", out=xn2[b][:], in_=x1[b][:], func=AF.Identity,
                  bias=st2[b][:, 1:2], scale=st2[b][:, 0:1])
            S_.op("pool", [rxn2[b], rB], [rxn2[b]], "tensor_tensor", out=xn2[b][:], in0=xn2[b][:], in1=sc2[:], op=ALU.mult)
            yield
            S_.op("dve", [rxn2[b], rB], [rhb2[b]], "tensor_tensor", out=hb2[b][:], in0=xn2[b][:], in1=sh2[:], op=ALU.add)
            for c in range(8):
                S_.op("pe", [rhb2[b], self.rc], [self.psr[pT]], "transpose", out=pst[:, c * 128:(c + 1) * 128],
                      in_=hb2[b][:, c * 128:(c + 1) * 128], identity=self.identb[:])
            S_.op("act", [self.psr[pT]], [rhT2[b]], "copy", out=hT2[b][:], in_=pst.rearrange("p (c n) -> p c n", c=8))
            S_.dma("pool", [rhb2[b]], [], self.scr["h2"][rows, :], hb2[b][:])
            yield
            for c in range(8):
                S_.op("pe", [rhT2[b], rW], [self.psr[2]], "matmul", self.ps[2][:, 0:NEXP], lhsT=hT2[b][:, c, :],
                      rhs=Wr[:, c, :], start=(c == 0), stop=(c == 7))
            S_.op("dve", [self.psr[2], rB], [rlg[b]], "tensor_tensor", out=lg[b][:], in0=self.ps[2][:, 0:NEXP], in1=rbb[:],
                  op=ALU.add)
            S_.op("dve", [rlg[b]], [rm8[b]], "max", out=m8[b][:], in_=lg[b][:])
            S_.op("dve", [rm8[b]], [rsm[b]], "tensor_scalar", out=sm[b][:, 0:1], in0=m8[b][:, 0:1], scalar1=-1.0, scalar2=None,
                  op0=ALU.mult)
            S_.op("act", [rlg[b], rsm[b]], [rex[b]], "activation", out=ex[b][:], in_=lg[b][:], func=AF.Exp,
                  bias=sm[b][:, 0:1], scale=1.0)
            yield
            S_.op("dve", [rlg[b], rm8[b], rex[b]], [rex[b]], "scalar_tensor_tensor", out=ex[b][:], in0=lg[b][:],
                  scalar=m8[b][:, 3:4], in1=ex[b][:], op0=ALU.is_ge, op1=ALU.mult)
            S_.op("dve", [rlg[b], rm8[b]], [self.rS], "tensor_scalar", out=self.Sall[:, t, :], in0=lg[b][:],
                  scalar1=m8[b][:, 3:4], scalar2=None, op0=ALU.is_ge)
            S_.op("dve", [rex[b]], [rsm[b]], "tensor_reduce", out=sm[b][:, 1:2], in_=ex[b][:], axis=AX.X, op=ALU.add)
            S_.op("dve", [rsm[b]], [rsm[b]], "reciprocal", out=sm[b][:, 2:3], in_=sm[b][:, 1:2])
            S_.op("dve", [rex[b], rsm[b]], [self.rG], "tensor_scalar", out=self.Gall[:, t, :], in0=ex[b][:],
                  scalar1=sm[b][:, 2:3], scalar2=None, op0=ALU.mult)
        round_robin((tile_body(t) for t in range(NT)), 2)
        S_.barrier()


Prog.phase_merge = _merge


def _moe(self):
    S_ = self.S_
    l = self.l
    TS = 1024
    with ExitStack() as es:
        wgu = [self.sb(es, f"wgu{i}", [128, 8, 2 * D], BF16) for i in range(2)]
        wdn = [self.sb(es, f"wdn{i}", [128, 8, D], BF16) for i in range(2)]
        rwg, rwd = [Res(), Res()], [Res(), Res()]
        bgu = self.sb(es, "bgu", [128, NEXP * 16], F32)
        bdn = self.sb(es, "bdn", [NEXP, D], F32)
        h2 = self.sb(es, "h2", [128, 8, TS], BF16)
        yacc = self.sb(es, "yacc", [128, 8, D], F32)
        GT = self.sb(es, "GT", [NEXP, 8, 128], F32)
        actT = [self.sb(es, f"actT{i}", [128, 8, 512], BF16) for i in range(2)]
        xg = [self.sb(es, f"xg{i}", [128, 512], F32) for i in range(2)]
        sg = [self.sb(es, f"sg{i}", [128, 512], F32) for i in range(2)]
        xl = [self.sb(es, f"xl{i}", [128, 512], F32) for i in range(2)]
        rB, rh2, rGT = Res(), Res(), Res()
        ry = [Res() for _ in range(8)]
        ract = [Res(), Res()]
        rxg, rsg, rxl = [Res(), Res()], [Res(), Res()], [Res(), Res()]
        S_.dma("sp", [], [rB], bgu[:], self.I["b_gu_l"][l])
        S_.dma("sp", [], [rB], bdn[:], self.I["b_down"][l])
        bguv = bgu[:].rearrange("p (e j two) -> p e j two", e=NEXP, two=2)

        def load_wg(e, wb):
            src = self.I["w_gate_up"][l, e].rearrange("(c p) n -> p c n", p=128)
            for hlf in range(2):
                S_.dma("pool", [], [rwg[wb]], wgu[wb][:, hlf * 4:(hlf + 1) * 4, :], src[:, hlf * 4:(hlf + 1) * 4, :])

        def load_wd(e, wb):
            S_.dma("pool", [], [rwd[wb]], wdn[wb][:], self.I["w_down"][l, e].rearrange("(c p) n -> p c n", p=128))
        kq = 0
        ky = 0
        kw = 0
        pend = None
        load_wg(0, 0)
        load_wd(0, 0)
        for ts in range(S // TS):
            S_.dma("sp", [], [rh2], h2[:], self.scr["h2t"][:, :, ts * TS:(ts + 1) * TS])
            for tt in range(8):
                tg = ts * 8 + tt
                pb = 4 + ky % 4
                ky += 1
                S_.op("pe", [self.rG, self.rc], [self.psr[pb]], "transpose", out=self.ps[pb][0:NEXP, 0:128],
                      in_=self.Gall[:, tg, :], identity=self.identf[:])
                S_.op("act", [self.psr[pb]], [rGT], "copy", out=GT[:, tt, :], in_=self.ps[pb][0:NEXP, 0:128])
                for half in range(2):
                    pb = 4 + ky % 4
                    ky += 1
                    S_.op("pe", [rGT, rB], [self.psr[pb]], "matmul", self.ps[pb][:, :], lhsT=GT[:, tt, :],
                          rhs=bdn[:, half * 512:(half + 1) * 512], start=True, stop=True)
                    S_.op("act", [self.psr[pb]], [ry[tt]], "copy", out=yacc[:, tt, half * 512:(half + 1) * 512],
                          in_=self.ps[pb][:, :])
            for e in range(NEXP):
                wb = kw % 2
                kw += 1
                more = not (ts == S // TS - 1 and e == NEXP - 1)
                if more:
                    load_wg((e + 1) % NEXP, (wb + 1) % 2)
                wv = wgu[wb][:].rearrange("p c (j two) -> p c two j", two=2)
                for ch in range(2):
                    ab = (kq // 8) % 2
                    for jc in range(8):
                        q2 = kq % 2
                        pg, pl = (0, 1) if (kq % 2 == 0) else (2, 3)
                        kq += 1
                        for two, pp in ((0, pg), (1, pl)):
                            for c in range(8):
                                S_.op("pe", [rwg[wb], rh2], [self.psr[pp]], "matmul", self.ps[pp][:, :],
                                      lhsT=wv[:, c, two, jc * 128:(jc + 1) * 128], rhs=h2[:, c, ch * 512:(ch + 1) * 512],
                                      start=(c == 0), stop=(c == 7))
                        S_.op("dve", [self.psr[pg], rB], [rxg[q2]], "tensor_scalar", out=xg[q2][:], in0=self.ps[pg][:, :],
                              scalar1=bguv[:, e, jc, 0:1], scalar2=7.0, op0=ALU.add, op1=ALU.min)
                        S_.op("act", [rxg[q2]], [rsg[q2]], "activation", out=sg[q2][:], in_=xg[q2][:], func=AF.Sigmoid,
                              scale=1.702)
                        S_.op("dve", [self.psr[pl], rB], [rxl[q2]], "tensor_scalar", out=xl[q2][:], in0=self.ps[pl][:, :],
                              scalar1=bguv[:, e, jc, 1:2], scalar2=7.0, op0=ALU.add, op1=ALU.min)
                        S_.op("dve", [rxl[q2]], [rxl[q2]], "tensor_scalar", out=xl[q2][:], in0=xl[q2][:], scalar1=-7.0,
                              scalar2=1.0, op0=ALU.max, op1=ALU.add)
                        S_.op("pool", [rxg[q2], rsg[q2]], [rxg[q2]], "tensor_tensor", out=xg[q2][:], in0=xg[q2][:],
                              in1=sg[q2][:], op=ALU.mult)
                        S_.op("dve", [rxg[q2], rxl[q2]], [ract[ab]], "tensor_tensor", out=actT[ab][:, jc, :], in0=xg[q2][:],
                              in1=xl[q2][:], op=ALU.mult)
                    if pend is not None:
                        pend()
                    if ch == 0 and more:
                        load_wd((e + 1) % NEXP, (wb + 1) % 2)

                    def down(e=e, ch=ch, ab=ab, wb=wb, ts=ts):
                        nonlocal ky
                        for t4 in range(4):
                            tt = ch * 4 + t4
                            for half in range(2):
                                pb = 4 + ky % 4
                                ky += 1
                                for jc in range(8):
                                    S_.op("pe", [ract[ab], rwd[wb]], [self.psr[pb]], "matmul", self.ps[pb][:, :],
                                          lhsT=actT[ab][:, jc, t4 * 128:(t4 + 1) * 128],
                                          rhs=wdn[wb][:, jc, half * 512:(half + 1) * 512], start=(jc == 0), stop=(jc == 7))
                                ysl = yacc[:, tt, half * 512:(half + 1) * 512]
                                S_.op("dve", [self.psr[pb], self.rG], [ry[tt]], "scalar_tensor_tensor", out=ysl,
                                      in0=self.ps[pb][:, :], scalar=self.Gall[:, ts * 8 + tt, e:e + 1], in1=ysl,
                                      op0=ALU.mult, op1=ALU.add)
                    pend = down
            pend()
            pend = None
            S_.dma("sp", ry, [], self.scr["ys"][ts * TS:(ts + 1) * TS, :].rearrange("(t p) d -> p t d", p=128), yacc[:])
        S_.barrier()


def _ln2(self, xin, xout):
    S_ = self.S_
    l = self.l
    with ExitStack() as es:
        rB = Res()
        g2b = self.bload(es, "g2b", self.scr["mod"][l:l + 1, 5 * D:6 * D], rB)
        l2g = self.bload(es, "l2g", self.I["ln2_g"][l:l + 1, :], rB)
        l2b = self.bload(es, "l2b", self.I["ln2_b"][l:l + 1, :], rB)
        xt = [self.sb(es, f"fx{i}", [128, D], F32) for i in range(2)]
        yt = [self.sb(es, f"fy{i}", [128, D], F32) for i in range(2)]
        ot = [self.sb(es, f"fo{i}", [128, D], F32) for i in range(2)]
        st = [self.sb(es, f"fs{i}", [128, 16], F32) for i in range(2)]
        rx, ry, ro, rs = [Res(), Res()], [Res(), Res()], [Res(), Res()], [Res(), Res()]
        for t in range(NT):
            b = t % 2
            rows = slice(t * 128, (t + 1) * 128)
            S_.dma("sp", [], [rx[b]], xt[b][:], xin[rows, :])
            S_.dma("sp", [], [ry[b]], yt[b][:], self.scr["ys"][rows, :])
            S_.op("pool", [ry[b], rB], [ry[b]], "tensor_tensor", out=yt[b][:], in0=yt[b][:], in1=g2b[:], op=ALU.mult)
            S_.op("dve", [rx[b], ry[b]], [ry[b]], "scalar_tensor_tensor", out=yt[b][:], in0=xt[b][:], scalar=ALPHA,
                  in1=yt[b][:], op0=ALU.mult, op1=ALU.add)
            self.ln_tile(yt[b], ry[b], None, None, st[b], rs[b])
            S_.op("act", [ry[b], rs[b]], [ry[b]], "activation", out=yt[b][:], in_=yt[b][:], func=AF.Identity,
                  bias=st[b][:, 1:2], scale=st[b][:, 0:1])
            S_.op("pool", [ry[b], rB], [ry[b]], "tensor_tensor", out=yt[b][:], in0=yt[b][:], in1=l2g[:], op=ALU.mult)
            S_.op("dve", [ry[b], rB], [ro[b]], "tensor_tensor", out=ot[b][:], in0=yt[b][:], in1=l2b[:], op=ALU.add)
            S_.dma("pool", [ro[b]], [], xout[rows, :], ot[b][:])
        S_.barrier()


Prog.phase_moe = _moe
Prog.phase_ln2 = _ln2


_CACHE = {}


def kernel(**inputs):
    consts = make_consts()
    if "nc" not in _CACHE:
        _CACHE["nc"] = Prog(consts, debug=False).build()
    nc = _CACHE["nc"]
    shared = host_shared(inputs)
    in_maps = [host_inputs(inputs, consts, b, shared) for b in range(NCORES)]
    res = run_bass_kernel_spmd(nc, in_maps, core_ids=list(range(NCORES)))
    out = np.stack([np.asarray(r["out"], dtype=np.float32) for r in res.results], axis=0)
    return out


def _route(self):
    S_ = self.S_
    C0 = 40000.0
    with ExitStack() as es:
        tri = self.sb(es, "tri", [128, 128], BF16)
        ones = self.sb(es, "ones", [128, 128], BF16)
        pidx = self.sb(es, "pidx", [128, 1], F32)
        Sb = self.sb(es, "Sb", [128, NT, NEXP], BF16)
        Rk = self.sb(es, "Rk", [128, NT, NEXP], F32)
        cnt = self.sb(es, "cnt", [128, NEXP], F32)
        nb = self.sb(es, "nb", [128, NEXP], F32)
        pad = self.sb(es, "pad", [128, NEXP], F32)
        pst = self.sb(es, "pst", [128, NEXP], F32)
        a = [self.sb(es, f"sc{i}", [128, NEXP], F32) for i in range(2)]
        m8 = self.sb(es, "rm8", [128, NT, 8], F32)
        d4f = self.sb(es, "d4f", [128, NT, 4], F32)
        tmp = [self.sb(es, f"rtmp{i}", [128, NEXP], F32) for i in range(2)]
        ebf = self.sb(es, "ebf", [128, NBLK], F32)
        rc_, rSb, rRk, rcnt, rsc, rm8, rtmp, reb = Res(), Res(), Res(), Res(), Res(), Res(), [Res(), Res()], Res()
        S_.dma("sp", [], [rc_], tri[:], self.C["tri"])
        S_.dma("sp", [], [rc_], pidx[:], self.C["pidx"])
        S_.op("pool", [], [rc_], "memset", ones[:], 1.0)
        S_.op("dve", [self.rS], [rSb], "tensor_copy", out=Sb[:], in_=self.Sall[:])
        for tt in range(NT):
            pb = tt % 2
            S_.op("pe", [rSb, rc_], [self.psr[pb]], "matmul", self.ps[pb][:, 0:NEXP], lhsT=tri[:], rhs=Sb[:, tt, :],
                  start=True, stop=(tt == 0))
            for t2 in range(tt):
                S_.op("pe", [rSb, rc_], [self.psr[pb]], "matmul", self.ps[pb][:, 0:NEXP], lhsT=ones[:], rhs=Sb[:, t2, :],
                      start=False, stop=(t2 == tt - 1))
            S_.op("act", [self.psr[pb]], [rRk], "copy", out=Rk[:, tt, :], in_=self.ps[pb][:, 0:NEXP])
        for tt in range(NT):
            S_.op("pe", [rSb, rc_], [self.psr[2]], "matmul", self.ps[2][:, 0:NEXP], lhsT=ones[:], rhs=Sb[:, tt, :],
                  start=(tt == 0), stop=(tt == NT - 1))
        S_.op("act", [self.psr[2]], [rcnt], "copy", out=cnt[:], in_=self.ps[2][:, 0:NEXP])
        S_.op("dve", [rcnt], [rsc], "tensor_scalar", out=nb[:], in0=cnt[:], scalar1=0.0, scalar2=None, op0=ALU.is_gt)
        for j in range(1, S // RB):
            S_.op("dve", [rcnt, rsc], [rsc], "scalar_tensor_tensor", out=nb[:], in0=cnt[:], scalar=float(RB * j), in1=nb[:],
                  op0=ALU.is_gt, op1=ALU.add)
        S_.op("dve", [rsc], [rsc], "tensor_scalar", out=pad[:], in0=nb[:], scalar1=float(RB), scalar2=None, op0=ALU.mult)
        S_.op("dve", [rsc], [rsc], "tensor_copy", out=a[0][:], in_=pad[:])
        cur = 0
        for sft in (1, 2, 4, 8, 16):
            nx = 1 - cur
            S_.op("dve", [rsc], [rsc], "tensor_copy", out=a[nx][:, 0:sft], in_=a[cur][:, 0:sft])
            S_.op("dve", [rsc], [rsc], "tensor_tensor", out=a[nx][:, sft:NEXP], in0=a[cur][:, sft:NEXP],
                  in1=a[cur][:, 0:NEXP - sft], op=ALU.add)
            cur = nx
        pend = a[cur]
        S_.op("dve", [rsc], [rsc], "tensor_tensor", out=pst[:], in0=pend[:], in1=pad[:], op=ALU.subtract)
        for tt in range(NT):
            S_.op("dve", [rRk, rsc], [rRk], "tensor_tensor", out=Rk[:, tt, :], in0=Rk[:, tt, :], in1=pst[:], op=ALU.add)
        Rf = Rk[:].rearrange("p t e -> p (t e)")
        Sf = self.Sall[:].rearrange("p t e -> p (t e)")
        S_.op("dve", [rRk], [rRk], "tensor_scalar", out=Rf, in0=Rf, scalar1=-1.0, scalar2=C0 + 1.0, op0=ALU.mult, op1=ALU.add)
        S_.op("dve", [rRk, self.rS], [rRk], "tensor_tensor", out=Rf, in0=Rf, in1=Sf, op=ALU.mult)
        S_.op("dve", [rRk], [rRk], "tensor_scalar", out=Rf, in0=Rf, scalar1=-1.0, scalar2=None, op0=ALU.add)
        for tt in range(NT):
            S_.op("dve", [rRk], [rm8], "max", out=m8[:, tt, :], in_=Rk[:, tt, :])
        S_.op("dve", [rm8], [rm8], "tensor_scalar", out=d4f[:], in0=m8[:, :, 0:4], scalar1=-1.0, scalar2=C0, op0=ALU.mult,
              op1=ALU.add)
        S_.op("dve", [rm8], [self.rDi], "tensor_copy", out=self.Di[:], in_=d4f[:])
        kk = 0
        for tt in range(NT):
            for k in range(4):
                i = kk % 2
                kk += 1
                S_.op("dve", [rRk, rm8, self.rG], [rtmp[i]], "scalar_tensor_tensor", out=tmp[i][:], in0=Rk[:, tt, :],
                      scalar=m8[:, tt, k:k + 1], in1=self.Gall[:, tt, :], op0=ALU.is_equal, op1=ALU.mult)
                S_.op("dve", [rtmp[i]], [self.rg4], "tensor_reduce", out=self.g4[:, tt, k:k + 1], in_=tmp[i][:], axis=AX.X,
                      op=ALU.add)
        for b in range(NBLK):
            i = kk % 2
            kk += 1
            S_.op("dve", [rsc], [rtmp[i]], "tensor_scalar", out=tmp[i][:], in0=pend[:], scalar1=float(RB * b), scalar2=None,
                  op0=ALU.is_le)
            S_.op("dve", [rtmp[i]], [reb], "tensor_reduce", out=ebf[:, b:b + 1], in_=tmp[i][:], axis=AX.X, op=ALU.add)
        S_.op("dve", [reb], [reb], "tensor_scalar", out=ebf[:], in0=ebf[:], scalar1=float(NEXP - 1), scalar2=128.0,
              op0=ALU.min, op1=ALU.mult)
        S_.op("dve", [reb, rc_], [reb], "tensor_scalar", out=ebf[:], in0=ebf[:], scalar1=pidx[:, 0:1],
              scalar2=float(self.l * NEXP * 128), op0=ALU.add, op1=ALU.add)
        S_.op("dve", [reb], [self.rIw], "tensor_copy", out=self.Iw[:], in_=ebf[:])
        ht = [self.sb(es, f"ht{i}", [128, D], BF16) for i in range(3)]
        rht = [Res() for _ in range(3)]
        for tt in range(NT):
            i = tt % 3
            S_.dma("sp", [], [rht[i]], ht[i][:], self.scr["h2"][tt * 128:(tt + 1) * 128, :])
            for k in range(4):
                S_.idma([rht[i], self.rDi], [], out=self.scr["xs2"][:, :],
                        out_offset=bass.IndirectOffsetOnAxis(ap=self.Di[:, tt, k:k + 1], axis=0), in_=ht[i][:, :],
                        in_offset=None)
        S_.barrier()


def _ffn(self):
    S_ = self.S_
    l = self.l
    wtab = self.I["w_gu_l"].rearrange("l r c -> (l r) c")
    dtab = self.I["w_dn_l"].rearrange("l r c -> (l r) c")
    btab = self.I["b_gu_l"].rearrange("l r c -> (l r) c")
    with ExitStack() as es:
        wgu = [self.sb(es, f"gwgu{i}", [128, 8 * 2 * D], BF16) for i in range(2)]
        wdn = [self.sb(es, f"gwdn{i}", [128, 8 * D], BF16) for i in range(2)]
        bgb = [self.sb(es, f"bgb{i}", [128, 16], F32) for i in range(2)]
        rw = [Res(), Res()]
        rwd = [Res(), Res()]
        xtok = [self.sb(es, f"xtok{i}", [128, 4, D], BF16) for i in range(2)]
        rxt = [Res(), Res()]
        hT = [self.sb(es, f"ghT{i}", [128, 8, RB], BF16) for i in range(2)]
        rhT = [Res(), Res()]
        actT = [self.sb(es, f"gact{i}", [128, 8, RB], BF16) for i in range(2)]
        ract = [Res(), Res()]
        xg = [self.sb(es, f"gxg{i}", [128, 512], F32) for i in range(2)]
        sg = [self.sb(es, f"gsg{i}", [128, 512], F32) for i in range(2)]
        xl = [self.sb(es, f"gxl{i}", [128, 512], F32) for i in range(2)]
        rxg, rsg, rxl = [Res(), Res()], [Res(), Res()], [Res(), Res()]
        yb = [self.sb(es, f"gyb{i}", [128, D], F32) for i in range(4)]
        ryb = [Res() for _ in range(4)]

        def load_w(b):
            wb = b % 2
            ix = bass.IndirectOffsetOnAxis(ap=self.Iw[:, b:b + 1], axis=0)
            S_.idma([self.rIw], [rw[wb]], out=wgu[wb][:, :], out_offset=None, in_=wtab[:, :], in_offset=ix)
            S_.idma([self.rIw], [rw[wb]], out=bgb[wb][:, :], out_offset=None, in_=btab[:, :], in_offset=ix)

        def load_wd(b):
            wb = b % 2
            ix = bass.IndirectOffsetOnAxis(ap=self.Iw[:, b:b + 1], axis=0)
            S_.idma([self.rIw], [rwd[wb]], out=wdn[wb][:, :], out_offset=None, in_=dtab[:, :], in_offset=ix)

        def load_x(b):
            S_.dma("sp", [], [rxt[b % 2]], xtok[b % 2][:],
                   self.scr["xs2"][b * RB:(b + 1) * RB, :].rearrange("(t p) d -> p t d", p=128))
        kq = 0
        ky = 0
        pend = None
        fin = None
        load_w(0)
        load_wd(0)
        load_x(0)
        for b in range(NBLK):
            wb = b % 2
            if b + 1 < NBLK:
                load_w(b + 1)
                load_x(b + 1)
            for t4 in range(4):
                pT = 6 + (t4 % 2)
                pst = self.ps[pT][:].bitcast(BF16)
                for c in range(8):
                    S_.op("pe", [rxt[wb], self.rc], [self.psr[pT]], "transpose", out=pst[:, c * 128:(c + 1) * 128],
                          in_=xtok[wb][:, t4, c * 128:(c + 1) * 128], identity=self.identb[:])
                S_.op("act", [self.psr[pT]], [rhT[wb]], "copy", out=hT[wb][:, :, t4 * 128:(t4 + 1) * 128],
                      in_=pst.rearrange("p (c n) -> p c n", c=8))
            wv = wgu[wb][:].rearrange("p (c j two) -> p c two j", c=8, two=2)
            for jc in range(8):
                q2 = kq % 2
                pg, pl = (0, 1) if (kq % 2 == 0) else (2, 3)
                kq += 1
                for two, pp in ((0, pg), (1, pl)):
                    for c in range(8):
                        S_.op("pe", [rw[wb], rhT[wb]], [self.psr[pp]], "matmul", self.ps[pp][:, :],
                              lhsT=wv[:, c, two, jc * 128:(jc + 1) * 128], rhs=hT[wb][:, c, :], start=(c == 0), stop=(c == 7))
                S_.op("dve", [self.psr[pg], rw[wb]], [rxg[q2]], "tensor_scalar", out=xg[q2][:], in0=self.ps[pg][:, :],
                      scalar1=bgb[wb][:, 2 * jc:2 * jc + 1], scalar2=7.0, op0=ALU.add, op1=ALU.min)
                S_.op("act", [rxg[q2]], [rsg[q2]], "activation", out=sg[q2][:], in_=xg[q2][:], func=AF.Sigmoid, scale=1.702)
                S_.op("dve", [self.psr[pl], rw[wb]], [rxl[q2]], "tensor_scalar", out=xl[q2][:], in0=self.ps[pl][:, :],
                      scalar1=bgb[wb][:, 2 * jc + 1:2 * jc + 2], scalar2=7.0, op0=ALU.add, op1=ALU.min)
                S_.op("dve", [rxl[q2]], [rxl[q2]], "tensor_scalar", out=xl[q2][:], in0=xl[q2][:], scalar1=-7.0, scalar2=1.0,
                      op0=ALU.max, op1=ALU.add)
                if fin is not None:
                    fin()

                def fin(q2=q2, jc=jc, wb=wb):
                    S_.op("dve", [rxg[q2], rsg[q2]], [rxg[q2]], "tensor_tensor", out=xg[q2][:], in0=xg[q2][:], in1=sg[q2][:],
                          op=ALU.mult)
                    S_.op("dve", [rxg[q2], rxl[q2]], [ract[wb]], "tensor_tensor", out=actT[wb][:, jc, :], in0=xg[q2][:],
                          in1=xl[q2][:], op=ALU.mult)
            fin()
            fin = None
            if pend is not None:
                pend()
            if b + 1 < NBLK:
                load_wd(b + 1)

            def down(b=b, wb=wb):
                nonlocal ky
                wd = wdn[wb][:].rearrange("p (c n) -> p c n", c=8)
                for t4 in range(4):
                    yi = ky % 4
                    ky += 1
                    for half in range(2):
                        pb = 4 + half
                        for jc in range(8):
                            S_.op("pe", [ract[wb], rwd[wb]], [self.psr[pb]], "matmul", self.ps[pb][:, :],
                                  lhsT=actT[wb][:, jc, t4 * 128:(t4 + 1) * 128], rhs=wd[:, jc, half * 512:(half + 1) * 512],
                                  start=(jc == 0), stop=(jc == 7))
                        S_.op("act", [self.psr[pb]], [ryb[yi]], "copy", out=yb[yi][:, half * 512:(half + 1) * 512],
                              in_=self.ps[pb][:, :])
                    r0 = b * RB + t4 * 128
                    S_.dma("sp", [ryb[yi]], [], self.scr["ys2"][r0:r0 + 128, :], yb[yi][:])
            pend = down
        pend()
        S_.barrier()


def _ln2g(self, xin, xout):
    S_ = self.S_
    l = self.l
    with ExitStack() as es:
        rB = Res()
        g2b = self.bload(es, "g2b", self.scr["mod"][l:l + 1, 5 * D:6 * D], rB)
        l2g = self.bload(es, "l2g", self.I["ln2_g"][l:l + 1, :], rB)
        l2b = self.bload(es, "l2b", self.I["ln2_b"][l:l + 1, :], rB)
        bdn = self.sb(es, "bdn", [NEXP, D], F32)
        S_.dma("sp", [], [rB], bdn[:], self.I["b_down"][l])
        GT = [self.sb(es, f"cGT{i}", [NEXP, 128], F32) for i in range(2)]
        xt = [self.sb(es, f"fx{i}", [128, D], F32) for i in range(2)]
        yt = [self.sb(es, f"fy{i}", [128, D], F32) for i in range(2)]
        ot = [self.sb(es, f"fo{i}", [128, D], F32) for i in range(2)]
        yg = [[self.sb(es, f"yg{i}{k}", [128, D], F32) for k in range(4)] for i in range(2)]
        st = [self.sb(es, f"fs{i}", [128, 16], F32) for i in range(2)]
        rx, ry, ro, rs, rGT = [Res(), Res()], [Res(), Res()], [Res(), Res()], [Res(), Res()], [Res(), Res()]
        ryg = [[Res() for _ in range(4)] for _ in range(2)]
        def tile_body(t):
            b = t % 2
            rows = slice(t * 128, (t + 1) * 128)
            S_.dma("sp", [], [rx[b]], xt[b][:], xin[rows, :])
            for k in range(4):
                S_.idma([self.rDi], [ryg[b][k]], out=yg[b][k][:, :], out_offset=None, in_=self.scr["ys2"][:, :],
                        in_offset=bass.IndirectOffsetOnAxis(ap=self.Di[:, t, k:k + 1], axis=0))
            pT = 6 + b
            S_.op("pe", [self.rG, self.rc], [self.psr[pT]], "transpose", out=self.ps[pT][0:NEXP, 0:128],
                  in_=self.Gall[:, t, :], identity=self.identf[:])
            S_.op("act", [self.psr[pT]], [rGT[b]], "copy", out=GT[b][:], in_=self.ps[pT][0:NEXP, 0:128])
            for half in range(2):
                pb = 2 * b + half
                hs_ = slice(half * 512, (half + 1) * 512)
                S_.op("pe", [rGT[b], rB], [self.psr[pb]], "matmul", self.ps[pb][:, :], lhsT=GT[b][:], rhs=bdn[:, hs_],
                      start=True, stop=True)
                S_.op("dve", [self.psr[pb], ryg[b][0], self.rg4], [ry[b]], "scalar_tensor_tensor", out=yt[b][:, hs_],
                      in0=yg[b][0][:, hs_], scalar=self.g4[:, t, 0:1], in1=self.ps[pb][:, :], op0=ALU.mult, op1=ALU.add)
            yield
            for k in range(1, 4):
                S_.op("dve", [ry[b], ryg[b][k], self.rg4], [ry[b]], "scalar_tensor_tensor", out=yt[b][:], in0=yg[b][k][:],
                      scalar=self.g4[:, t, k:k + 1], in1=yt[b][:], op0=ALU.mult, op1=ALU.add)
            S_.op("pool", [ry[b], rB], [ry[b]], "tensor_tensor", out=yt[b][:], in0=yt[b][:], in1=g2b[:], op=ALU.mult)
            yield
            S_.op("dve", [rx[b], ry[b]], [ry[b]], "scalar_tensor_tensor", out=yt[b][:], in0=xt[b][:], scalar=ALPHA,
                  in1=yt[b][:], op0=ALU.mult, op1=ALU.add)
            self.ln_tile(yt[b], ry[b], None, None, st[b], rs[b], split=True)
            yield
            self.ln_tile2(st[b], rs[b])
            S_.op("act", [ry[b], rs[b]], [ry[b]], "activation", out=yt[b][:], in_=yt[b][:], func=AF.Identity,
                  bias=st[b][:, 1:2], scale=st[b][:, 0:1])
            S_.op("pool", [ry[b], rB], [ry[b]], "tensor_tensor", out=yt[b][:], in0=yt[b][:], in1=l2g[:], op=ALU.mult)
            yield
            S_.op("dve", [ry[b], rB], [ro[b]], "tensor_tensor", out=ot[b][:], in0=yt[b][:], in1=l2b[:], op=ALU.add)
            S_.dma("sp", [ro[b]], [], xout[rows, :], ot[b][:])
        round_robin((tile_body(t) for t in range(NT)), 2)
        S_.barrier()


Prog.phase_route = _route
Prog.phase_ffn = _ffn
Prog.phase_ln2 = _ln2g
```

```python
import numpy as np
import ml_dtypes
from contextlib import ExitStack
import concourse.bass as bass
import concourse.mybir as mybir
from concourse.bass_utils import run_bass_kernel_spmd

F32 = mybir.dt.float32
BF16 = mybir.dt.bfloat16
I32 = mybir.dt.int32
AF = mybir.ActivationFunctionType
ALU = mybir.AluOpType
AX = mybir.AxisListType

D = 1024
S = 4096
NT = S // 128
DEPTH = 2
NCORES = 8
LN_EPS = 1e-5
ALPHA = (2 * DEPTH) ** 0.25
NEG = -30000.0
BIG = 1.0e30
SCALE = 64 ** -0.5
NEXP = 32
RB = 512
NBLK = 64
C_AQ, C_AK, C_AV, C_BQ, C_BKC, C_BVC, C_BKS, C_BVS, C_BKW, C_BVW, C_BG, C_CQ, C_CK, C_CV, C_MG = (
    0, 256, 512, 768, 1152, 1280, 1408, 1536, 1664, 1792, 1920, 1938, 2322, 2706, 3090)
IN_W = 6162
FB_AQ, FB_AK, FB_BQ, FB_KC, FB_KS, FB_KW, FB_CQ, FB_CK, FB_VC = 0, 2, 4, 7, 8, 9, 10, 13, 16
NFB = 17


class Res:
    __slots__ = ("lw", "rd")

    def __init__(self):
        self.lw = None
        self.rd = {}


class Sched:
    def __init__(self, nc, es):
        self.nc = nc
        self.eng = {"pe": nc.tensor, "act": nc.scalar, "dve": nc.vector, "pool": nc.gpsimd, "sp": nc.sync}
        self.sem = {k: es.enter_context(nc.semaphore("prog_" + k)) for k in self.eng}
        self.cnt = {k: 0 for k in self.eng}
        self.seen = {k: {} for k in self.eng}
        self.own = {self.sem[k]: k for k in self.eng}
        self.dq = {}
        for q, n in (("sp", 20), ("pool", 12), ("act", 4)):
            self.dq[q] = {"sems": [es.enter_context(nc.semaphore(f"dma_{q}_{i}")) for i in range(n)],
                          "val": [0] * n, "i": 0}
        self.n_ins = 0

    def _wait(self, e, tok, raw):
        sem, val = tok
        if self.seen[e].get(sem, 0) >= val:
            return
        o = self.own.get(sem)
        if o == e:
            if e == "pe" or not raw or self.cnt[e] - val >= 2:
                return
        self.eng[e].wait_ge(sem, val)
        self.seen[e][sem] = val
        self.n_ins += 1

    def _deps(self, e, R, W):
        for r in R:
            if r.lw is not None:
                self._wait(e, r.lw, True)
        for w in W:
            if w.lw is not None:
                self._wait(e, w.lw, False)
            for sem, val in w.rd.items():
                self._wait(e, (sem, val), False)

    def _commit(self, tok, R, W):
        sem, val = tok
        for r in R:
            if r.rd.get(sem, 0) < val:
                r.rd[sem] = val
        for w in W:
            w.lw = tok
            w.rd = {}

    def op(self, e, R, W, name, *a, **k):
        self._deps(e, R, W)
        ins = getattr(self.eng[e], name)(*a, **k)
        self.cnt[e] += 1
        ins.then_inc(self.sem[e], 1)
        self._commit((self.sem[e], self.cnt[e]), R, W)
        self.n_ins += 1
        return ins

    def dma(self, q, R, W, out, in_, **k):
        d = self.dq[q]
        i = d["i"] % len(d["sems"])
        d["i"] += 1
        sem = d["sems"][i]
        if d["val"][i] > 0:
            self._wait(q, (sem, d["val"][i]), False)
        self._deps(q, R, W)
        ins = self.eng[q].dma_start(out=out, in_=in_, **k)
        ins.then_inc(sem, 16)
        d["val"][i] += 16
        self._commit((sem, d["val"][i]), R, W)
        self.n_ins += 1

    def idma(self, R, W, **k):
        q = "pool"
        d = self.dq[q]
        i = d["i"] % len(d["sems"])
        d["i"] += 1
        sem = d["sems"][i]
        if d["val"][i] > 0:
            self._wait(q, (sem, d["val"][i]), False)
        self._deps(q, R, W)
        ins = self.eng[q].indirect_dma_start(**k)
        ins.then_inc(sem, 16)
        d["val"][i] += 16
        self._commit((sem, d["val"][i]), R, W)
        self.n_ins += 1

    def barrier(self):
        toks = [(self.sem[f], self.cnt[f]) for f in self.eng if self.cnt[f] > 0]
        for q in self.dq.values():
            for s_, v in zip(q["sems"], q["val"]):
                if v > 0:
                    toks.append((s_, v))
        for e in self.eng:
            for (sem, val) in toks:
                if self.own.get(sem) == e:
                    continue
                if self.seen[e].get(sem, 0) >= val:
                    continue
                self.eng[e].wait_ge(sem, val)
                self.seen[e][sem] = val
                self.n_ins += 1


def round_robin(gens, width):
    it = iter(gens)
    active = []
    more = True
    while True:
        while more and len(active) < width:
            try:
                active.append(next(it))
            except StopIteration:
                more = False
        if not active:
            break
        for g in list(active):
            try:
                next(g)
            except StopIteration:
                active.remove(g)


def bf16_np(a):
    return np.asarray(a, np.float32).astype(ml_dtypes.bfloat16)


def make_consts():
    c = {}
    c["identb"] = bf16_np(np.eye(128))
    c["identf"] = np.eye(128, dtype=np.float32)
    k = np.arange(128)[:, None]
    q = np.arange(128)[None, :]
    masks = np.zeros((128, 3, 128), np.float32)
    masks[:, 0] = np.where(k <= q, 0.0, NEG)
    masks[:, 1] = np.where(k > q, 0.0, NEG)
    masks[:, 2] = np.where(k >= q, 0.0, NEG)
    c["masks"] = bf16_np(masks)
    e16 = np.zeros((16, 16, 128), np.float32)
    for n in range(16):
        e16[n, n, :] = 1.0
    c["esel16"] = bf16_np(e16)
    e64 = np.zeros((64, 32, 128), np.float32)
    for kt in range(32):
        e64[2 * kt, kt, :64] = 1.0
        e64[2 * kt + 1, kt, 64:] = 1.0
    c["esel64"] = bf16_np(e64)
    inv = (10000.0 ** (-np.arange(0, 64, 2, dtype=np.float32) / 64)).astype(np.float32)
    rp = np.zeros((128, 2), np.float32)
    for f in range(128):
        rp[f, 0] = inv[f % 32]
        rp[f, 1] = -1.0 if (f % 64) < 32 else 1.0
    c["ropec"] = rp
    n = (np.arange(2)[None, :, None] * 128 + np.arange(128)[:, None, None])
    t = np.arange(S)[None, None, :]
    c["cmpmask"] = bf16_np(np.where(16 * n + 31 <= t, 0.0, NEG))
    ov = np.zeros((128, 2, 64), np.float32)
    for nt in range(2):
        for kk in range(128):
            nn = nt * 128 + kk
            if nn >= 255:
                continue
            for m in range(64):
                if 16 * nn < 64 * m + 64 and 16 * nn + 32 > 64 * m:
                    ov[kk, nt, m] = 1.0
    c["overlap"] = bf16_np(ov)
    add = np.zeros((128, 32, 64), np.float32)
    mn = np.zeros((128, 32, 64), np.float32)
    for qt in range(32):
        for p in range(128):
            qb = 2 * qt + (1 if p >= 64 else 0)
            for m in range(64):
                forced = (m == 0) or (m == qb) or (m == qb - 1)
                add[p, qt, m] = 1.0e6 if forced else 0.0
                mn[p, qt, m] = BIG if m <= qb else -BIG
    c["nsa_add"] = add
    c["nsa_min"] = mn
    c["tri"] = bf16_np((np.arange(128)[:, None] < np.arange(128)[None, :]).astype(np.float32))
    c["pidx"] = np.arange(128, dtype=np.float32).reshape(128, 1)
    return c


WNAMES = ["w_ada", "b_ada", "w_in", "cmp_w1_k", "cmp_w2_k", "cmp_peT_k", "cmp_w1_v", "cmp_w2_v", "cmp_peT_v",
          "w_branch_a", "w_branch_b", "w_branch_c", "w_out", "ln1_g", "ln1_b", "router_w", "router_b",
          "w_gu_l", "b_gu_l", "w_dn_l", "b_down", "ln2_g", "ln2_b"]
WSHAPES = {
    "w_ada": [DEPTH, D, 6 * D], "b_ada": [DEPTH, 6 * D], "w_in": [DEPTH, D, IN_W],
    "cmp_w1_k": [DEPTH, 2048, 128], "cmp_w2_k": [DEPTH, 128, 64], "cmp_peT_k": [DEPTH, 64, 32],
    "cmp_w1_v": [DEPTH, 2048, 128], "cmp_w2_v": [DEPTH, 128, 64], "cmp_peT_v": [DEPTH, 64, 32],
    "w_branch_a": [DEPTH, 256, D], "w_branch_b": [DEPTH, 384, D], "w_branch_c": [DEPTH, 128, D],
    "w_out": [DEPTH, D, D], "ln1_g": [DEPTH, D], "ln1_b": [DEPTH, D], "router_w": [DEPTH, D, NEXP],
    "router_b": [DEPTH, NEXP], "w_gu_l": [DEPTH, NEXP * 128, 16 * D], "b_gu_l": [DEPTH, NEXP * 128, 16],
    "w_dn_l": [DEPTH, NEXP * 128, 8 * D], "b_down": [DEPTH, NEXP, D], "ln2_g": [DEPTH, D], "ln2_b": [DEPTH, D],
}
CONST_DT = {"identb": BF16, "identf": F32, "masks": BF16, "esel16": BF16, "esel64": BF16, "ropec": F32,
            "cmpmask": BF16, "overlap": BF16, "nsa_add": F32, "nsa_min": F32, "tri": BF16, "pidx": F32}


class Prog:
    def __init__(self, consts, debug=False, stop_after=None, nlayers=DEPTH):
        self.debug = debug
        self.stop_after = stop_after
        self.nlayers = nlayers
        nc = self.nc = bass.Bass("TRN2", target_bir_lowering=False)
        self.es = ExitStack()
        self.I = {}
        self.I["x"] = nc.dram_tensor("x", [S, D], F32, kind="ExternalInput").ap()
        self.I["cT"] = nc.dram_tensor("cT", [128, 8], F32, kind="ExternalInput").ap()
        self.I["pos"] = nc.dram_tensor("pos", [1, S], I32, kind="ExternalInput").ap()
        for n in WNAMES:
            self.I[n] = nc.dram_tensor(n, WSHAPES[n], F32, kind="ExternalInput").ap()
        self.C = {}
        for n, a in consts.items():
            self.C[n] = nc.dram_tensor("k_" + n, list(a.shape), CONST_DT[n], kind="ExternalInput").ap()
        self.out = nc.dram_tensor("out", [S, D], F32, kind="ExternalOutput").ap()
        sk = "ExternalOutput" if debug else "Internal"
        self.scr = {}

        def scr(name, shape, dt):
            self.scr[name] = nc.dram_tensor("s_" + name, shape, dt, kind=sk).ap()
        scr("rope", [2, 128, S], F32)
        scr("mod", [DEPTH, 6 * D], F32)
        scr("ft", [NFB, 128, S], BF16)
        scr("tv", [S, 896], BF16)
        scr("mg", [S, 3072], BF16)
        scr("o", [S, 768], BF16)
        scr("oc", [3, S, 130], F32)
        scr("xs", [2, S, D], F32)
        scr("h2t", [128, 8, S], BF16)
        scr("h2", [S, D], BF16)
        scr("xs2", [NBLK * RB, D], BF16)
        scr("ys2", [NBLK * RB, D], F32)
        if debug:
            scr("dbg1", [128, 256], BF16)
            scr("dbg2", [128, 2 * 2 * 129], BF16)

    def sb(self, es, name, shape, dt):
        self._uid = getattr(self, "_uid", 0) + 1
        return es.enter_context(self.nc.sbuf_tensor(f"{name}_{self._uid}", shape, dt))

    def build(self):
        nc = self.nc
        with self.es as es:
            self.S_ = S_ = Sched(nc, es)
            self.ps = [es.enter_context(nc.psum_tensor(f"ps{i}", [128, 512], F32)) for i in range(8)]
            self.psr = [Res() for _ in range(8)]
            self.identb = self.sb(es, "identb", [128, 128], BF16)
            self.identf = self.sb(es, "identf", [128, 128], F32)
            self.masks = self.sb(es, "masks", [128, 3, 128], BF16)
            self.Gall = self.sb(es, "Gall", [128, NT, NEXP], F32)
            self.BGs = self.sb(es, "BGs", [128, NT, 18], F32)
            self.rc = Res()
            self.epsc = self.sb(es, "epsc", [128, 1], F32)
            self.zeros = self.sb(es, "zeros", [128, 512], BF16)
            S_.op("dve", [], [self.rc], "memset", self.zeros[:], 0.0)
            S_.op("dve", [], [self.rc], "memset", self.epsc[:], LN_EPS)
            for n_, t_ in (("identb", self.identb), ("identf", self.identf), ("masks", self.masks)):
                S_.dma("sp", [], [self.rc], t_[:], self.C[n_])
            self.rG = Res()
            self.rBG = Res()
            self.Sall = self.sb(es, "Sall", [128, NT, NEXP], F32)
            self.Di = self.sb(es, "Di", [128, NT, 4], I32)
            self.g4 = self.sb(es, "g4", [128, NT, 4], F32)
            self.Iw = self.sb(es, "Iw", [128, NBLK], I32)
            self.rS, self.rDi, self.rg4, self.rIw = Res(), Res(), Res(), Res()
            self.phase_rope()
            S_.barrier()
            xin = self.I["x"]
            for l in range(self.nlayers):
                self.l = l
                for pn in ("phase_ada", "phase_ln1", "phase_proj", "phase_moba", "phase_nsa",
                           "phase_dil", "phase_merge", "phase_route", "phase_ffn", "phase_ln2"):
                    ph = getattr(self, pn)
                    if pn == "phase_ln1":
                        ph(xin)
                    elif pn == "phase_merge":
                        ph(xin, self.scr["xs"][0])
                    elif pn == "phase_ln2":
                        dst = self.out if l == self.nlayers - 1 else self.scr["xs"][1]
                        ph(self.scr["xs"][0], dst)
                    else:
                        ph()
                    S_.barrier()
                    if self.stop_after == (l, pn):
                        return nc
                xin = self.scr["xs"][1]
            S_.barrier()
        return nc

    def phase_rope(self):
        import math
        S_ = self.S_
        with ExitStack() as es:
            posb = self.sb(es, "posb", [128, S], I32)
            ang = self.sb(es, "ang", [128, S], F32)
            arg = self.sb(es, "arg", [128, S], F32)
            res_ = self.sb(es, "rres", [128, S], F32)
            rpc = self.sb(es, "rpc", [128, 2], F32)
            npi = self.sb(es, "npi", [128, 1], F32)
            r1, r2, r3, r4, r5 = Res(), Res(), Res(), Res(), Res()
            S_.dma("sp", [], [r1], posb[:], self.I["pos"].partition_broadcast(128))
            S_.dma("sp", [], [r5], rpc[:], self.C["ropec"])
            S_.op("dve", [], [r5], "memset", npi[:], -math.pi)
            S_.op("dve", [r1], [r2], "tensor_copy", out=ang[:], in_=posb[:])
            S_.op("dve", [r2, r5], [r2], "tensor_scalar", out=ang[:], in0=ang[:], scalar1=rpc[:, 0:1], scalar2=None,
                  op0=ALU.mult)
            ki = posb
            for i, off in enumerate((0.5 * math.pi, 0.0)):
                S_.op("dve", [r2], [r3], "tensor_scalar", out=arg[:], in0=ang[:], scalar1=off, scalar2=None,
                      op0=ALU.add)
                S_.op("dve", [r3], [r4], "tensor_scalar", out=res_[:], in0=arg[:], scalar1=1.0 / (2 * math.pi),
                      scalar2=None, op0=ALU.mult)
                S_.op("dve", [r4], [r1], "tensor_copy", out=ki[:], in_=res_[:])
                S_.op("dve", [r1], [r4], "tensor_copy", out=res_[:], in_=ki[:])
                S_.op("dve", [r4, r3], [r3], "scalar_tensor_tensor", out=arg[:], in0=res_[:], scalar=-2 * math.pi,
                      in1=arg[:], op0=ALU.mult, op1=ALU.add)
                S_.op("dve", [r3], [r4], "tensor_scalar", out=res_[:], in0=arg[:], scalar1=math.pi,
                      scalar2=-2 * math.pi, op0=ALU.is_gt, op1=ALU.mult)
                S_.op("dve", [r3, r4], [r3], "tensor_tensor", out=arg[:], in0=arg[:], in1=res_[:], op=ALU.add)
                S_.op("dve", [r3], [r3], "tensor_scalar", out=arg[:], in0=arg[:], scalar1=-math.pi, scalar2=math.pi,
                      op0=ALU.max, op1=ALU.min)
                S_.op("act", [r3], [r4], "activation", out=res_[:], in_=arg[:], func=AF.Sin)
                if i == 1:
                    S_.op("dve", [r4, r5], [r4], "tensor_scalar", out=res_[:], in0=res_[:], scalar1=rpc[:, 1:2],
                          scalar2=None, op0=ALU.mult)
                S_.dma("pool", [r4], [], self.scr["rope"][i], res_[:])
            S_.barrier()

    def phase_ada(self):
        S_ = self.S_
        l = self.l
        with ExitStack() as es:
            cs = self.sb(es, "cs", [128, 8], F32)
            brow = self.sb(es, "brow", [1, 6 * D], F32)
            mrow = self.sb(es, "mrow", [1, 6 * D], F32)
            wa = [self.sb(es, f"wa{i}", [128, 8, 512], F32) for i in range(2)]
            rcs, rb, rm = Res(), Res(), Res()
            rwa = [Res(), Res()]
            S_.dma("sp", [], [rcs], cs[:], self.I["cT"])
            S_.dma("sp", [], [rb], brow[:], self.I["b_ada"][l:l + 1, :])
            S_.op("act", [rcs], [rcs], "activation", out=cs[:], in_=cs[:], func=AF.Silu)
            for nb in range(12):
                w = wa[nb % 2]
                S_.dma("sp", [], [rwa[nb % 2]], w[:],
                       self.I["w_ada"][l][:, nb * 512:(nb + 1) * 512].rearrange("(c p) n -> p c n", p=128))
                pr = self.psr[nb % 2]
                for c in range(8):
                    S_.op("pe", [rcs, rwa[nb % 2]], [pr], "matmul", self.ps[nb % 2][0:1, :], lhsT=cs[:, c:c + 1],
                          rhs=w[:, c, :], start=(c == 0), stop=(c == 7))
                S_.op("dve", [pr, rb], [rm], "tensor_tensor", out=mrow[:, nb * 512:(nb + 1) * 512],
                      in0=self.ps[nb % 2][0:1, :], in1=brow[:, nb * 512:(nb + 1) * 512], op=ALU.add)
            for seg in (1, 4):
                S_.op("dve", [rm], [rm], "tensor_scalar", out=mrow[:, seg * D:(seg + 1) * D],
                      in0=mrow[:, seg * D:(seg + 1) * D], scalar1=1.0, scalar2=None, op0=ALU.add)
            S_.dma("pool", [rm], [], self.scr["mod"][l:l + 1, :], mrow[:])

    def bload(self, es, name, src_row, r):
        t = self.sb(es, name, [128, D], F32)
        self.S_.dma("sp", [], [r], t[:], src_row.partition_broadcast(128))
        return t

    def ln_tile(self, xt, rx, tmp, rtmp, stat, rstat, split=False):
        S_ = self.S_
        st6 = stat[:, 4:16].rearrange("p (a b) -> p a b", a=2)
        for a in range(2):
            S_.op("dve", [rx], [rstat], "bn_stats", out=st6[:, a, :], in_=xt[:, a * 512:(a + 1) * 512])
        S_.op("dve", [rstat], [rstat], "bn_aggr", out=stat[:, 2:4], in_=st6)
        S_.op("act", [rstat, self.rc], [rstat], "activation", out=stat[:, 0:1], in_=stat[:, 3:4], func=AF.Sqrt,
              bias=self.epsc[:, 0:1], scale=1.0)
        if split:
            return
        self.ln_tile2(stat, rstat)

    def ln_tile2(self, stat, rstat):
        S_ = self.S_
        S_.op("dve", [rstat], [rstat], "reciprocal", out=stat[:, 0:1], in_=stat[:, 0:1])
        S_.op("dve", [rstat], [rstat], "scalar_tensor_tensor", out=stat[:, 1:2], in0=stat[:, 2:3], scalar=-1.0,
              in1=stat[:, 0:1], op0=ALU.mult, op1=ALU.mult)

    def phase_ln1(self, xin):
        S_ = self.S_
        l = self.l
        es = self.es_h = ExitStack()
        self.hT = self.sb(es, "hT", [128, 8, S], BF16)
        self.rhT = [Res() for _ in range(NT)]
        with ExitStack() as e2:
            rmod = Res()
            scp = self.bload(e2, "scp", self.scr["mod"][l:l + 1, D:2 * D], rmod)
            shf = self.bload(e2, "shf", self.scr["mod"][l:l + 1, 0:D], rmod)
            xt = [self.sb(e2, f"xt{i}", [128, D], F32) for i in range(2)]
            xn = [self.sb(e2, f"xn{i}", [128, D], F32) for i in range(2)]
            hb = [self.sb(e2, f"hb{i}", [128, D], BF16) for i in range(2)]
            stt = [self.sb(e2, f"stt{i}", [128, 16], F32) for i in range(2)]
            rx, rxn, rhb, rst = [Res(), Res()], [Res(), Res()], [Res(), Res()], [Res(), Res()]
            for t in range(NT):
                b = t % 2
                S_.dma("sp", [], [rx[b]], xt[b][:], xin[t * 128:(t + 1) * 128, :])
                self.ln_tile(xt[b], rx[b], None, None, stt[b], rst[b])
                S_.op("act", [rx[b], rst[b]], [rxn[b]], "activation", out=xn[b][:], in_=xt[b][:], func=AF.Identity,
                      bias=stt[b][:, 1:2], scale=stt[b][:, 0:1])
                S_.op("pool", [rxn[b], rmod], [rxn[b]], "tensor_tensor", out=xn[b][:], in0=xn[b][:], in1=scp[:],
                      op=ALU.mult)
                S_.op("dve", [rxn[b], rmod], [rhb[b]], "tensor_tensor", out=hb[b][:], in0=xn[b][:], in1=shf[:],
                      op=ALU.add)
                pb = 6 + b
                pst = self.ps[pb][:].bitcast(BF16)
                for c in range(8):
                    S_.op("pe", [rhb[b], self.rc], [self.psr[pb]], "transpose", out=pst[:, c * 128:(c + 1) * 128],
                          in_=hb[b][:, c * 128:(c + 1) * 128], identity=self.identb[:])
                S_.op("act", [self.psr[pb]], [self.rhT[t]], "tensor_copy" if False else "copy",
                      out=self.hT[:, :, t * 128:(t + 1) * 128],
                      in_=pst.rearrange("p (c n) -> p c n", c=8))

    def phase_proj(self):
        S_ = self.S_
        l = self.l
        win = self.I["w_in"][l]

        def wsrc(c0, n):
            return win[:, c0:c0 + n].rearrange("(c p) n -> p c n", p=128)
        with ExitStack() as es:
            Wb = self.sb(es, "Wb", [128, 8, 3090], BF16)
            Wr = self.sb(es, "Wr", [128, 8, 2048], BF16)
            rW = Res()
            segs = [(0, C_AQ, 512), (896, C_BKC, 128), (1024, C_BKS, 128), (1152, C_BKW, 128), (1280, C_CQ, 768),
                    (2048, C_BVC, 128), (2176, C_AV, 256), (2432, C_BVS, 128), (2560, C_BVW, 128),
                    (2688, C_CV, 384), (3072, C_BG, 18)]
            for r in range(3):
                for g in range(2):
                    segs.append((512 + r * 128 + g * 64, C_BQ + (3 * g + r) * 64, 64))
            for (o, c0, n) in segs:
                S_.dma("pool", [], [rW], Wb[:, :, o:o + n], wsrc(c0, n))
            rWr = Res()
            for c in range(8):
                src = Wb[:, c, 0:2048].rearrange("p (h two d) -> p h two d", two=2, d=32)
                dst = Wr[:, c, :].rearrange("p (h two d) -> p h two d", two=2, d=32)
                S_.op("pool", [rW], [rWr], "tensor_copy", out=dst[:, :, 0, :], in_=src[:, :, 1, :])
                S_.op("pool", [rW], [rWr], "tensor_copy", out=dst[:, :, 1, :], in_=src[:, :, 0, :])
            cs = [self.sb(es, f"cosc{i}", [128, 2, 512], F32) for i in range(2)]
            rcs = [Res(), Res()]
            t1 = [self.sb(es, f"t1_{i}", [128, 512], F32) for i in range(2)]
            t2 = [self.sb(es, f"t2_{i}", [128, 512], F32) for i in range(2)]
            ob = [self.sb(es, f"ob{i}", [128, 512], BF16) for i in range(4)]
            rt1, rt2, rob = [Res(), Res()], [Res(), Res()], [Res() for _ in range(4)]
            k = 0
            for tc in range(8):
                cb = tc % 2
                S_.dma("sp", [], [rcs[cb]], cs[cb][:],
                       self.scr["rope"][:, :, tc * 512:(tc + 1) * 512].rearrange("a p n -> p a n"))
                rh = self.rhT[tc * 4:(tc + 1) * 4]
                for fb in range(NFB):
                    pa, pb = (0, 1) if k % 2 == 0 else (2, 3)
                    for c in range(8):
                        S_.op("pe", [rW] + rh, [self.psr[pa]], "matmul", self.ps[pa][:, :],
                              lhsT=Wb[:, c, fb * 128:(fb + 1) * 128], rhs=self.hT[:, c, tc * 512:(tc + 1) * 512],
                              start=(c == 0), stop=(c == 7))
                    o_ = k % 4
                    if fb < 16:
                        for c in range(8):
                            S_.op("pe", [rWr] + rh, [self.psr[pb]], "matmul", self.ps[pb][:, :],
                                  lhsT=Wr[:, c, fb * 128:(fb + 1) * 128], rhs=self.hT[:, c, tc * 512:(tc + 1) * 512],
                                  start=(c == 0), stop=(c == 7))
                        b2 = k % 2
                        S_.op("dve", [self.psr[pa], rcs[cb]], [rt1[b2]], "tensor_tensor", out=t1[b2][:],
                              in0=self.ps[pa][:, :], in1=cs[cb][:, 0, :], op=ALU.mult)
                        S_.op("dve", [self.psr[pb], rcs[cb]], [rt2[b2]], "tensor_tensor", out=t2[b2][:],
                              in0=self.ps[pb][:, :], in1=cs[cb][:, 1, :], op=ALU.mult)
                        S_.op("pool", [rt1[b2], rt2[b2]], [rob[o_]], "tensor_tensor", out=ob[o_][:], in0=t1[b2][:],
                              in1=t2[b2][:], op=ALU.add)
                    else:
                        S_.op("act", [self.psr[pa]], [rob[o_]], "copy", out=ob[o_][:], in_=self.ps[pa][:, :])
                    S_.dma("pool", [rob[o_]], [], self.scr["ft"][fb][:, tc * 512:(tc + 1) * 512], ob[o_][:])
                    k += 1
            tvb = [self.sb(es, f"tvb{i}", [128, 896], BF16) for i in range(2)]
            rtv = [Res(), Res()]
            for t in range(NT):
                b = t % 2
                pa, pb = (4, 5) if b == 0 else (6, 7)
                for (pp, c0, n) in ((pa, 2176, 512), (pb, 2688, 402)):
                    for c in range(8):
                        S_.op("pe", [rW, self.rhT[t]], [self.psr[pp]], "matmul", self.ps[pp][:, 0:n],
                              lhsT=self.hT[:, c, t * 128:(t + 1) * 128], rhs=Wb[:, c, c0:c0 + n],
                              start=(c == 0), stop=(c == 7))
                S_.op("act", [self.psr[pa]], [rtv[b]], "copy", out=tvb[b][:, 0:512], in_=self.ps[pa][:, :])
                S_.op("act", [self.psr[pb]], [rtv[b]], "copy", out=tvb[b][:, 512:896], in_=self.ps[pb][:, 0:384])
                S_.op("act", [self.psr[pb]], [self.rBG], "activation", out=self.BGs[:, t, :],
                      in_=self.ps[pb][:, 384:402], func=AF.Sigmoid)
                S_.dma("pool", [rtv[b]], [], self.scr["tv"][t * 128:(t + 1) * 128, :], tvb[b][:])
            S_.barrier()
        with ExitStack() as es:
            Wm = self.sb(es, "Wm", [128, 8, 3072], BF16)
            rW = Res()
            for j in range(6):
                S_.dma("pool", [], [rW], Wm[:, :, j * 512:(j + 1) * 512], wsrc(C_MG + j * 512, 512))
            mgb = [self.sb(es, f"mgb{i}", [128, 3072], BF16) for i in range(2)]
            rmg = [Res(), Res()]
            k = 0
            for t in range(NT):
                b = t % 2
                for j in range(6):
                    pp = k % 8
                    k += 1
                    for c in range(8):
                        S_.op("pe", [rW, self.rhT[t]], [self.psr[pp]], "matmul", self.ps[pp][:, :],
                              lhsT=self.hT[:, c, t * 128:(t + 1) * 128], rhs=Wm[:, c, j * 512:(j + 1) * 512],
                              start=(c == 0), stop=(c == 7))
                    S_.op("act", [self.psr[pp]], [rmg[b]], "activation", out=mgb[b][:, j * 512:(j + 1) * 512],
                          in_=self.ps[pp][:, :], func=AF.Sigmoid)
                S_.dma("pool", [rmg[b]], [], self.scr["mg"][t * 128:(t + 1) * 128, :], mgb[b][:])
            S_.barrier()
        self.es_h.close()


def host_shared(inp):
    w = np.asarray(inp["w_gate_up"], np.float32).reshape(DEPTH, NEXP, 8, 128, 2 * D)
    wg = np.ascontiguousarray(np.transpose(w, (0, 1, 3, 2, 4))).reshape(DEPTH, NEXP * 128, 16 * D)
    w = np.asarray(inp["w_down"], np.float32).reshape(DEPTH, NEXP, 8, 128, D)
    wd = np.ascontiguousarray(np.transpose(w, (0, 1, 3, 2, 4))).reshape(DEPTH, NEXP * 128, 8 * D)
    return {"w_gu_l": wg, "w_dn_l": wd}


def host_inputs(inp, consts, b, shared=None):
    if shared is None:
        shared = host_shared(inp)
    m = {"x": np.ascontiguousarray(inp["x"][b], dtype=np.float32),
         "cT": np.ascontiguousarray(np.asarray(inp["c"][b], np.float32).reshape(8, 128).T),
         "pos": np.ascontiguousarray(inp["positions"][b:b + 1]).astype(np.int32)}
    for n in WNAMES:
        if n == "cmp_peT_k":
            m[n] = np.ascontiguousarray(np.transpose(np.asarray(inp["cmp_pe_k"], np.float32), (0, 2, 1)))
        elif n == "cmp_peT_v":
            m[n] = np.ascontiguousarray(np.transpose(np.asarray(inp["cmp_pe_v"], np.float32), (0, 2, 1)))
        elif n == "b_gu_l":
            bg = np.asarray(inp["b_gate_up"], np.float32).reshape(DEPTH, NEXP, 8, 128, 2)
            m[n] = np.ascontiguousarray(np.transpose(bg, (0, 1, 3, 2, 4)).reshape(DEPTH, NEXP * 128, 16))
        elif n == "w_gu_l":
            m[n] = shared["w_gu_l"]
        elif n == "w_dn_l":
            m[n] = shared["w_dn_l"]
        else:
            m[n] = np.ascontiguousarray(inp[n], dtype=np.float32)
    for n, a in consts.items():
        m["k_" + n] = a
    return m


class AttnPipe:
    def __init__(self, prog, es, ncol, nbuf=3):
        self.p = prog
        self.S_ = prog.S_
        self.ncol = ncol
        self.G = 512 // ncol
        self.pt = [prog.sb(es, f"pt{i}", [128, 512], BF16) for i in range(nbuf)]
        self.rpt = [Res() for _ in range(nbuf)]
        self.k = 0
        self.pending = None
        self.sbanks = (0, 1, 2)

    def qtile(self, items, qrhs, nh, obank, oregs, rq, post):
        p, S_ = self.p, self.S_
        ncol = self.ncol
        n = len(items)
        for g0 in range(0, n, self.G):
            grp = items[g0:g0 + self.G]
            bank = self.sbanks[self.k % 3]
            buf = self.k % len(self.pt)
            self.k += 1
            for j, (kT, v, extras, rds) in enumerate(grp):
                reg = p.ps[bank][:, j * ncol:(j + 1) * ncol]
                S_.op("pe", rq + rds, [p.psr[bank]], "matmul", reg, lhsT=kT, rhs=qrhs, start=True,
                      stop=(len(extras) == 0))
                for xi, (xl, xr) in enumerate(extras):
                    for hh in range(nh):
                        S_.op("pe", rq + rds, [p.psr[bank]], "matmul", reg[:, hh * 128:(hh + 1) * 128], lhsT=xl,
                              rhs=xr, start=False, stop=(xi == len(extras) - 1))
            if self.pending is not None:
                self.pending()
            w = len(grp) * ncol
            S_.op("act", [p.psr[bank]], [self.rpt[buf]], "activation", out=self.pt[buf][:, 0:w],
                  in_=p.ps[bank][:, 0:w], func=AF.Exp, scale=SCALE)
            first = (g0 == 0)
            last = (g0 + self.G >= n)

            def pv(grp=grp, buf=buf, first=first, last=last, g0=g0):
                multi = nh > 1
                if multi and first:
                    wtot = oregs[-1][0] + oregs[-1][1]
                    S_.op("pe", [p.rc], [p.psr[obank]], "matmul", p.ps[obank][:, 0:wtot], lhsT=p.zeros[:, 0:128],
                          rhs=p.zeros[:, 0:wtot], start=True, stop=False)
                for j, (kT, v, extras, rds) in enumerate(grp):
                    for hh in range(nh):
                        c0, wd = oregs[hh]
                        S_.op("pe", [self.rpt[buf]] + rds, [p.psr[obank]], "matmul", p.ps[obank][:, c0:c0 + wd],
                              lhsT=self.pt[buf][:, j * ncol + hh * 128:j * ncol + (hh + 1) * 128], rhs=v,
                              start=(first and j == 0 and not multi),
                              stop=(last and j == len(grp) - 1 and hh == nh - 1))
                if last:
                    post()
            self.pending = pv

    def flush(self):
        if self.pending is not None:
            self.pending()
            self.pending = None


def _moba(self):
    S_ = self.S_
    for pr in range(2):
        with ExitStack() as es:
            Qh = [self.sb(es, "QA", [128, S], BF16), self.sb(es, "QB", [128, S], BF16)]
            KT = self.sb(es, "KT", [128, S], BF16)
            V = self.sb(es, "V", [128, NT, 2, 65], BF16)
            km = self.sb(es, "km", [128, 16], F32)
            kmb = self.sb(es, "kmb", [128, 16], BF16)
            bT = [self.sb(es, f"bT{i}", [128, S], BF16) for i in range(2)]
            e16 = self.sb(es, "e16", [128, 16, 128], BF16)
            O = self.sb(es, "O", [128, NT, 128], BF16)
            wk = [self.sb(es, f"wk{i}", [128, 16], F32) for i in range(2)]
            m8 = [self.sb(es, f"m8{i}", [128, 8], F32) for i in range(2)]
            bs = [self.sb(es, f"bs{i}", [128, 16], F32) for i in range(2)]
            rd = [self.sb(es, f"rd{i}", [128, 1], F32) for i in range(2)]
            rQ, rK, rV, rkm, re, rO = Res(), Res(), Res(), Res(), Res(), Res()
            rbT = [[Res() for _ in range(NT)] for _ in range(2)]
            rwk, rm8, rbs, rrd = [Res(), Res()], [Res(), Res()], [Res(), Res()], [Res(), Res()]
            S_.op("pool", [], [rQ], "memset", Qh[0][64:128, :], 0.0)
            S_.op("pool", [], [rQ], "memset", Qh[1][0:64, :], 0.0)
            S_.dma("sp", [], [rQ], Qh[0][0:64, :], self.scr["ft"][FB_AQ + pr][0:64, :])
            S_.dma("sp", [], [rQ], Qh[1][64:128, :], self.scr["ft"][FB_AQ + pr][64:128, :])
            S_.dma("sp", [], [rK], KT[:], self.scr["ft"][FB_AK + pr])
            S_.op("pool", [], [re], "memset", e16[:], 0.0)
            S_.dma("sp", [], [re], e16[0:16, :, :], self.C["esel16"])
            rbT0 = Res()
            for i in range(2):
                S_.op("pool", [], [rbT0], "memset", bT[i][:], 0.0)
            S_.op("pool", [], [rV], "memset", V[:, :, :, 64:65], 1.0)
            for hh in range(2):
                S_.dma("sp", [], [rV], V[:, :, hh, 0:64],
                       self.scr["tv"][:, pr * 128 + hh * 64:pr * 128 + (hh + 1) * 64].rearrange("(t p) d -> p t d", p=128))
            S_.op("dve", [rK], [rkm], "tensor_reduce", out=km[:], in_=KT[:].rearrange("p (n k) -> p n k", k=256),
                  axis=AX.X, op=ALU.add)
            S_.op("dve", [rkm], [rkm], "tensor_scalar", out=kmb[:], in0=km[:], scalar1=1.0 / 256, scalar2=None,
                  op0=ALU.mult)
            def bias_body(hh, qt, kk):
                h0 = 64 * hh
                own = qt // 2
                b = kk % 2
                pg = 5 + (kk % 2)
                S_.op("dve", [], [rwk[b]], "memset", wk[b][:, own:16], -BIG)
                S_.op("pe", [rQ, rkm], [self.psr[pg]], "matmul", self.ps[pg][:, 0:16],
                      lhsT=Qh[hh][:, qt * 128:(qt + 1) * 128], rhs=kmb[:, :], start=True, stop=True)
                S_.op("dve", [self.psr[pg]], [rwk[b]], "tensor_copy", out=wk[b][:, 0:own], in_=self.ps[pg][:, 0:own])
                S_.op("dve", [rwk[b]], [rm8[b]], "max", out=m8[b][:], in_=wk[b][:])
                S_.op("dve", [rwk[b], rm8[b]], [rbs[b]], "tensor_scalar", out=bs[b][:], in0=wk[b][:],
                      scalar1=m8[b][:, 2:3], scalar2=NEG, op0=ALU.is_lt, op1=ALU.mult)
                yield
                S_.op("pe", [rbs[b], self.rc], [self.psr[7]], "transpose", out=self.ps[7][0:16, 0:128],
                      in_=bs[b][:], identity=self.identf[:])
                S_.op("act", [self.psr[7], rbT0], [rbT[hh][qt]], "copy", out=bT[hh][0:16, qt * 128:(qt + 1) * 128],
                      in_=self.ps[7][0:16, 0:128])
            round_robin((bias_body(hh, qt, i) for i, (hh, qt) in
                         enumerate((hh, qt) for hh in range(2) for qt in range(8, NT))), 2)
            ap_ = AttnPipe(self, es, 128)
            for hh in range(2):
                h0 = 64 * hh
                for qt in range(NT):
                    own = qt // 2
                    items = []
                    for kt in range(qt + 1):
                        n = kt // 2
                        ex = []
                        rds = [rK, rV]
                        if n < own and qt >= 8:
                            ex = [(e16[:, n, :], bT[hh][:, qt * 128:(qt + 1) * 128])]
                            rds = rds + [re, rbT[hh][qt]]
                        elif kt == qt:
                            ex = [(self.identb[:], self.masks[:, 0, :])]
                        items.append((KT[:, kt * 128:(kt + 1) * 128], V[:, kt, hh, :], ex, rds))
                    ob = 3 + (qt % 2)

                    def post(ob=ob, qt=qt, hh=hh):
                        b = qt % 2
                        S_.op("dve", [self.psr[ob]], [rrd[b]], "reciprocal", out=rd[b][:], in_=self.ps[ob][:, 64:65])
                        S_.op("dve", [self.psr[ob], rrd[b]], [rO], "tensor_scalar", out=O[:, qt, hh * 64:(hh + 1) * 64],
                              in0=self.ps[ob][:, 0:64], scalar1=rd[b][:, 0:1], scalar2=None, op0=ALU.mult)
                    ap_.qtile(items, Qh[hh][:, qt * 128:(qt + 1) * 128], 1, ob, [(0, 65)], [rQ], post)
            ap_.flush()
            S_.dma("pool", [rO], [], self.scr["o"][:, pr * 128:(pr + 1) * 128].rearrange("(t p) c -> p t c", p=128), O[:])
            S_.barrier()


Prog.phase_moba = _moba


def _nsa(self):
    S_ = self.S_
    l = self.l
    with ExitStack() as es:
        KsT = self.sb(es, "KsT", [128, S], BF16)
        KwT = self.sb(es, "KwT", [128, S], BF16)
        Vs = self.sb(es, "Vs", [128, NT, 2, 65], BF16)
        Vw = self.sb(es, "Vw", [128, NT, 2, 65], BF16)
        cmpm = self.sb(es, "cmpm", [128, 2, S], BF16)
        nadd = self.sb(es, "nadd", [128, NT, 64], F32)
        nmin = self.sb(es, "nmin", [128, NT, 64], F32)
        e64 = self.sb(es, "e64", [128, 32, 128], BF16)
        ovl = self.sb(es, "ovl", [128, 2, 64], BF16)
        KcT = self.sb(es, "KcT", [128, 256], BF16)
        VcA = self.sb(es, "VcA", [128, 2, 2, 129], BF16)
        Oall = self.sb(es, "Oall", [128, NT, 384], BF16)
        ec = ExitStack()
        KcP = self.sb(ec, "KcP", [128, S], BF16)
        VcP = self.sb(ec, "VcP", [128, S], BF16)
        w1 = {"k": self.sb(ec, "w1k", [128, 32, 128], BF16), "v": self.sb(ec, "w1v", [128, 32, 128], BF16)}
        pe_ = {"k": self.sb(ec, "pek", [128, 32], BF16), "v": self.sb(ec, "pev", [128, 32], BF16)}
        w2kp = self.sb(ec, "w2kp", [128, 2, 128], BF16)
        w2v = self.sb(ec, "w2v", [128, 64], BF16)
        hid = [self.sb(ec, f"hid{i}", [128, 256], BF16) for i in range(2)]
        peb = [self.sb(ec, f"peb{i}", [128, 1], F32) for i in range(2)]
        rld, rw, rV, rKc, rVc, rO = Res(), Res(), Res(), Res(), Res(), Res()
        for t_, fb in ((KcP, FB_KC), (VcP, FB_VC), (KsT, FB_KS), (KwT, FB_KW)):
            S_.dma("sp", [], [rld], t_[:], self.scr["ft"][fb])
        S_.op("pool", [], [rld], "memset", e64[64:128, :, :], 0.0)
        S_.dma("sp", [], [rld], e64[0:64, :, :], self.C["esel64"])
        for t_, n_ in ((cmpm, "cmpmask"), (nadd, "nsa_add"), (nmin, "nsa_min"), (ovl, "overlap")):
            S_.dma("sp", [], [rld], t_[:], self.C[n_])
        for vt, c0 in ((Vs, 256), (Vw, 384)):
            S_.op("pool", [], [rV], "memset", vt[:, :, :, 64:65], 1.0)
            for g in range(2):
                S_.dma("sp", [], [rV], vt[:, :, g, 0:64],
                       self.scr["tv"][:, c0 + g * 64:c0 + (g + 1) * 64].rearrange("(t p) d -> p t d", p=128))
        S_.op("pool", [], [rw], "memset", w2kp[:], 0.0)
        for kind in ("k", "v"):
            for g in range(2):
                S_.dma("pool", [], [rw], w1[kind][64 * g:64 * g + 64, :, :],
                       self.I["cmp_w1_" + kind][l].rearrange("(l d) h -> d l h", d=64))
                S_.dma("pool", [], [rw], pe_[kind][64 * g:64 * g + 64, :], self.I["cmp_peT_" + kind][l])
        for g in range(2):
            S_.dma("pool", [], [rw], w2kp[:, g, 64 * g:64 * g + 64], self.I["cmp_w2_k"][l])
        S_.dma("pool", [], [rw], w2v[:], self.I["cmp_w2_v"][l])
        S_.op("pool", [], [rVc], "memset", VcA[:, :, :, 64:65], 1.0)
        for nt in range(2):
            for g in range(2):
                S_.op("pool", [rld], [rVc], "tensor_copy", out=VcA[:, nt, g, 65:129], in_=ovl[:, nt, :])
        rhid, rpeb = [Res(), Res()], [Res(), Res()]
        kk = 0
        for kind, src in (("k", KcP), ("v", VcP)):
            s16 = src[:].rearrange("p (n s) -> p n s", s=16)
            for g in range(2):
                h0 = 64 * g
                b = kk % 2
                kk += 1
                for li in range(32):
                    S_.op("pe", [rw], [self.psr[5]], "matmul", self.ps[5][:, 0:1], lhsT=w1[kind][h0:h0 + 64, li, :],
                          rhs=pe_[kind][h0:h0 + 64, li:li + 1], start=(li == 0), stop=(li == 31))
                S_.op("dve", [self.psr[5]], [rpeb[b]], "tensor_copy", out=peb[b][:], in_=self.ps[5][:, 0:1])
                for li in range(32):
                    rhs = s16[h0:h0 + 64, 0:255, li] if li < 16 else s16[h0:h0 + 64, 1:256, li - 16]
                    S_.op("pe", [rw, rld], [self.psr[6]], "matmul", self.ps[6][:, 0:255], lhsT=w1[kind][h0:h0 + 64, li, :],
                          rhs=rhs, start=(li == 0), stop=(li == 31))
                S_.op("dve", [], [rhid[b]], "memset", hid[b][:, 255:256], 0.0)
                S_.op("act", [self.psr[6], rpeb[b]], [rhid[b]], "activation", out=hid[b][:, 0:255], in_=self.ps[6][:, 0:255],
                      func=AF.Silu, bias=peb[b][:, 0:1], scale=1.0)
                if kind == "k":
                    S_.op("pe", [rw, rhid[b]], [self.psr[7]], "matmul", self.ps[7][:, 0:256], lhsT=w2kp[:, g, :],
                          rhs=hid[b][:], start=(g == 0), stop=(g == 1))
                    if g == 1:
                        S_.op("act", [self.psr[7]], [rKc], "copy", out=KcT[:], in_=self.ps[7][:, 0:256])
                else:
                    for nt in range(2):
                        S_.op("pe", [rw, rhid[b]], [self.psr[7]], "matmul", self.ps[7][:, nt * 64:(nt + 1) * 64],
                              lhsT=hid[b][:, nt * 128:(nt + 1) * 128], rhs=w2v[:], start=True, stop=True)
                    S_.op("act", [self.psr[7]], [rVc], "copy", out=VcA[:, :, g, 0:64],
                          in_=self.ps[7][:, 0:128].rearrange("p (n d) -> p n d", d=64))
        S_.barrier()
        ec.close()
        QG = [self.sb(es, f"QG{g}", [128, 3, S], BF16) for g in range(2)]
        S_.op("pool", [], [rld], "memset", QG[0][64:128, :, :], 0.0)
        S_.op("pool", [], [rld], "memset", QG[1][0:64, :, :], 0.0)
        for r in range(3):
            S_.dma("sp", [], [rld], QG[0][0:64, r, :], self.scr["ft"][FB_BQ + r][0:64, :])
            S_.dma("sp", [], [rld], QG[1][64:128, r, :], self.scr["ft"][FB_BQ + r][64:128, :])
        import os
        if self.debug:
            S_.dma("pool", [rKc], [], self.scr["dbg1"], KcT[:])
            S_.dma("pool", [rVc], [], self.scr["dbg2"], VcA[:].rearrange("p a b c -> p (a b c)"))
        if os.environ.get("NSA_STOP") == "1":
            S_.barrier()
            return
        bT = [[self.sb(es, f"nbT{g}{b}", [128, 128], BF16) for b in range(2)] for g in range(2)]
        rbT = [[Res(), Res()], [Res(), Res()]]
        for g in range(2):
            for b in range(2):
                S_.op("pool", [], [rbT[g][b]], "memset", bT[g][b][:], 0.0)
        Ob = [self.sb(es, f"Ob{b}", [128, 6, 64], F32) for b in range(2)]
        rOb = [Res(), Res()]
        NS = 4
        rdn = [self.sb(es, f"rdn{i}", [128, 3], F32) for i in range(NS)]
        sc3 = [self.sb(es, f"sc3{i}", [128, 3], F32) for i in range(NS)]
        imp = [self.sb(es, f"imp{i}", [128, 64], F32) for i in range(2)]
        imp2 = [self.sb(es, f"imp2{i}", [128, 64], F32) for i in range(2)]
        m8a = [self.sb(es, f"m8a{i}", [128, 8], F32) for i in range(2)]
        m8b = [self.sb(es, f"m8b{i}", [128, 8], F32) for i in range(2)]
        bsf = [self.sb(es, f"bsf{i}", [128, 64], F32) for i in range(2)]
        rsm = [Res() for _ in range(NS)]
        rimp = [Res(), Res()]
        ap_ = AttnPipe(self, es, 384)
        cnt = {"o": 0, "s": 0, "i": 0}

        def gw(qt, g, br):
            return self.BGs[:, qt, :].rearrange("p (h k) -> p h k", k=3)[:, 3 * g:3 * g + 3, br]

        def post_common(ob, wd, qt, g, br, first):
            i = cnt["s"] % NS
            cnt["s"] += 1
            b = qt % 2
            den = self.ps[ob][:, 0:3 * wd].rearrange("p (r w) -> p r w", w=wd)[:, :, 64]
            S_.op("dve", [self.psr[ob]], [rsm[i]], "tensor_scalar", out=rdn[i][:], in0=den, scalar1=1e-30, scalar2=None,
                  op0=ALU.max)
            S_.op("dve", [rsm[i]], [rsm[i]], "reciprocal", out=rdn[i][:], in_=rdn[i][:])
            S_.op("dve", [rsm[i], self.rBG], [rsm[i]], "tensor_tensor", out=sc3[i][:], in0=rdn[i][:], in1=gw(qt, g, br),
                  op=ALU.mult)
            for r in range(3):
                src = self.ps[ob][:, r * wd:r * wd + 64]
                if first:
                    S_.op("dve", [self.psr[ob], rsm[i]], [rOb[b]], "tensor_scalar", out=Ob[b][:, 3 * g + r, :], in0=src,
                          scalar1=sc3[i][:, r:r + 1], scalar2=None, op0=ALU.mult)
                else:
                    S_.op("dve", [self.psr[ob], rsm[i]], [rOb[b]], "scalar_tensor_tensor", out=Ob[b][:, 3 * g + r, :],
                          in0=src, scalar=sc3[i][:, r:r + 1], in1=Ob[b][:, 3 * g + r, :], op0=ALU.mult, op1=ALU.add)
            return i

        nqt = int(os.environ.get("NSA_NQT", NT))
        brs = os.environ.get("NSA_BR", "csw")
        deferred = []

        def emit_cmp(qt):
            b = qt % 2
            qs = slice(qt * 128, (qt + 1) * 128)
            for g in range(2):
                h0 = 64 * g
                qrhs = QG[g][:, :, qs]
                items = []
                for nt in ([0, 1] if qt >= 16 else [0]):
                    ex = []
                    if nt == 1 or qt < 17:
                        ex = [(self.identb[:], cmpm[:, nt, qs])]
                    items.append((KcT[:, nt * 128:(nt + 1) * 128], VcA[:, nt, g, :], ex, [rKc, rVc, rld]))
                ob = 3 + cnt["o"] % 3
                cnt["o"] += 1

                def post_cmp(ob=ob, qt=qt, g=g, b=b):
                    i = post_common(ob, 129, qt, g, 0, True)
                    j = cnt["i"] % 2
                    cnt["i"] += 1
                    for r in range(3):
                        src = self.ps[ob][:, r * 129 + 65:r * 129 + 129]
                        if r == 0:
                            S_.op("dve", [self.psr[ob], rsm[i]], [rimp[j]], "tensor_scalar", out=imp[j][:], in0=src,
                                  scalar1=rdn[i][:, 0:1], scalar2=None, op0=ALU.mult)
                        else:
                            S_.op("dve", [self.psr[ob], rsm[i]], [rimp[j]], "scalar_tensor_tensor", out=imp[j][:], in0=src,
                                  scalar=rdn[i][:, r:r + 1], in1=imp[j][:], op0=ALU.mult, op1=ALU.add)
                    S_.op("dve", [rld], [rimp[j]], "tensor_tensor", out=imp[j][:], in0=imp[j][:], in1=nadd[:, qt, :], op=ALU.add)
                    S_.op("dve", [rld], [rimp[j]], "tensor_tensor", out=imp[j][:], in0=imp[j][:], in1=nmin[:, qt, :], op=ALU.min)
                    S_.op("dve", [rimp[j]], [rimp[j]], "max", out=m8a[j][:], in_=imp[j][:])
                    S_.op("dve", [rimp[j]], [rimp[j]], "match_replace", out=imp2[j][:], in_to_replace=m8a[j][:],
                          in_values=imp[j][:], imm_value=-BIG)
                    S_.op("dve", [rimp[j]], [rimp[j]], "max", out=m8b[j][:], in_=imp2[j][:])
                    S_.op("dve", [rimp[j]], [rimp[j]], "tensor_scalar", out=bsf[j][:], in0=imp[j][:], scalar1=m8b[j][:, 7:8],
                          scalar2=NEG, op0=ALU.is_lt, op1=ALU.mult)
                    def tr(j=j, g=g, b=b):
                        S_.op("pe", [rimp[j], self.rc], [self.psr[7]], "transpose", out=self.ps[7][0:64, 0:128], in_=bsf[j][:],
                              identity=self.identf[:])
                        S_.op("act", [self.psr[7]], [rbT[g][b]], "copy", out=bT[g][b][0:64, :], in_=self.ps[7][0:64, 0:128])
                    deferred.append(tr)
                ap_.qtile(items, qrhs, 3, ob, [(r * 129, 129) for r in range(3)], [rld], post_cmp)

        def emit_sw(qt):
            b = qt % 2
            qs = slice(qt * 128, (qt + 1) * 128)
            for br, KT_, V_ in ((1, KsT, Vs), (2, KwT, Vw)):
                if "csw"[br] not in brs:
                    continue
                for g in range(2):
                    h0 = 64 * g
                    qrhs = QG[g][:, :, qs]
                    items = []
                    k0 = 0 if br == 1 else max(0, qt - 4)
                    for kt in range(k0, qt + 1):
                        ex = []
                        rds = [rld, rV]
                        if br == 1:
                            ex.append((e64[:, kt, :], bT[g][b][:]))
                            rds = rds + [rbT[g][b]]
                        if kt == qt:
                            ex.append((self.identb[:], self.masks[:, 0, :]))
                        elif br == 2 and kt == qt - 4:
                            ex.append((self.identb[:], self.masks[:, 1, :]))
                        items.append((KT_[:, kt * 128:(kt + 1) * 128], V_[:, kt, g, :], ex, rds))
                    ob = 3 + cnt["o"] % 3
                    cnt["o"] += 1

                    def post_sw(ob=ob, qt=qt, g=g, br=br, b=b):
                        post_common(ob, 65, qt, g, br, False)
                        if br == 2 and g == 1:
                            S_.op("pool", [rOb[b]], [rO], "tensor_copy", out=Oall[:, qt, :],
                                  in_=Ob[b][:].rearrange("p h d -> p (h d)"))
                    ap_.qtile(items, qrhs, 3, ob, [(r * 65, 65) for r in range(3)], [rld], post_sw)

        for qt in range(nqt + 1):
            if qt < nqt:
                emit_cmp(qt)
            if qt >= 1:
                emit_sw(qt - 1)
            else:
                ap_.flush()
            while deferred:
                deferred.pop(0)()
        ap_.flush()
        S_.dma("pool", [rO], [], self.scr["o"][:, 256:640].rearrange("(t p) c -> p t c", p=128), Oall[:])
        S_.barrier()


Prog.phase_nsa = _nsa


def _dil(self):
    S_ = self.S_
    with ExitStack() as es:
        QD = [self.sb(es, f"QD{i}", [128, 3, S], BF16) for i in range(2)]
        KTc = self.sb(es, "KTc", [128, 3, S], BF16)
        Vg = [self.sb(es, f"Vg{gi}", [128, NT, 2, 65], BF16) for gi in range(3)]
        rld, rV = Res(), Res()
        S_.op("pool", [], [rld], "memset", QD[0][64:128, :, :], 0.0)
        S_.op("pool", [], [rld], "memset", QD[1][0:64, :, :], 0.0)
        for gi in range(3):
            S_.dma("sp", [], [rld], QD[0][0:64, gi, :], self.scr["ft"][FB_CQ + gi][0:64, :])
            S_.dma("sp", [], [rld], QD[1][64:128, gi, :], self.scr["ft"][FB_CQ + gi][64:128, :])
            S_.dma("sp", [], [rld], KTc[:, gi, :], self.scr["ft"][FB_CK + gi])
        for gi, dil in enumerate((1, 4, 16)):
            ntile = NT // dil
            S_.op("pool", [], [rV], "memset", Vg[gi][:, :, :, 64:65], 1.0)
            for r in range(dil):
                for hs in range(2):
                    c0 = 512 + gi * 128 + hs * 64
                    src = self.scr["tv"][:, c0:c0 + 64].rearrange("(l d) c -> d l c", d=dil)[r]
                    S_.dma("sp", [], [rV], Vg[gi][:, r * ntile:(r + 1) * ntile, hs, 0:64],
                           src.rearrange("(j p) c -> p j c", p=128))
        ocs = [self.sb(es, f"ocs{i}", [128, 65], F32) for i in range(4)]
        rocs = [Res() for _ in range(4)]
        ap_ = AttnPipe(self, es, 128)
        cnt = {"o": 0}
        for gi, dil in enumerate((1, 4, 16)):
            ntile = NT // dil
            ocv = self.scr["oc"][gi].rearrange("(l d) c -> d l c", d=dil)
            for hs in range(2):
                h0 = 64 * hs
                qv = QD[hs][:, gi, :].rearrange("p (l d) -> p d l", d=dil)
                kv = KTc[:, gi, :].rearrange("p (l d) -> p d l", d=dil)
                for r in range(dil):
                    for j in range(ntile):
                        items = []
                        for kt in ([j - 1, j] if j >= 1 else [j]):
                            mk = 0 if kt == j else 2
                            items.append((kv[:, r, kt * 128:(kt + 1) * 128], Vg[gi][:, r * ntile + kt, hs, :],
                                          [(self.identb[:], self.masks[:, mk, :])], [rld, rV]))
                        ob = 3 + cnt["o"] % 3
                        cnt["o"] += 1

                        def post(ob=ob, r=r, j=j, hs=hs, ocv=ocv, i=cnt["o"] % 4):
                            S_.op("dve", [self.psr[ob]], [rocs[i]], "tensor_copy", out=ocs[i][:], in_=self.ps[ob][:, 0:65])
                            S_.dma("pool", [rocs[i]], [], ocv[r][j * 128:(j + 1) * 128, hs * 65:(hs + 1) * 65], ocs[i][:])
                        ap_.qtile(items, qv[:, r, j * 128:(j + 1) * 128], 1, ob, [(0, 65)], [rld], post)
        ap_.flush()
        S_.barrier()
        Oc = self.sb(es, "Oc", [128, NT, 128], BF16)
        rOc = Res()
        acc = [[self.sb(es, f"acc{i}{gi}", [128, 8, 130], F32) for gi in range(3)] for i in range(2)]
        racc = [Res(), Res()]
        rdc = [self.sb(es, f"rdc{i}", [128, 8, 2], F32) for i in range(2)]
        for ch in range(4):
            i = ch % 2
            for gi in range(3):
                S_.dma("sp", [], [racc[i]], acc[i][gi][:],
                       self.scr["oc"][gi][ch * 1024:(ch + 1) * 1024, :].rearrange("(t p) c -> p t c", p=128))
            S_.op("dve", [racc[i]], [racc[i]], "tensor_tensor", out=acc[i][0][:], in0=acc[i][0][:], in1=acc[i][1][:], op=ALU.add)
            S_.op("dve", [racc[i]], [racc[i]], "tensor_tensor", out=acc[i][0][:], in0=acc[i][0][:], in1=acc[i][2][:], op=ALU.add)
            for hs in range(2):
                S_.op("dve", [racc[i]], [racc[i]], "reciprocal", out=rdc[i][:, :, hs:hs + 1],
                      in_=acc[i][0][:, :, hs * 65 + 64:hs * 65 + 65])
            for t in range(8):
                for hs in range(2):
                    S_.op("dve", [racc[i]], [rOc], "tensor_scalar", out=Oc[:, ch * 8 + t, hs * 64:(hs + 1) * 64],
                          in0=acc[i][0][:, t, hs * 65:hs * 65 + 64], scalar1=rdc[i][:, t, hs:hs + 1], scalar2=None,
                          op0=ALU.mult)
        S_.dma("pool", [rOc], [], self.scr["o"][:, 640:768].rearrange("(t p) c -> p t c", p=128), Oc[:])
        S_.barrier()


Prog.phase_dil = _dil


def _merge(self, xin, xout):
    S_ = self.S_
    l = self.l
    with ExitStack() as es:
        Wa = self.sb(es, "Wa", [128, 6, D], BF16)
        Wo = self.sb(es, "Wo", [128, 8, D], BF16)
        Wr = self.sb(es, "Wr", [128, 8, NEXP], BF16)
        rW, rB = Res(), Res()
        for nm, c0, nck in (("w_branch_a", 0, 2), ("w_branch_b", 2, 3), ("w_branch_c", 5, 1)):
            S_.dma("pool", [], [rW], Wa[:, c0:c0 + nck, :], self.I[nm][l].rearrange("(c p) n -> p c n", p=128))
        S_.dma("pool", [], [rW], Wo[:], self.I["w_out"][l].rearrange("(c p) n -> p c n", p=128))
        S_.dma("pool", [], [rW], Wr[:], self.I["router_w"][l].rearrange("(c p) n -> p c n", p=128))
        g1b = self.bload(es, "g1b", self.scr["mod"][l:l + 1, 2 * D:3 * D], rB)
        l1g = self.bload(es, "l1g", self.I["ln1_g"][l:l + 1, :], rB)
        l1b = self.bload(es, "l1b", self.I["ln1_b"][l:l + 1, :], rB)
        sc2 = self.bload(es, "sc2", self.scr["mod"][l:l + 1, 4 * D:5 * D], rB)
        sh2 = self.bload(es, "sh2", self.scr["mod"][l:l + 1, 3 * D:4 * D], rB)
        rbb = self.sb(es, "rbb", [128, NEXP], F32)
        S_.dma("sp", [], [rB], rbb[:], self.I["router_b"][l:l + 1, :].partition_broadcast(128))

        def dbl(name, shape, dt):
            return [self.sb(es, f"{name}{i}", shape, dt) for i in range(2)], [Res(), Res()]
        ot, rot = dbl("ot", [128, 768], BF16)
        gt, rgt = dbl("gt", [128, 3072], BF16)
        xt, rxt = dbl("mxt", [128, D], F32)
        oT, roT = dbl("oT", [128, 6, 128], BF16)
        m1, rm1 = dbl("m1", [128, 512], F32)
        m2, rm2 = dbl("m2", [128, 512], F32)
        m3, rm3 = dbl("m3", [128, 512], F32)
        mb, rmb = dbl("mb", [128, D], BF16)
        mT, rmT = dbl("mT", [128, 8, 128], BF16)
        yt, ryt = dbl("yt", [128, D], F32)
        zt, rzt = dbl("zt", [128, D], F32)
        x1, rx1 = dbl("x1", [128, D], F32)
        xn2, rxn2 = dbl("xn2", [128, D], F32)
        hb2, rhb2 = dbl("hb2", [128, D], BF16)
        hT2, rhT2 = dbl("hT2", [128, 8, 128], BF16)
        st1, rst1 = dbl("st1", [128, 16], F32)
        st2, rst2 = dbl("st2", [128, 16], F32)
        lg, rlg = dbl("lg", [128, NEXP], F32)
        ex, rex = dbl("ex", [128, NEXP], F32)
        m8, rm8 = dbl("rm8", [128, 8], F32)
        sm, rsm = dbl("rsm", [128, 4], F32)
        def tile_body(t):
            b = t % 2
            rows = slice(t * 128, (t + 1) * 128)
            S_.dma("sp", [], [rot[b]], ot[b][:], self.scr["o"][rows, :])
            S_.dma("sp", [], [rgt[b]], gt[b][:], self.scr["mg"][rows, :])
            S_.dma("sp", [], [rxt[b]], xt[b][:], xin[rows, :])
            pT = 6 + b
            pst = self.ps[pT][:].bitcast(BF16)
            for c in range(6):
                S_.op("pe", [rot[b], self.rc], [self.psr[pT]], "transpose", out=pst[:, c * 128:(c + 1) * 128],
                      in_=ot[b][:, c * 128:(c + 1) * 128], identity=self.identb[:])
            S_.op("act", [self.psr[pT]], [roT[b]], "copy", out=oT[b][:], in_=pst[:, 0:768].rearrange("p (c n) -> p c n", c=6))
            yield
            for half in range(2):
                hs_ = slice(half * 512, (half + 1) * 512)
                pbs = (0, 1, 2) if half == 0 else (3, 4, 5)
                for bi, (c0, nck) in enumerate(((0, 2), (2, 3), (5, 1))):
                    for c in range(nck):
                        S_.op("pe", [roT[b], rW], [self.psr[pbs[bi]]], "matmul", self.ps[pbs[bi]][:, :],
                              lhsT=oT[b][:, c0 + c, :], rhs=Wa[:, c0 + c, hs_], start=(c == 0), stop=(c == nck - 1))
                for bi, (mm_, rmm) in enumerate(((m1, rm1), (m2, rm2), (m3, rm3))):
                    S_.op("dve", [self.psr[pbs[bi]], rgt[b]], [rmm[b]], "tensor_tensor", out=mm_[b][:],
                          in0=self.ps[pbs[bi]][:, :], in1=gt[b][:, bi * 1024 + half * 512:bi * 1024 + (half + 1) * 512],
                          op=ALU.mult)
                S_.op("pool", [rm1[b], rm2[b]], [rm1[b]], "tensor_tensor", out=m1[b][:], in0=m1[b][:], in1=m2[b][:], op=ALU.add)
                S_.op("pool", [rm1[b], rm3[b]], [rmb[b]], "tensor_tensor", out=mb[b][:, hs_], in0=m1[b][:], in1=m3[b][:],
                      op=ALU.add)
                yield
            for c in range(8):
                S_.op("pe", [rmb[b], self.rc], [self.psr[pT]], "transpose", out=pst[:, c * 128:(c + 1) * 128],
                      in_=mb[b][:, c * 128:(c + 1) * 128], identity=self.identb[:])
            S_.op("act", [self.psr[pT]], [rmT[b]], "copy", out=mT[b][:], in_=pst.rearrange("p (c n) -> p c n", c=8))
            yield
            for half in range(2):
                hs_ = slice(half * 512, (half + 1) * 512)
                pb_ = half
                for c in range(8):
                    S_.op("pe", [rmT[b], rW], [self.psr[pb_]], "matmul", self.ps[pb_][:, :], lhsT=mT[b][:, c, :],
                          rhs=Wo[:, c, hs_], start=(c == 0), stop=(c == 7))
                S_.op("dve", [self.psr[pb_], rB], [ryt[b]], "tensor_tensor", out=yt[b][:, hs_], in0=self.ps[pb_][:, :],
                      in1=g1b[:, hs_], op=ALU.mult)
            yield
            S_.op("dve", [ryt[b], rxt[b]], [rzt[b]], "scalar_tensor_tensor", out=zt[b][:], in0=xt[b][:], scalar=ALPHA,
                  in1=yt[b][:], op0=ALU.mult, op1=ALU.add)
            self.ln_tile(zt[b], rzt[b], None, None, st1[b], rst1[b], split=True)
            yield
            self.ln_tile2(st1[b], rst1[b])
            S_.op("act", [rzt[b], rst1[b]], [rzt[b]], "activation", out=zt[b][:], in_=zt[b][:], func=AF.Identity,
                  bias=st1[b][:, 1:2], scale=st1[b][:, 0:1])
            S_.op("pool", [rzt[b], rB], [rzt[b]], "tensor_tensor", out=zt[b][:], in0=zt[b][:], in1=l1g[:], op=ALU.mult)
            yield
            S_.op("dve", [rzt[b], rB], [rx1[b]], "tensor_tensor", out=x1[b][:], in0=zt[b][:], in1=l1b[:], op=ALU.add)
            S_.dma("pool", [rx1[b]], [], xout[rows, :], x1[b][:])
            self.ln_tile(x1[b], rx1[b], None, None, st2[b], rst2[b], split=True)
            yield
            self.ln_tile2(st2[b], rst2[b])
            S_.op("act", [rx1[b], rst2[b]], [rxn2[b]], "activation", out=xn2[b][:], in_=x1[b][:], func=AF.Identity,
                  bias=st2[b][:, 1:2], scale=st2[b][:, 0:1])
            S_.op("pool", [rxn2[b], rB], [rxn2[b]], "tensor_tensor", out=xn2[b][:], in0=xn2[b][:], in1=sc2[:], op=ALU.mult)
            yield
            S_.op("dve", [rxn2[b], rB], [rhb2[b]], "tensor_tensor", out=hb2[b][:], in0=xn2[b][:], in1=sh2[:], op=ALU.add)
            for c in range(8):
                S_.op("pe", [rhb2[b], self.rc], [self.psr[pT]], "transpose", out=pst[:, c * 128:(c + 1) * 128],
                      in_=hb2[b][:, c * 128:(c + 1) * 128], identity=self.identb[:])
            S_.op("act", [self.psr[pT]], [rhT2[b]], "copy", out=hT2[b][:], in_=pst.rearrange("p (c n) -> p c n", c=8))
            S_.dma("pool", [rhb2[b]], [], self.scr["h2"][rows, :], hb2[b][:])
            yield
            for c in range(8):
                S_.op("pe", [rhT2[b], rW], [self.psr[2]], "matmul", self.ps[2][:, 0:NEXP], lhsT=hT2[b][:, c, :],
                      rhs=Wr[:, c, :], start=(c == 0), stop=(c == 7))
            S_.op("dve", [self.psr[2], rB], [rlg[b]], "tensor_tensor", out=lg[b][:], in0=self.ps[2][:, 0:NEXP], in1=rbb[:],
                  op=ALU.add)
            S_.op("dve", [rlg[b]], [rm8[b]], "max", out=m8[b][:], in_=lg[b][:])
            S_.op("dve", [rm8[b]], [rsm[b]], "tensor_scalar", out=sm[b][:, 0:1], in0=m8[b][:, 0:1], scalar1=-1.0, scalar2=None,
                  op0=ALU.mult)
            S_.op("act", [rlg[b], rsm[b]], [rex[b]], "activation", out=ex[b][:], in_=lg[b][:], func=AF.Exp,
                  bias=sm[b][:, 0:1], scale=1.0)
            yield
            S_.op("dve", [rlg[b], rm8[b], rex[b]], [rex[b]], "scalar_tensor_tensor", out=ex[b][:], in0=lg[b][:],
                  scalar=m8[b][:, 3:4], in1=ex[b][:], op0=ALU.is_ge, op1=ALU.mult)
            S_.op("dve", [rlg[b], rm8[b]], [self.rS], "tensor_scalar", out=self.Sall[:, t, :], in0=lg[b][:],
                  scalar1=m8[b][:, 3:4], scalar2=None, op0=ALU.is_ge)
            S_.op("dve", [rex[b]], [rsm[b]], "tensor_reduce", out=sm[b][:, 1:2], in_=ex[b][:], axis=AX.X, op=ALU.add)
            S_.op("dve", [rsm[b]], [rsm[b]], "reciprocal", out=sm[b][:, 2:3], in_=sm[b][:, 1:2])
            S_.op("dve", [rex[b], rsm[b]], [self.rG], "tensor_scalar", out=self.Gall[:, t, :], in0=ex[b][:],
                  scalar1=sm[b][:, 2:3], scalar2=None, op0=ALU.mult)
        round_robin((tile_body(t) for t in range(NT)), 2)
        S_.barrier()


Prog.phase_merge = _merge


def _moe(self):
    S_ = self.S_
    l = self.l
    TS = 1024
    with ExitStack() as es:
        wgu = [self.sb(es, f"wgu{i}", [128, 8, 2 * D], BF16) for i in range(2)]
        wdn = [self.sb(es, f"wdn{i}", [128, 8, D], BF16) for i in range(2)]
        rwg, rwd = [Res(), Res()], [Res(), Res()]
        bgu = self.sb(es, "bgu", [128, NEXP * 16], F32)
        bdn = self.sb(es, "bdn", [NEXP, D], F32)
        h2 = self.sb(es, "h2", [128, 8, TS], BF16)
        yacc = self.sb(es, "yacc", [128, 8, D], F32)
        GT = self.sb(es, "GT", [NEXP, 8, 128], F32)
        actT = [self.sb(es, f"actT{i}", [128, 8, 512], BF16) for i in range(2)]
        xg = [self.sb(es, f"xg{i}", [128, 512], F32) for i in range(2)]
        sg = [self.sb(es, f"sg{i}", [128, 512], F32) for i in range(2)]
        xl = [self.sb(es, f"xl{i}", [128, 512], F32) for i in range(2)]
        rB, rh2, rGT = Res(), Res(), Res()
        ry = [Res() for _ in range(8)]
        ract = [Res(), Res()]
        rxg, rsg, rxl = [Res(), Res()], [Res(), Res()], [Res(), Res()]
        S_.dma("sp", [], [rB], bgu[:], self.I["b_gu_l"][l])
        S_.dma("sp", [], [rB], bdn[:], self.I["b_down"][l])
        bguv = bgu[:].rearrange("p (e j two) -> p e j two", e=NEXP, two=2)

        def load_wg(e, wb):
            src = self.I["w_gate_up"][l, e].rearrange("(c p) n -> p c n", p=128)
            for hlf in range(2):
                S_.dma("pool", [], [rwg[wb]], wgu[wb][:, hlf * 4:(hlf + 1) * 4, :], src[:, hlf * 4:(hlf + 1) * 4, :])

        def load_wd(e, wb):
            S_.dma("pool", [], [rwd[wb]], wdn[wb][:], self.I["w_down"][l, e].rearrange("(c p) n -> p c n", p=128))
        kq = 0
        ky = 0
        kw = 0
        pend = None
        load_wg(0, 0)
        load_wd(0, 0)
        for ts in range(S // TS):
            S_.dma("sp", [], [rh2], h2[:], self.scr["h2t"][:, :, ts * TS:(ts + 1) * TS])
            for tt in range(8):
                tg = ts * 8 + tt
                pb = 4 + ky % 4
                ky += 1
                S_.op("pe", [self.rG, self.rc], [self.psr[pb]], "transpose", out=self.ps[pb][0:NEXP, 0:128],
                      in_=self.Gall[:, tg, :], identity=self.identf[:])
                S_.op("act", [self.psr[pb]], [rGT], "copy", out=GT[:, tt, :], in_=self.ps[pb][0:NEXP, 0:128])
                for half in range(2):
                    pb = 4 + ky % 4
                    ky += 1
                    S_.op("pe", [rGT, rB], [self.psr[pb]], "matmul", self.ps[pb][:, :], lhsT=GT[:, tt, :],
                          rhs=bdn[:, half * 512:(half + 1) * 512], start=True, stop=True)
                    S_.op("act", [self.psr[pb]], [ry[tt]], "copy", out=yacc[:, tt, half * 512:(half + 1) * 512],
                          in_=self.ps[pb][:, :])
            for e in range(NEXP):
                wb = kw % 2
                kw += 1
                more = not (ts == S // TS - 1 and e == NEXP - 1)
                if more:
                    load_wg((e + 1) % NEXP, (wb + 1) % 2)
                wv = wgu[wb][:].rearrange("p c (j two) -> p c two j", two=2)
                for ch in range(2):
                    ab = (kq // 8) % 2
                    for jc in range(8):
                        q2 = kq % 2
                        pg, pl = (0, 1) if (kq % 2 == 0) else (2, 3)
                        kq += 1
                        for two, pp in ((0, pg), (1, pl)):
                            for c in range(8):
                                S_.op("pe", [rwg[wb], rh2], [self.psr[pp]], "matmul", self.ps[pp][:, :],
                                      lhsT=wv[:, c, two, jc * 128:(jc + 1) * 128], rhs=h2[:, c, ch * 512:(ch + 1) * 512],
                                      start=(c == 0), stop=(c == 7))
                        S_.op("dve", [self.psr[pg], rB], [rxg[q2]], "tensor_scalar", out=xg[q2][:], in0=self.ps[pg][:, :],
                              scalar1=bguv[:, e, jc, 0:1], scalar2=7.0, op0=ALU.add, op1=ALU.min)
                        S_.op("act", [rxg[q2]], [rsg[q2]], "activation", out=sg[q2][:], in_=xg[q2][:], func=AF.Sigmoid,
                              scale=1.702)
                        S_.op("dve", [self.psr[pl], rB], [rxl[q2]], "tensor_scalar", out=xl[q2][:], in0=self.ps[pl][:, :],
                              scalar1=bguv[:, e, jc, 1:2], scalar2=7.0, op0=ALU.add, op1=ALU.min)
                        S_.op("dve", [rxl[q2]], [rxl[q2]], "tensor_scalar", out=xl[q2][:], in0=xl[q2][:], scalar1=-7.0,
                              scalar2=1.0, op0=ALU.max, op1=ALU.add)
                        S_.op("pool", [rxg[q2], rsg[q2]], [rxg[q2]], "tensor_tensor", out=xg[q2][:], in0=xg[q2][:],
                              in1=sg[q2][:], op=ALU.mult)
                        S_.op("dve", [rxg[q2], rxl[q2]], [ract[ab]], "tensor_tensor", out=actT[ab][:, jc, :], in0=xg[q2][:],
                              in1=xl[q2][:], op=ALU.mult)
                    if pend is not None:
                        pend()
                    if ch == 0 and more:
                        load_wd((e + 1) % NEXP, (wb + 1) % 2)

                    def down(e=e, ch=ch, ab=ab, wb=wb, ts=ts):
                        nonlocal ky
                        for t4 in range(4):
                            tt = ch * 4 + t4
                            for half in range(2):
                                pb = 4 + ky % 4
                                ky += 1
                                for jc in range(8):
                                    S_.op("pe", [ract[ab], rwd[wb]], [self.psr[pb]], "matmul", self.ps[pb][:, :],
                                          lhsT=actT[ab][:, jc, t4 * 128:(t4 + 1) * 128],
                                          rhs=wdn[wb][:, jc, half * 512:(half + 1) * 512], start=(jc == 0), stop=(jc == 7))
                                ysl = yacc[:, tt, half * 512:(half + 1) * 512]
                                S_.op("dve", [self.psr[pb], self.rG], [ry[tt]], "scalar_tensor_tensor", out=ysl,
                                      in0=self.ps[pb][:, :], scalar=self.Gall[:, ts * 8 + tt, e:e + 1], in1=ysl,
                                      op0=ALU.mult, op1=ALU.add)
                    pend = down
            pend()
            pend = None
            S_.dma("sp", ry, [], self.scr["ys"][ts * TS:(ts + 1) * TS, :].rearrange("(t p) d -> p t d", p=128), yacc[:])
        S_.barrier()


def _ln2(self, xin, xout):
    S_ = self.S_
    l = self.l
    with ExitStack() as es:
        rB = Res()
        g2b = self.bload(es, "g2b", self.scr["mod"][l:l + 1, 5 * D:6 * D], rB)
        l2g = self.bload(es, "l2g", self.I["ln2_g"][l:l + 1, :], rB)
        l2b = self.bload(es, "l2b", self.I["ln2_b"][l:l + 1, :], rB)
        xt = [self.sb(es, f"fx{i}", [128, D], F32) for i in range(2)]
        yt = [self.sb(es, f"fy{i}", [128, D], F32) for i in range(2)]
        ot = [self.sb(es, f"fo{i}", [128, D], F32) for i in range(2)]
        st = [self.sb(es, f"fs{i}", [128, 16], F32) for i in range(2)]
        rx, ry, ro, rs = [Res(), Res()], [Res(), Res()], [Res(), Res()], [Res(), Res()]
        for t in range(NT):
            b = t % 2
            rows = slice(t * 128, (t + 1) * 128)
            S_.dma("sp", [], [rx[b]], xt[b][:], xin[rows, :])
            S_.dma("sp", [], [ry[b]], yt[b][:], self.scr["ys"][rows, :])
            S_.op("pool", [ry[b], rB], [ry[b]], "tensor_tensor", out=yt[b][:], in0=yt[b][:], in1=g2b[:], op=ALU.mult)
            S_.op("dve", [rx[b], ry[b]], [ry[b]], "scalar_tensor_tensor", out=yt[b][:], in0=xt[b][:], scalar=ALPHA,
                  in1=yt[b][:], op0=ALU.mult, op1=ALU.add)
            self.ln_tile(yt[b], ry[b], None, None, st[b], rs[b])
            S_.op("act", [ry[b], rs[b]], [ry[b]], "activation", out=yt[b][:], in_=yt[b][:], func=AF.Identity,
                  bias=st[b][:, 1:2], scale=st[b][:, 0:1])
            S_.op("pool", [ry[b], rB], [ry[b]], "tensor_tensor", out=yt[b][:], in0=yt[b][:], in1=l2g[:], op=ALU.mult)
            S_.op("dve", [ry[b], rB], [ro[b]], "tensor_tensor", out=ot[b][:], in0=yt[b][:], in1=l2b[:], op=ALU.add)
            S_.dma("pool", [ro[b]], [], xout[rows, :], ot[b][:])
        S_.barrier()


Prog.phase_moe = _moe
Prog.phase_ln2 = _ln2


_CACHE = {}


def kernel(**inputs):
    consts = make_consts()
    if "nc" not in _CACHE:
        _CACHE["nc"] = Prog(consts, debug=False).build()
    nc = _CACHE["nc"]
    shared = host_shared(inputs)
    in_maps = [host_inputs(inputs, consts, b, shared) for b in range(NCORES)]
    res = run_bass_kernel_spmd(nc, in_maps, core_ids=list(range(NCORES)))
    out = np.stack([np.asarray(r["out"], dtype=np.float32) for r in res.results], axis=0)
    return out


def _route(self):
    S_ = self.S_
    C0 = 40000.0
    with ExitStack() as es:
        tri = self.sb(es, "tri", [128, 128], BF16)
        ones = self.sb(es, "ones", [128, 128], BF16)
        pidx = self.sb(es, "pidx", [128, 1], F32)
        Sb = self.sb(es, "Sb", [128, NT, NEXP], BF16)
        Rk = self.sb(es, "Rk", [128, NT, NEXP], F32)
        cnt = self.sb(es, "cnt", [128, NEXP], F32)
        nb = self.sb(es, "nb", [128, NEXP], F32)
        pad = self.sb(es, "pad", [128, NEXP], F32)
        pst = self.sb(es, "pst", [128, NEXP], F32)
        a = [self.sb(es, f"sc{i}", [128, NEXP], F32) for i in range(2)]
        m8 = self.sb(es, "rm8", [128, NT, 8], F32)
        d4f = self.sb(es, "d4f", [128, NT, 4], F32)
        tmp = [self.sb(es, f"rtmp{i}", [128, NEXP], F32) for i in range(2)]
        ebf = self.sb(es, "ebf", [128, NBLK], F32)
        rc_, rSb, rRk, rcnt, rsc, rm8, rtmp, reb = Res(), Res(), Res(), Res(), Res(), Res(), [Res(), Res()], Res()
        S_.dma("sp", [], [rc_], tri[:], self.C["tri"])
        S_.dma("sp", [], [rc_], pidx[:], self.C["pidx"])
        S_.op("pool", [], [rc_], "memset", ones[:], 1.0)
        S_.op("dve", [self.rS], [rSb], "tensor_copy", out=Sb[:], in_=self.Sall[:])
        for tt in range(NT):
            pb = tt % 2
            S_.op("pe", [rSb, rc_], [self.psr[pb]], "matmul", self.ps[pb][:, 0:NEXP], lhsT=tri[:], rhs=Sb[:, tt, :],
                  start=True, stop=(tt == 0))
            for t2 in range(tt):
                S_.op("pe", [rSb, rc_], [self.psr[pb]], "matmul", self.ps[pb][:, 0:NEXP], lhsT=ones[:], rhs=Sb[:, t2, :],
                      start=False, stop=(t2 == tt - 1))
            S_.op("act", [self.psr[pb]], [rRk], "copy", out=Rk[:, tt, :], in_=self.ps[pb][:, 0:NEXP])
        for tt in range(NT):
            S_.op("pe", [rSb, rc_], [self.psr[2]], "matmul", self.ps[2][:, 0:NEXP], lhsT=ones[:], rhs=Sb[:, tt, :],
                  start=(tt == 0), stop=(tt == NT - 1))
        S_.op("act", [self.psr[2]], [rcnt], "copy", out=cnt[:], in_=self.ps[2][:, 0:NEXP])
        S_.op("dve", [rcnt], [rsc], "tensor_scalar", out=nb[:], in0=cnt[:], scalar1=0.0, scalar2=None, op0=ALU.is_gt)
        for j in range(1, S // RB):
            S_.op("dve", [rcnt, rsc], [rsc], "scalar_tensor_tensor", out=nb[:], in0=cnt[:], scalar=float(RB * j), in1=nb[:],
                  op0=ALU.is_gt, op1=ALU.add)
        S_.op("dve", [rsc], [rsc], "tensor_scalar", out=pad[:], in0=nb[:], scalar1=float(RB), scalar2=None, op0=ALU.mult)
        S_.op("dve", [rsc], [rsc], "tensor_copy", out=a[0][:], in_=pad[:])
        cur = 0
        for sft in (1, 2, 4, 8, 16):
            nx = 1 - cur
            S_.op("dve", [rsc], [rsc], "tensor_copy", out=a[nx][:, 0:sft], in_=a[cur][:, 0:sft])
            S_.op("dve", [rsc], [rsc], "tensor_tensor", out=a[nx][:, sft:NEXP], in0=a[cur][:, sft:NEXP],
                  in1=a[cur][:, 0:NEXP - sft], op=ALU.add)
            cur = nx
        pend = a[cur]
        S_.op("dve", [rsc], [rsc], "tensor_tensor", out=pst[:], in0=pend[:], in1=pad[:], op=ALU.subtract)
        for tt in range(NT):
            S_.op("dve", [rRk, rsc], [rRk], "tensor_tensor", out=Rk[:, tt, :], in0=Rk[:, tt, :], in1=pst[:], op=ALU.add)
        Rf = Rk[:].rearrange("p t e -> p (t e)")
        Sf = self.Sall[:].rearrange("p t e -> p (t e)")
        S_.op("dve", [rRk], [rRk], "tensor_scalar", out=Rf, in0=Rf, scalar1=-1.0, scalar2=C0 + 1.0, op0=ALU.mult, op1=ALU.add)
        S_.op("dve", [rRk, self.rS], [rRk], "tensor_tensor", out=Rf, in0=Rf, in1=Sf, op=ALU.mult)
        S_.op("dve", [rRk], [rRk], "tensor_scalar", out=Rf, in0=Rf, scalar1=-1.0, scalar2=None, op0=ALU.add)
        for tt in range(NT):
            S_.op("dve", [rRk], [rm8], "max", out=m8[:, tt, :], in_=Rk[:, tt, :])
        S_.op("dve", [rm8], [rm8], "tensor_scalar", out=d4f[:], in0=m8[:, :, 0:4], scalar1=-1.0, scalar2=C0, op0=ALU.mult,
              op1=ALU.add)
        S_.op("dve", [rm8], [self.rDi], "tensor_copy", out=self.Di[:], in_=d4f[:])
        kk = 0
        for tt in range(NT):
            for k in range(4):
                i = kk % 2
                kk += 1
                S_.op("dve", [rRk, rm8, self.rG], [rtmp[i]], "scalar_tensor_tensor", out=tmp[i][:], in0=Rk[:, tt, :],
                      scalar=m8[:, tt, k:k + 1], in1=self.Gall[:, tt, :], op0=ALU.is_equal, op1=ALU.mult)
                S_.op("dve", [rtmp[i]], [self.rg4], "tensor_reduce", out=self.g4[:, tt, k:k + 1], in_=tmp[i][:], axis=AX.X,
                      op=ALU.add)
        for b in range(NBLK):
            i = kk % 2
            kk += 1
            S_.op("dve", [rsc], [rtmp[i]], "tensor_scalar", out=tmp[i][:], in0=pend[:], scalar1=float(RB * b), scalar2=None,
                  op0=ALU.is_le)
            S_.op("dve", [rtmp[i]], [reb], "tensor_reduce", out=ebf[:, b:b + 1], in_=tmp[i][:], axis=AX.X, op=ALU.add)
        S_.op("dve", [reb], [reb], "tensor_scalar", out=ebf[:], in0=ebf[:], scalar1=float(NEXP - 1), scalar2=128.0,
              op0=ALU.min, op1=ALU.mult)
        S_.op("dve", [reb, rc_], [reb], "tensor_scalar", out=ebf[:], in0=ebf[:], scalar1=pidx[:, 0:1],
              scalar2=float(self.l * NEXP * 128), op0=ALU.add, op1=ALU.add)
        S_.op("dve", [reb], [self.rIw], "tensor_copy", out=self.Iw[:], in_=ebf[:])
        ht = [self.sb(es, f"ht{i}", [128, D], BF16) for i in range(3)]
        rht = [Res() for _ in range(3)]
        for tt in range(NT):
            i = tt % 3
            S_.dma("sp", [], [rht[i]], ht[i][:], self.scr["h2"][tt * 128:(tt + 1) * 128, :])
            for k in range(4):
                S_.idma([rht[i], self.rDi], [], out=self.scr["xs2"][:, :],
                        out_offset=bass.IndirectOffsetOnAxis(ap=self.Di[:, tt, k:k + 1], axis=0), in_=ht[i][:, :],
                        in_offset=None)
        S_.barrier()


def _ffn(self):
    S_ = self.S_
    l = self.l
    wtab = self.I["w_gu_l"].rearrange("l r c -> (l r) c")
    dtab = self.I["w_dn_l"].rearrange("l r c -> (l r) c")
    btab = self.I["b_gu_l"].rearrange("l r c -> (l r) c")
    with ExitStack() as es:
        wgu = [self.sb(es, f"gwgu{i}", [128, 8 * 2 * D], BF16) for i in range(2)]
        wdn = [self.sb(es, f"gwdn{i}", [128, 8 * D], BF16) for i in range(2)]
        bgb = [self.sb(es, f"bgb{i}", [128, 16], F32) for i in range(2)]
        rw = [Res(), Res()]
        rwd = [Res(), Res()]
        xtok = [self.sb(es, f"xtok{i}", [128, 4, D], BF16) for i in range(2)]
        rxt = [Res(), Res()]
        hT = [self.sb(es, f"ghT{i}", [128, 8, RB], BF16) for i in range(2)]
        rhT = [Res(), Res()]
        actT = [self.sb(es, f"gact{i}", [128, 8, RB], BF16) for i in range(2)]
        ract = [Res(), Res()]
        xg = [self.sb(es, f"gxg{i}", [128, 512], F32) for i in range(2)]
        sg = [self.sb(es, f"gsg{i}", [128, 512], F32) for i in range(2)]
        xl = [self.sb(es, f"gxl{i}", [128, 512], F32) for i in range(2)]
        rxg, rsg, rxl = [Res(), Res()], [Res(), Res()], [Res(), Res()]
        yb = [self.sb(es, f"gyb{i}", [128, D], F32) for i in range(4)]
        ryb = [Res() for _ in range(4)]

        def load_w(b):
            wb = b % 2
            ix = bass.IndirectOffsetOnAxis(ap=self.Iw[:, b:b + 1], axis=0)
            S_.idma([self.rIw], [rw[wb]], out=wgu[wb][:, :], out_offset=None, in_=wtab[:, :], in_offset=ix)
            S_.idma([self.rIw], [rw[wb]], out=bgb[wb][:, :], out_offset=None, in_=btab[:, :], in_offset=ix)

        def load_wd(b):
            wb = b % 2
            ix = bass.IndirectOffsetOnAxis(ap=self.Iw[:, b:b + 1], axis=0)
            S_.idma([self.rIw], [rwd[wb]], out=wdn[wb][:, :], out_offset=None, in_=dtab[:, :], in_offset=ix)

        def load_x(b):
            S_.dma("sp", [], [rxt[b % 2]], xtok[b % 2][:],
                   self.scr["xs2"][b * RB:(b + 1) * RB, :].rearrange("(t p) d -> p t d", p=128))
        kq = 0
        ky = 0
        pend = None
        fin = None
        load_w(0)
        load_wd(0)
        load_x(0)
        for b in range(NBLK):
            wb = b % 2
            if b + 1 < NBLK:
                load_w(b + 1)
                load_x(b + 1)
            for t4 in range(4):
                pT = 6 + (t4 % 2)
                pst = self.ps[pT][:].bitcast(BF16)
                for c in range(8):
                    S_.op("pe", [rxt[wb], self.rc], [self.psr[pT]], "transpose", out=pst[:, c * 128:(c + 1) * 128],
                          in_=xtok[wb][:, t4, c * 128:(c + 1) * 128], identity=self.identb[:])
                S_.op("act", [self.psr[pT]], [rhT[wb]], "copy", out=hT[wb][:, :, t4 * 128:(t4 + 1) * 128],
                      in_=pst.rearrange("p (c n) -> p c n", c=8))
            wv = wgu[wb][:].rearrange("p (c j two) -> p c two j", c=8, two=2)
            for jc in range(8):
                q2 = kq % 2
                pg, pl = (0, 1) if (kq % 2 == 0) else (2, 3)
                kq += 1
                for two, pp in ((0, pg), (1, pl)):
                    for c in range(8):
                        S_.op("pe", [rw[wb], rhT[wb]], [self.psr[pp]], "matmul", self.ps[pp][:, :],
                              lhsT=wv[:, c, two, jc * 128:(jc + 1) * 128], rhs=hT[wb][:, c, :], start=(c == 0), stop=(c == 7))
                S_.op("dve", [self.psr[pg], rw[wb]], [rxg[q2]], "tensor_scalar", out=xg[q2][:], in0=self.ps[pg][:, :],
                      scalar1=bgb[wb][:, 2 * jc:2 * jc + 1], scalar2=7.0, op0=ALU.add, op1=ALU.min)
                S_.op("act", [rxg[q2]], [rsg[q2]], "activation", out=sg[q2][:], in_=xg[q2][:], func=AF.Sigmoid, scale=1.702)
                S_.op("dve", [self.psr[pl], rw[wb]], [rxl[q2]], "tensor_scalar", out=xl[q2][:], in0=self.ps[pl][:, :],
                      scalar1=bgb[wb][:, 2 * jc + 1:2 * jc + 2], scalar2=7.0, op0=ALU.add, op1=ALU.min)
                S_.op("dve", [rxl[q2]], [rxl[q2]], "tensor_scalar", out=xl[q2][:], in0=xl[q2][:], scalar1=-7.0, scalar2=1.0,
                      op0=ALU.max, op1=ALU.add)
                if fin is not None:
                    fin()

                def fin(q2=q2, jc=jc, wb=wb):
                    S_.op("dve", [rxg[q2], rsg[q2]], [rxg[q2]], "tensor_tensor", out=xg[q2][:], in0=xg[q2][:], in1=sg[q2][:],
                          op=ALU.mult)
                    S_.op("dve", [rxg[q2], rxl[q2]], [ract[wb]], "tensor_tensor", out=actT[wb][:, jc, :], in0=xg[q2][:],
                          in1=xl[q2][:], op=ALU.mult)
            fin()
            fin = None
            if pend is not None:
                pend()
            if b + 1 < NBLK:
                load_wd(b + 1)

            def down(b=b, wb=wb):
                nonlocal ky
                wd = wdn[wb][:].rearrange("p (c n) -> p c n", c=8)
                for t4 in range(4):
                    yi = ky % 4
                    ky += 1
                    for half in range(2):
                        pb = 4 + half
                        for jc in range(8):
                            S_.op("pe", [ract[wb], rwd[wb]], [self.psr[pb]], "matmul", self.ps[pb][:, :],
                                  lhsT=actT[wb][:, jc, t4 * 128:(t4 + 1) * 128], rhs=wd[:, jc, half * 512:(half + 1) * 512],
                                  start=(jc == 0), stop=(jc == 7))
                        S_.op("act", [self.psr[pb]], [ryb[yi]], "copy", out=yb[yi][:, half * 512:(half + 1) * 512],
                              in_=self.ps[pb][:, :])
                    r0 = b * RB + t4 * 128
                    S_.dma("sp", [ryb[yi]], [], self.scr["ys2"][r0:r0 + 128, :], yb[yi][:])
            pend = down
        pend()
        S_.barrier()


def _ln2g(self, xin, xout):
    S_ = self.S_
    l = self.l
    with ExitStack() as es:
        rB = Res()
        g2b = self.bload(es, "g2b", self.scr["mod"][l:l + 1, 5 * D:6 * D], rB)
        l2g = self.bload(es, "l2g", self.I["ln2_g"][l:l + 1, :], rB)
        l2b = self.bload(es, "l2b", self.I["ln2_b"][l:l + 1, :], rB)
        bdn = self.sb(es, "bdn", [NEXP, D], F32)
        S_.dma("sp", [], [rB], bdn[:], self.I["b_down"][l])
        GT = [self.sb(es, f"cGT{i}", [NEXP, 128], F32) for i in range(2)]
        xt = [self.sb(es, f"fx{i}", [128, D], F32) for i in range(2)]
        yt = [self.sb(es, f"fy{i}", [128, D], F32) for i in range(2)]
        ot = [self.sb(es, f"fo{i}", [128, D], F32) for i in range(2)]
        yg = [[self.sb(es, f"yg{i}{k}", [128, D], F32) for k in range(4)] for i in range(2)]
        st = [self.sb(es, f"fs{i}", [128, 16], F32) for i in range(2)]
        rx, ry, ro, rs, rGT = [Res(), Res()], [Res(), Res()], [Res(), Res()], [Res(), Res()], [Res(), Res()]
        ryg = [[Res() for _ in range(4)] for _ in range(2)]
        def tile_body(t):
            b = t % 2
            rows = slice(t * 128, (t + 1) * 128)
            S_.dma("sp", [], [rx[b]], xt[b][:], xin[rows, :])
            for k in range(4):
                S_.idma([self.rDi], [ryg[b][k]], out=yg[b][k][:, :], out_offset=None, in_=self.scr["ys2"][:, :],
                        in_offset=bass.IndirectOffsetOnAxis(ap=self.Di[:, t, k:k + 1], axis=0))
            pT = 6 + b
            S_.op("pe", [self.rG, self.rc], [self.psr[pT]], "transpose", out=self.ps[pT][0:NEXP, 0:128],
                  in_=self.Gall[:, t, :], identity=self.identf[:])
            S_.op("act", [self.psr[pT]], [rGT[b]], "copy", out=GT[b][:], in_=self.ps[pT][0:NEXP, 0:128])
            for half in range(2):
                pb = 2 * b + half
                hs_ = slice(half * 512, (half + 1) * 512)
                S_.op("pe", [rGT[b], rB], [self.psr[pb]], "matmul", self.ps[pb][:, :], lhsT=GT[b][:], rhs=bdn[:, hs_],
                      start=True, stop=True)
                S_.op("dve", [self.psr[pb], ryg[b][0], self.rg4], [ry[b]], "scalar_tensor_tensor", out=yt[b][:, hs_],
                      in0=yg[b][0][:, hs_], scalar=self.g4[:, t, 0:1], in1=self.ps[pb][:, :], op0=ALU.mult, op1=ALU.add)
            yield
            for k in range(1, 4):
                S_.op("dve", [ry[b], ryg[b][k], self.rg4], [ry[b]], "scalar_tensor_tensor", out=yt[b][:], in0=yg[b][k][:],
                      scalar=self.g4[:, t, k:k + 1], in1=yt[b][:], op0=ALU.mult, op1=ALU.add)
            S_.op("pool", [ry[b], rB], [ry[b]], "tensor_tensor", out=yt[b][:], in0=yt[b][:], in1=g2b[:], op=ALU.mult)
            yield
            S_.op("dve", [rx[b], ry[b]], [ry[b]], "scalar_tensor_tensor", out=yt[b][:], in0=xt[b][:], scalar=ALPHA,
                  in1=yt[b][:], op0=ALU.mult, op1=ALU.add)
            self.ln_tile(yt[b], ry[b], None, None, st[b], rs[b], split=True)
            yield
            self.ln_tile2(st[b], rs[b])
            S_.op("act", [ry[b], rs[b]], [ry[b]], "activation", out=yt[b][:], in_=yt[b][:], func=AF.Identity,
                  bias=st[b][:, 1:2], scale=st[b][:, 0:1])
            S_.op("pool", [ry[b], rB], [ry[b]], "tensor_tensor", out=yt[b][:], in0=yt[b][:], in1=l2g[:], op=ALU.mult)
            yield
            S_.op("dve", [ry[b], rB], [ro[b]], "tensor_tensor", out=ot[b][:], in0=yt[b][:], in1=l2b[:], op=ALU.add)
            S_.dma("sp", [ro[b]], [], xout[rows, :], ot[b][:])
        round_robin((tile_body(t) for t in range(NT)), 2)
        S_.barrier()


Prog.phase_route = _route
Prog.phase_ffn = _ffn
Prog.phase_ln2 = _ln2g
```

```python
import numpy as np
import ml_dtypes
from contextlib import ExitStack
import concourse.bass as bass
import concourse.mybir as mybir
from concourse.bass_utils import run_bass_kernel_spmd

F32 = mybir.dt.float32
BF16 = mybir.dt.bfloat16
I32 = mybir.dt.int32
AF = mybir.ActivationFunctionType
ALU = mybir.AluOpType
AX = mybir.AxisListType

D = 1024
S = 4096
NT = S // 128
DEPTH = 2
NCORES = 8
LN_EPS = 1e-5
ALPHA = (2 * DEPTH) ** 0.25
NEG = -30000.0
BIG = 1.0e30
SCALE = 64 ** -0.5
NEXP = 32
RB = 512
NBLK = 64
C_AQ, C_AK, C_AV, C_BQ, C_BKC, C_BVC, C_BKS, C_BVS, C_BKW, C_BVW, C_BG, C_CQ, C_CK, C_CV, C_MG = (
    0, 256, 512, 768, 1152, 1280, 1408, 1536, 1664, 1792, 1920, 1938, 2322, 2706, 3090)
IN_W = 6162
FB_AQ, FB_AK, FB_BQ, FB_KC, FB_KS, FB_KW, FB_CQ, FB_CK, FB_VC = 0, 2, 4, 7, 8, 9, 10, 13, 16
NFB = 17


class Res:
    __slots__ = ("lw", "rd")

    def __init__(self):
        self.lw = None
        self.rd = {}


class Sched:
    def __init__(self, nc, es):
        self.nc = nc
        self.eng = {"pe": nc.tensor, "act": nc.scalar, "dve": nc.vector, "pool": nc.gpsimd, "sp": nc.sync}
        self.sem = {k: es.enter_context(nc.semaphore("prog_" + k)) for k in self.eng}
        self.cnt = {k: 0 for k in self.eng}
        self.seen = {k: {} for k in self.eng}
        self.own = {self.sem[k]: k for k in self.eng}
        self.dq = {}
        for q, n in (("sp", 20), ("pool", 12), ("act", 4)):
            self.dq[q] = {"sems": [es.enter_context(nc.semaphore(f"dma_{q}_{i}")) for i in range(n)],
                          "val": [0] * n, "i": 0}
        self.n_ins = 0

    def _wait(self, e, tok, raw):
        sem, val = tok
        if self.seen[e].get(sem, 0) >= val:
            return
        o = self.own.get(sem)
        if o == e:
            if e == "pe" or not raw or self.cnt[e] - val >= 2:
                return
        self.eng[e].wait_ge(sem, val)
        self.seen[e][sem] = val
        self.n_ins += 1

    def _deps(self, e, R, W):
        for r in R:
            if r.lw is not None:
                self._wait(e, r.lw, True)
        for w in W:
            if w.lw is not None:
                self._wait(e, w.lw, False)
            for sem, val in w.rd.items():
                self._wait(e, (sem, val), False)

    def _commit(self, tok, R, W):
        sem, val = tok
        for r in R:
            if r.rd.get(sem, 0) < val:
                r.rd[sem] = val
        for w in W:
            w.lw = tok
            w.rd = {}

    def op(self, e, R, W, name, *a, **k):
        self._deps(e, R, W)
        ins = getattr(self.eng[e], name)(*a, **k)
        self.cnt[e] += 1
        ins.then_inc(self.sem[e], 1)
        self._commit((self.sem[e], self.cnt[e]), R, W)
        self.n_ins += 1
        return ins

    def dma(self, q, R, W, out, in_, **k):
        d = self.dq[q]
        i = d["i"] % len(d["sems"])
        d["i"] += 1
        sem = d["sems"][i]
        if d["val"][i] > 0:
            self._wait(q, (sem, d["val"][i]), False)
        self._deps(q, R, W)
        ins = self.eng[q].dma_start(out=out, in_=in_, **k)
        ins.then_inc(sem, 16)
        d["val"][i] += 16
        self._commit((sem, d["val"][i]), R, W)
        self.n_ins += 1

    def idma(self, R, W, **k):
        q = "pool"
        d = self.dq[q]
        i = d["i"] % len(d["sems"])
        d["i"] += 1
        sem = d["sems"][i]
        if d["val"][i] > 0:
            self._wait(q, (sem, d["val"][i]), False)
        self._deps(q, R, W)
        ins = self.eng[q].indirect_dma_start(**k)
        ins.then_inc(sem, 16)
        d["val"][i] += 16
        self._commit((sem, d["val"][i]), R, W)
        self.n_ins += 1

    def barrier(self):
        toks = [(self.sem[f], self.cnt[f]) for f in self.eng if self.cnt[f] > 0]
        for q in self.dq.values():
            for s_, v in zip(q["sems"], q["val"]):
                if v > 0:
                    toks.append((s_, v))
        for e in self.eng:
            for (sem, val) in toks:
                if self.own.get(sem) == e:
                    continue
                if self.seen[e].get(sem, 0) >= val:
                    continue
                self.eng[e].wait_ge(sem, val)
                self.seen[e][sem] = val
                self.n_ins += 1


def round_robin(gens, width):
    it = iter(gens)
    active = []
    more = True
    while True:
        while more and len(active) < width:
            try:
                active.append(next(it))
            except StopIteration:
                more = False
        if not active:
            break
        for g in list(active):
            try:
                next(g)
            except StopIteration:
                active.remove(g)


def bf16_np(a):
    return np.asarray(a, np.float32).astype(ml_dtypes.bfloat16)


def make_consts():
    c = {}
    c["identb"] = bf16_np(np.eye(128))
    c["identf"] = np.eye(128, dtype=np.float32)
    k = np.arange(128)[:, None]
    q = np.arange(128)[None, :]
    masks = np.zeros((128, 3, 128), np.float32)
    masks[:, 0] = np.where(k <= q, 0.0, NEG)
    masks[:, 1] = np.where(k > q, 0.0, NEG)
    masks[:, 2] = np.where(k >= q, 0.0, NEG)
    c["masks"] = bf16_np(masks)
    e16 = np.zeros((16, 16, 128), np.float32)
    for n in range(16):
        e16[n, n, :] = 1.0
    c["esel16"] = bf16_np(e16)
    e64 = np.zeros((64, 32, 128), np.float32)
    for kt in range(32):
        e64[2 * kt, kt, :64] = 1.0
        e64[2 * kt + 1, kt, 64:] = 1.0
    c["esel64"] = bf16_np(e64)
    inv = (10000.0 ** (-np.arange(0, 64, 2, dtype=np.float32) / 64)).astype(np.float32)
    rp = np.zeros((128, 2), np.float32)
    for f in range(128):
        rp[f, 0] = inv[f % 32]
        rp[f, 1] = -1.0 if (f % 64) < 32 else 1.0
    c["ropec"] = rp
    n = (np.arange(2)[None, :, None] * 128 + np.arange(128)[:, None, None])
    t = np.arange(S)[None, None, :]
    c["cmpmask"] = bf16_np(np.where(16 * n + 31 <= t, 0.0, NEG))
    ov = np.zeros((128, 2, 64), np.float32)
    for nt in range(2):
        for kk in range(128):
            nn = nt * 128 + kk
            if nn >= 255:
                continue
            for m in range(64):
                if 16 * nn < 64 * m + 64 and 16 * nn + 32 > 64 * m:
                    ov[kk, nt, m] = 1.0
    c["overlap"] = bf16_np(ov)
    add = np.zeros((128, 32, 64), np.float32)
    mn = np.zeros((128, 32, 64), np.float32)
    for qt in range(32):
        for p in range(128):
            qb = 2 * qt + (1 if p >= 64 else 0)
            for m in range(64):
                forced = (m == 0) or (m == qb) or (m == qb - 1)
                add[p, qt, m] = 1.0e6 if forced else 0.0
                mn[p, qt, m] = BIG if m <= qb else -BIG
    c["nsa_add"] = add
    c["nsa_min"] = mn
    c["tri"] = bf16_np((np.arange(128)[:, None] < np.arange(128)[None, :]).astype(np.float32))
    c["pidx"] = np.arange(128, dtype=np.float32).reshape(128, 1)
    return c


WNAMES = ["w_ada", "b_ada", "w_in", "cmp_w1_k", "cmp_w2_k", "cmp_peT_k", "cmp_w1_v", "cmp_w2_v", "cmp_peT_v",
          "w_branch_a", "w_branch_b", "w_branch_c", "w_out", "ln1_g", "ln1_b", "router_w", "router_b",
          "w_gu_l", "b_gu_l", "w_dn_l", "b_down", "ln2_g", "ln2_b"]
WSHAPES = {
    "w_ada": [DEPTH, D, 6 * D], "b_ada": [DEPTH, 6 * D], "w_in": [DEPTH, D, IN_W],
    "cmp_w1_k": [DEPTH, 2048, 128], "cmp_w2_k": [DEPTH, 128, 64], "cmp_peT_k": [DEPTH, 64, 32],
    "cmp_w1_v": [DEPTH, 2048, 128], "cmp_w2_v": [DEPTH, 128, 64], "cmp_peT_v": [DEPTH, 64, 32],
    "w_branch_a": [DEPTH, 256, D], "w_branch_b": [DEPTH, 384, D], "w_branch_c": [DEPTH, 128, D],
    "w_out": [DEPTH, D, D], "ln1_g": [DEPTH, D], "ln1_b": [DEPTH, D], "router_w": [DEPTH, D, NEXP],
    "router_b": [DEPTH, NEXP], "w_gu_l": [DEPTH, NEXP * 128, 16 * D], "b_gu_l": [DEPTH, NEXP * 128, 16],
    "w_dn_l": [DEPTH, NEXP * 128, 8 * D], "b_down": [DEPTH, NEXP, D], "ln2_g": [DEPTH, D], "ln2_b": [DEPTH, D],
}
CONST_DT = {"identb": BF16, "identf": F32, "masks": BF16, "esel16": BF16, "esel64": BF16, "ropec": F32,
            "cmpmask": BF16, "overlap": BF16, "nsa_add": F32, "nsa_min": F32, "tri": BF16, "pidx": F32}


class Prog:
    def __init__(self, consts, debug=False, stop_after=None, nlayers=DEPTH):
        self.debug = debug
        self.stop_after = stop_after
        self.nlayers = nlayers
        nc = self.nc = bass.Bass("TRN2", target_bir_lowering=False)
        self.es = ExitStack()
        self.I = {}
        self.I["x"] = nc.dram_tensor("x", [S, D], F32, kind="ExternalInput").ap()
        self.I["cT"] = nc.dram_tensor("cT", [128, 8], F32, kind="ExternalInput").ap()
        self.I["pos"] = nc.dram_tensor("pos", [1, S], I32, kind="ExternalInput").ap()
        for n in WNAMES:
            self.I[n] = nc.dram_tensor(n, WSHAPES[n], F32, kind="ExternalInput").ap()
        self.C = {}
        for n, a in consts.items():
            self.C[n] = nc.dram_tensor("k_" + n, list(a.shape), CONST_DT[n], kind="ExternalInput").ap()
        self.out = nc.dram_tensor("out", [S, D], F32, kind="ExternalOutput").ap()
        sk = "ExternalOutput" if debug else "Internal"
        self.scr = {}

        def scr(name, shape, dt):
            self.scr[name] = nc.dram_tensor("s_" + name, shape, dt, kind=sk).ap()
        scr("rope", [2, 128, S], F32)
        scr("mod", [DEPTH, 6 * D], F32)
        scr("ft", [NFB, 128, S], BF16)
        scr("tv", [S, 896], BF16)
        scr("mg", [S, 3072], BF16)
        scr("o", [S, 768], BF16)
        scr("oc", [3, S, 130], F32)
        scr("xs", [2, S, D], F32)
        scr("h2t", [128, 8, S], BF16)
        scr("h2", [S, D], BF16)
        scr("xs2", [NBLK * RB, D], BF16)
        scr("ys2", [NBLK * RB, D], F32)
        if debug:
            scr("dbg1", [128, 256], BF16)
            scr("dbg2", [128, 2 * 2 * 129], BF16)

    def sb(self, es, name, shape, dt):
        self._uid = getattr(self, "_uid", 0) + 1
        return es.enter_context(self.nc.sbuf_tensor(f"{name}_{self._uid}", shape, dt))

    def build(self):
        nc = self.nc
        with self.es as es:
            self.S_ = S_ = Sched(nc, es)
            self.ps = [es.enter_context(nc.psum_tensor(f"ps{i}", [128, 512], F32)) for i in range(8)]
            self.psr = [Res() for _ in range(8)]
            self.identb = self.sb(es, "identb", [128, 128], BF16)
            self.identf = self.sb(es, "identf", [128, 128], F32)
            self.masks = self.sb(es, "masks", [128, 3, 128], BF16)
            self.Gall = self.sb(es, "Gall", [128, NT, NEXP], F32)
            self.BGs = self.sb(es, "BGs", [128, NT, 18], F32)
            self.rc = Res()
            self.epsc = self.sb(es, "epsc", [128, 1], F32)
            self.zeros = self.sb(es, "zeros", [128, 512], BF16)
            S_.op("dve", [], [self.rc], "memset", self.zeros[:], 0.0)
            S_.op("dve", [], [self.rc], "memset", self.epsc[:], LN_EPS)
            for n_, t_ in (("identb", self.identb), ("identf", self.identf), ("masks", self.masks)):
                S_.dma("sp", [], [self.rc], t_[:], self.C[n_])
            self.rG = Res()
            self.rBG = Res()
            self.Sall = self.sb(es, "Sall", [128, NT, NEXP], F32)
            self.Di = self.sb(es, "Di", [128, NT, 4], I32)
            self.g4 = self.sb(es, "g4", [128, NT, 4], F32)
            self.Iw = self.sb(es, "Iw", [128, NBLK], I32)
            self.rS, self.rDi, self.rg4, self.rIw = Res(), Res(), Res(), Res()
            self.phase_rope()
            S_.barrier()
            xin = self.I["x"]
            for l in range(self.nlayers):
                self.l = l
                for pn in ("phase_ada", "phase_ln1", "phase_proj", "phase_moba", "phase_nsa",
                           "phase_dil", "phase_merge", "phase_route", "phase_ffn", "phase_ln2"):
                    ph = getattr(self, pn)
                    if pn == "phase_ln1":
                        ph(xin)
                    elif pn == "phase_merge":
                        ph(xin, self.scr["xs"][0])
                    elif pn == "phase_ln2":
                        dst = self.out if l == self.nlayers - 1 else self.scr["xs"][1]
                        ph(self.scr["xs"][0], dst)
                    else:
                        ph()
                    S_.barrier()
                    if self.stop_after == (l, pn):
                        return nc
                xin = self.scr["xs"][1]
            S_.barrier()
        return nc

    def phase_rope(self):
        import math
        S_ = self.S_
        with ExitStack() as es:
            posb = self.sb(es, "posb", [128, S], I32)
            ang = self.sb(es, "ang", [128, S], F32)
            arg = self.sb(es, "arg", [128, S], F32)
            res_ = self.sb(es, "rres", [128, S], F32)
            rpc = self.sb(es, "rpc", [128, 2], F32)
            npi = self.sb(es, "npi", [128, 1], F32)
            r1, r2, r3, r4, r5 = Res(), Res(), Res(), Res(), Res()
            S_.dma("sp", [], [r1], posb[:], self.I["pos"].partition_broadcast(128))
            S_.dma("sp", [], [r5], rpc[:], self.C["ropec"])
            S_.op("dve", [], [r5], "memset", npi[:], -math.pi)
            S_.op("dve", [r1], [r2], "tensor_copy", out=ang[:], in_=posb[:])
            S_.op("dve", [r2, r5], [r2], "tensor_scalar", out=ang[:], in0=ang[:], scalar1=rpc[:, 0:1], scalar2=None,
                  op0=ALU.mult)
            ki = posb
            for i, off in enumerate((0.5 * math.pi, 0.0)):
                S_.op("dve", [r2], [r3], "tensor_scalar", out=arg[:], in0=ang[:], scalar1=off, scalar2=None,
                      op0=ALU.add)
                S_.op("dve", [r3], [r4], "tensor_scalar", out=res_[:], in0=arg[:], scalar1=1.0 / (2 * math.pi),
                      scalar2=None, op0=ALU.mult)
                S_.op("dve", [r4], [r1], "tensor_copy", out=ki[:], in_=res_[:])
                S_.op("dve", [r1], [r4], "tensor_copy", out=res_[:], in_=ki[:])
                S_.op("dve", [r4, r3], [r3], "scalar_tensor_tensor", out=arg[:], in0=res_[:], scalar=-2 * math.pi,
                      in1=arg[:], op0=ALU.mult, op1=ALU.add)
                S_.op("dve", [r3], [r4], "tensor_scalar", out=res_[:], in0=arg[:], scalar1=math.pi,
                      scalar2=-2 * math.pi, op0=ALU.is_gt, op1=ALU.mult)
                S_.op("dve", [r3, r4], [r3], "tensor_tensor", out=arg[:], in0=arg[:], in1=res_[:], op=ALU.add)
                S_.op("dve", [r3], [r3], "tensor_scalar", out=arg[:], in0=arg[:], scalar1=-math.pi, scalar2=math.pi,
                      op0=ALU.max, op1=ALU.min)
                S_.op("act", [r3], [r4], "activation", out=res_[:], in_=arg[:], func=AF.Sin)
                if i == 1:
                    S_.op("dve", [r4, r5], [r4], "tensor_scalar", out=res_[:], in0=res_[:], scalar1=rpc[:, 1:2],
                          scalar2=None, op0=ALU.mult)
                S_.dma("pool", [r4], [], self.scr["rope"][i], res_[:])
            S_.barrier()

    def phase_ada(self):
        S_ = self.S_
        l = self.l
        with ExitStack() as es:
            cs = self.sb(es, "cs", [128, 8], F32)
            brow = self.sb(es, "brow", [1, 6 * D], F32)
            mrow = self.sb(es, "mrow", [1, 6 * D], F32)
            wa = [self.sb(es, f"wa{i}", [128, 8, 512], F32) for i in range(2)]
            rcs, rb, rm = Res(), Res(), Res()
            rwa = [Res(), Res()]
            S_.dma("sp", [], [rcs], cs[:], self.I["cT"])
            S_.dma("sp", [], [rb], brow[:], self.I["b_ada"][l:l + 1, :])
            S_.op("act", [rcs], [rcs], "activation", out=cs[:], in_=cs[:], func=AF.Silu)
            for nb in range(12):
                w = wa[nb % 2]
                S_.dma("sp", [], [rwa[nb % 2]], w[:],
                       self.I["w_ada"][l][:, nb * 512:(nb + 1) * 512].rearrange("(c p) n -> p c n", p=128))
                pr = self.psr[nb % 2]
                for c in range(8):
                    S_.op("pe", [rcs, rwa[nb % 2]], [pr], "matmul", self.ps[nb % 2][0:1, :], lhsT=cs[:, c:c + 1],
                          rhs=w[:, c, :], start=(c == 0), stop=(c == 7))
                S_.op("dve", [pr, rb], [rm], "tensor_tensor", out=mrow[:, nb * 512:(nb + 1) * 512],
                      in0=self.ps[nb % 2][0:1, :], in1=brow[:, nb * 512:(nb + 1) * 512], op=ALU.add)
            for seg in (1, 4):
                S_.op("dve", [rm], [rm], "tensor_scalar", out=mrow[:, seg * D:(seg + 1) * D],
                      in0=mrow[:, seg * D:(seg + 1) * D], scalar1=1.0, scalar2=None, op0=ALU.add)
            S_.dma("pool", [rm], [], self.scr["mod"][l:l + 1, :], mrow[:])

    def bload(self, es, name, src_row, r):
        t = self.sb(es, name, [128, D], F32)
        self.S_.dma("sp", [], [r], t[:], src_row.partition_broadcast(128))
        return t

    def ln_tile(self, xt, rx, tmp, rtmp, stat, rstat, split=False):
        S_ = self.S_
        st6 = stat[:, 4:16].rearrange("p (a b) -> p a b", a=2)
        for a in range(2):
            S_.op("dve", [rx], [rstat], "bn_stats", out=st6[:, a, :], in_=xt[:, a * 512:(a + 1) * 512])
        S_.op("dve", [rstat], [rstat], "bn_aggr", out=stat[:, 2:4], in_=st6)
        S_.op("act", [rstat, self.rc], [rstat], "activation", out=stat[:, 0:1], in_=stat[:, 3:4], func=AF.Sqrt,
              bias=self.epsc[:, 0:1], scale=1.0)
        if split:
            return
        self.ln_tile2(stat, rstat)

    def ln_tile2(self, stat, rstat):
        S_ = self.S_
        S_.op("dve", [rstat], [rstat], "reciprocal", out=stat[:, 0:1], in_=stat[:, 0:1])
        S_.op("dve", [rstat], [rstat], "scalar_tensor_tensor", out=stat[:, 1:2], in0=stat[:, 2:3], scalar=-1.0,
              in1=stat[:, 0:1], op0=ALU.mult, op1=ALU.mult)

    def phase_ln1(self, xin):
        S_ = self.S_
        l = self.l
        es = self.es_h = ExitStack()
        self.hT = self.sb(es, "hT", [128, 8, S], BF16)
        self.rhT = [Res() for _ in range(NT)]
        with ExitStack() as e2:
            rmod = Res()
            scp = self.bload(e2, "scp", self.scr["mod"][l:l + 1, D:2 * D], rmod)
            shf = self.bload(e2, "shf", self.scr["mod"][l:l + 1, 0:D], rmod)
            xt = [self.sb(e2, f"xt{i}", [128, D], F32) for i in range(2)]
            xn = [self.sb(e2, f"xn{i}", [128, D], F32) for i in range(2)]
            hb = [self.sb(e2, f"hb{i}", [128, D], BF16) for i in range(2)]
            stt = [self.sb(e2, f"stt{i}", [128, 16], F32) for i in range(2)]
            rx, rxn, rhb, rst = [Res(), Res()], [Res(), Res()], [Res(), Res()], [Res(), Res()]
            for t in range(NT):
                b = t % 2
                S_.dma("sp", [], [rx[b]], xt[b][:], xin[t * 128:(t + 1) * 128, :])
                self.ln_tile(xt[b], rx[b], None, None, stt[b], rst[b])
                S_.op("act", [rx[b], rst[b]], [rxn[b]], "activation", out=xn[b][:], in_=xt[b][:], func=AF.Identity,
                      bias=stt[b][:, 1:2], scale=stt[b][:, 0:1])
                S_.op("pool", [rxn[b], rmod], [rxn[b]], "tensor_tensor", out=xn[b][:], in0=xn[b][:], in1=scp[:],
                      op=ALU.mult)
                S_.op("dve", [rxn[b], rmod], [rhb[b]], "tensor_tensor", out=hb[b][:], in0=xn[b][:], in1=shf[:],
                      op=ALU.add)
                pb = 6 + b
                pst = self.ps[pb][:].bitcast(BF16)
                for c in range(8):
                    S_.op("pe", [rhb[b], self.rc], [self.psr[pb]], "transpose", out=pst[:, c * 128:(c + 1) * 128],
                          in_=hb[b][:, c * 128:(c + 1) * 128], identity=self.identb[:])
                S_.op("act", [self.psr[pb]], [self.rhT[t]], "tensor_copy" if False else "copy",
                      out=self.hT[:, :, t * 128:(t + 1) * 128],
                      in_=pst.rearrange("p (c n) -> p c n", c=8))

    def phase_proj(self):
        S_ = self.S_
        l = self.l
        win = self.I["w_in"][l]

        def wsrc(c0, n):
            return win[:, c0:c0 + n].rearrange("(c p) n -> p c n", p=128)
        with ExitStack() as es:
            Wb = self.sb(es, "Wb", [128, 8, 3090], BF16)
            Wr = self.sb(es, "Wr", [128, 8, 2048], BF16)
            rW = Res()
            segs = [(0, C_AQ, 512), (896, C_BKC, 128), (1024, C_BKS, 128), (1152, C_BKW, 128), (1280, C_CQ, 768),
                    (2048, C_BVC, 128), (2176, C_AV, 256), (2432, C_BVS, 128), (2560, C_BVW, 128),
                    (2688, C_CV, 384), (3072, C_BG, 18)]
            for r in range(3):
                for g in range(2):
                    segs.append((512 + r * 128 + g * 64, C_BQ + (3 * g + r) * 64, 64))
            for (o, c0, n) in segs:
                S_.dma("pool", [], [rW], Wb[:, :, o:o + n], wsrc(c0, n))
            rWr = Res()
            for c in range(8):
                src = Wb[:, c, 0:2048].rearrange("p (h two d) -> p h two d", two=2, d=32)
                dst = Wr[:, c, :].rearrange("p (h two d) -> p h two d", two=2, d=32)
                S_.op("pool", [rW], [rWr], "tensor_copy", out=dst[:, :, 0, :], in_=src[:, :, 1, :])
                S_.op("pool", [rW], [rWr], "tensor_copy", out=dst[:, :, 1, :], in_=src[:, :, 0, :])
            cs = [self.sb(es, f"cosc{i}", [128, 2, 512], F32) for i in range(2)]
            rcs = [Res(), Res()]
            t1 = [self.sb(es, f"t1_{i}", [128, 512], F32) for i in range(2)]
            t2 = [self.sb(es, f"t2_{i}", [128, 512], F32) for i in range(2)]
            ob = [self.sb(es, f"ob{i}", [128, 512], BF16) for i in range(4)]
            rt1, rt2, rob = [Res(), Res()], [Res(), Res()], [Res() for _ in range(4)]
            k = 0
            for tc in range(8):
                cb = tc % 2
                S_.dma("sp", [], [rcs[cb]], cs[cb][:],
                       self.scr["rope"][:, :, tc * 512:(tc + 1) * 512].rearrange("a p n -> p a n"))
                rh = self.rhT[tc * 4:(tc + 1) * 4]
                for fb in range(NFB):
                    pa, pb = (0, 1) if k % 2 == 0 else (2, 3)
                    for c in range(8):
                        S_.op("pe", [rW] + rh, [self.psr[pa]], "matmul", self.ps[pa][:, :],
                              lhsT=Wb[:, c, fb * 128:(fb + 1) * 128], rhs=self.hT[:, c, tc * 512:(tc + 1) * 512],
                              start=(c == 0), stop=(c == 7))
                    o_ = k % 4
                    if fb < 16:
                        for c in range(8):
                            S_.op("pe", [rWr] + rh, [self.psr[pb]], "matmul", self.ps[pb][:, :],
                                  lhsT=Wr[:, c, fb * 128:(fb + 1) * 128], rhs=self.hT[:, c, tc * 512:(tc + 1) * 512],
                                  start=(c == 0), stop=(c == 7))
                        b2 = k % 2
                        S_.op("dve", [self.psr[pa], rcs[cb]], [rt1[b2]], "tensor_tensor", out=t1[b2][:],
                              in0=self.ps[pa][:, :], in1=cs[cb][:, 0, :], op=ALU.mult)
                        S_.op("dve", [self.psr[pb], rcs[cb]], [rt2[b2]], "tensor_tensor", out=t2[b2][:],
                              in0=self.ps[pb][:, :], in1=cs[cb][:, 1, :], op=ALU.mult)
                        S_.op("pool", [rt1[b2], rt2[b2]], [rob[o_]], "tensor_tensor", out=ob[o_][:], in0=t1[b2][:],
                              in1=t2[b2][:], op=ALU.add)
                    else:
                        S_.op("act", [self.psr[pa]], [rob[o_]], "copy", out=ob[o_][:], in_=self.ps[pa][:, :])
                    S_.dma("pool", [rob[o_]], [], self.scr["ft"][fb][:, tc * 512:(tc + 1) * 512], ob[o_][:])
                    k += 1
            tvb = [self.sb(es, f"tvb{i}", [128, 896], BF16) for i in range(2)]
            rtv = [Res(), Res()]
            for t in range(NT):
                b = t % 2
                pa, pb = (4, 5) if b == 0 else (6, 7)
                for (pp, c0, n) in ((pa, 2176, 512), (pb, 2688, 402)):
                    for c in range(8):
                        S_.op("pe", [rW, self.rhT[t]], [self.psr[pp]], "matmul", self.ps[pp][:, 0:n],
                              lhsT=self.hT[:, c, t * 128:(t + 1) * 128], rhs=Wb[:, c, c0:c0 + n],
                              start=(c == 0), stop=(c == 7))
                S_.op("act", [self.psr[pa]], [rtv[b]], "copy", out=tvb[b][:, 0:512], in_=self.ps[pa][:, :])
                S_.op("act", [self.psr[pb]], [rtv[b]], "copy", out=tvb[b][:, 512:896], in_=self.ps[pb][:, 0:384])
                S_.op("act", [self.psr[pb]], [self.rBG], "activation", out=self.BGs[:, t, :],
                      in_=self.ps[pb][:, 384:402], func=AF.Sigmoid)
                S_.dma("pool", [rtv[b]], [], self.scr["tv"][t * 128:(t + 1) * 128, :], tvb[b][:])
            S_.barrier()
        with ExitStack() as es:
            Wm = self.sb(es, "Wm", [128, 8, 3072], BF16)
            rW = Res()
            for j in range(6):
                S_.dma("pool", [], [rW], Wm[:, :, j * 512:(j + 1) * 512], wsrc(C_MG + j * 512, 512))
            mgb = [self.sb(es, f"mgb{i}", [128, 3072], BF16) for i in range(2)]
            rmg = [Res(), Res()]
            k = 0
            for t in range(NT):
                b = t % 2
                for j in range(6):
                    pp = k % 8
                    k += 1
                    for c in range(8):
                        S_.op("pe", [rW, self.rhT[t]], [self.psr[pp]], "matmul", self.ps[pp][:, :],
                              lhsT=self.hT[:, c, t * 128:(t + 1) * 128], rhs=Wm[:, c, j * 512:(j + 1) * 512],
                              start=(c == 0), stop=(c == 7))
                    S_.op("act", [self.psr[pp]], [rmg[b]], "activation", out=mgb[b][:, j * 512:(j + 1) * 512],
                          in_=self.ps[pp][:, :], func=AF.Sigmoid)
                S_.dma("pool", [rmg[b]], [], self.scr["mg"][t * 128:(t + 1) * 128, :], mgb[b][:])
            S_.barrier()
        self.es_h.close()


def host_shared(inp):
    w = np.asarray(inp["w_gate_up"], np.float32).reshape(DEPTH, NEXP, 8, 128, 2 * D)
    wg = np.ascontiguousarray(np.transpose(w, (0, 1, 3, 2, 4))).reshape(DEPTH, NEXP * 128, 16 * D)
    w = np.asarray(inp["w_down"], np.float32).reshape(DEPTH, NEXP, 8, 128, D)
    wd = np.ascontiguousarray(np.transpose(w, (0, 1, 3, 2, 4))).reshape(DEPTH, NEXP * 128, 8 * D)
    return {"w_gu_l": wg, "w_dn_l": wd}


def host_inputs(inp, consts, b, shared=None):
    if shared is None:
        shared = host_shared(inp)
    m = {"x": np.ascontiguousarray(inp["x"][b], dtype=np.float32),
         "cT": np.ascontiguousarray(np.asarray(inp["c"][b], np.float32).reshape(8, 128).T),
         "pos": np.ascontiguousarray(inp["positions"][b:b + 1]).astype(np.int32)}
    for n in WNAMES:
        if n == "cmp_peT_k":
            m[n] = np.ascontiguousarray(np.transpose(np.asarray(inp["cmp_pe_k"], np.float32), (0, 2, 1)))
        elif n == "cmp_peT_v":
            m[n] = np.ascontiguousarray(np.transpose(np.asarray(inp["cmp_pe_v"], np.float32), (0, 2, 1)))
        elif n == "b_gu_l":
            bg = np.asarray(inp["b_gate_up"], np.float32).reshape(DEPTH, NEXP, 8, 128, 2)
            m[n] = np.ascontiguousarray(np.transpose(bg, (0, 1, 3, 2, 4)).reshape(DEPTH, NEXP * 128, 16))
        elif n == "w_gu_l":
            m[n] = shared["w_gu_l"]
        elif n == "w_dn_l":
            m[n] = shared["w_dn_l"]
        else:
            m[n] = np.ascontiguousarray(inp[n], dtype=np.float32)
    for n, a in consts.items():
        m["k_" + n] = a
    return m


class AttnPipe:
    def __init__(self, prog, es, ncol, nbuf=3):
        self.p = prog
        self.S_ = prog.S_
        self.ncol = ncol
        self.G = 512 // ncol
        self.pt = [prog.sb(es, f"pt{i}", [128, 512], BF16) for i in range(nbuf)]
        self.rpt = [Res() for _ in range(nbuf)]
        self.k = 0
        self.pending = []
        self.depth = 2
        self.sbanks = (0, 1, 2)

    def qtile(self, items, qrhs, nh, obank, oregs, rq, post):
        p, S_ = self.p, self.S_
        ncol = self.ncol
        n = len(items)
        for g0 in range(0, n, self.G):
            grp = items[g0:g0 + self.G]
            bank = self.sbanks[self.k % 3]
            buf = self.k % len(self.pt)
            self.k += 1
            for j, (kT, v, extras, rds) in enumerate(grp):
                reg = p.ps[bank][:, j * ncol:(j + 1) * ncol]
                S_.op("pe", rq + rds, [p.psr[bank]], "matmul", reg, lhsT=kT, rhs=qrhs, start=True,
                      stop=(len(extras) == 0))
                for xi, (xl, xr) in enumerate(extras):
                    for hh in range(nh):
                        S_.op("pe", rq + rds, [p.psr[bank]], "matmul", reg[:, hh * 128:(hh + 1) * 128], lhsT=xl,
                              rhs=xr, start=False, stop=(xi == len(extras) - 1))
            if len(self.pending) >= self.depth:
                self.pending.pop(0)()
            w = len(grp) * ncol
            S_.op("act", [p.psr[bank]], [self.rpt[buf]], "activation", out=self.pt[buf][:, 0:w],
                  in_=p.ps[bank][:, 0:w], func=AF.Exp, scale=SCALE)
            first = (g0 == 0)
            last = (g0 + self.G >= n)

            def pv(grp=grp, buf=buf, first=first, last=last, g0=g0):
                multi = nh > 1
                if multi and first:
                    wtot = oregs[-1][0] + oregs[-1][1]
                    S_.op("pe", [p.rc], [p.psr[obank]], "matmul", p.ps[obank][:, 0:wtot], lhsT=p.zeros[:, 0:128],
                          rhs=p.zeros[:, 0:wtot], start=True, stop=False)
                for j, (kT, v, extras, rds) in enumerate(grp):
                    for hh in range(nh):
                        c0, wd = oregs[hh]
                        S_.op("pe", [self.rpt[buf]] + rds, [p.psr[obank]], "matmul", p.ps[obank][:, c0:c0 + wd],
                              lhsT=self.pt[buf][:, j * ncol + hh * 128:j * ncol + (hh + 1) * 128], rhs=v,
                              start=(first and j == 0 and not multi),
                              stop=(last and j == len(grp) - 1 and hh == nh - 1))
                if last:
                    post()
            self.pending.append(pv)

    def flush(self):
        while self.pending:
            self.pending.pop(0)()


def _moba(self):
    S_ = self.S_
    for pr in range(2):
        with ExitStack() as es:
            Qh = [self.sb(es, "QA", [128, S], BF16), self.sb(es, "QB", [128, S], BF16)]
            KT = self.sb(es, "KT", [128, S], BF16)
            V = self.sb(es, "V", [128, NT, 2, 65], BF16)
            km = self.sb(es, "km", [128, 16], F32)
            kmb = self.sb(es, "kmb", [128, 16], BF16)
            bT = [self.sb(es, f"bT{i}", [128, S], BF16) for i in range(2)]
            e16 = self.sb(es, "e16", [128, 16, 128], BF16)
            O = self.sb(es, "O", [128, NT, 128], BF16)
            wk = [self.sb(es, f"wk{i}", [128, 16], F32) for i in range(2)]
            m8 = [self.sb(es, f"m8{i}", [128, 8], F32) for i in range(2)]
            bs = [self.sb(es, f"bs{i}", [128, 16], F32) for i in range(2)]
            rd = [self.sb(es, f"rd{i}", [128, 1], F32) for i in range(2)]
            rQ, rK, rV, rkm, re, rO = Res(), Res(), Res(), Res(), Res(), Res()
            rbT = [[Res() for _ in range(NT)] for _ in range(2)]
            rwk, rm8, rbs, rrd = [Res(), Res()], [Res(), Res()], [Res(), Res()], [Res(), Res()]
            S_.op("pool", [], [rQ], "memset", Qh[0][64:128, :], 0.0)
            S_.op("pool", [], [rQ], "memset", Qh[1][0:64, :], 0.0)
            S_.dma("sp", [], [rQ], Qh[0][0:64, :], self.scr["ft"][FB_AQ + pr][0:64, :])
            S_.dma("sp", [], [rQ], Qh[1][64:128, :], self.scr["ft"][FB_AQ + pr][64:128, :])
            S_.dma("sp", [], [rK], KT[:], self.scr["ft"][FB_AK + pr])
            S_.op("pool", [], [re], "memset", e16[:], 0.0)
            S_.dma("sp", [], [re], e16[0:16, :, :], self.C["esel16"])
            rbT0 = Res()
            for i in range(2):
                S_.op("pool", [], [rbT0], "memset", bT[i][:], 0.0)
            S_.op("pool", [], [rV], "memset", V[:, :, :, 64:65], 1.0)
            for hh in range(2):
                S_.dma("sp", [], [rV], V[:, :, hh, 0:64],
                       self.scr["tv"][:, pr * 128 + hh * 64:pr * 128 + (hh + 1) * 64].rearrange("(t p) d -> p t d", p=128))
            S_.op("dve", [rK], [rkm], "tensor_reduce", out=km[:], in_=KT[:].rearrange("p (n k) -> p n k", k=256),
                  axis=AX.X, op=ALU.add)
            S_.op("dve", [rkm], [rkm], "tensor_scalar", out=kmb[:], in0=km[:], scalar1=1.0 / 256, scalar2=None,
                  op0=ALU.mult)
            def bias_body(hh, qt, kk):
                h0 = 64 * hh
                own = qt // 2
                b = kk % 2
                pg = 5 + (kk % 2)
                S_.op("dve", [], [rwk[b]], "memset", wk[b][:, own:16], -BIG)
                S_.op("pe", [rQ, rkm], [self.psr[pg]], "matmul", self.ps[pg][:, 0:16],
                      lhsT=Qh[hh][:, qt * 128:(qt + 1) * 128], rhs=kmb[:, :], start=True, stop=True)
                S_.op("dve", [self.psr[pg]], [rwk[b]], "tensor_copy", out=wk[b][:, 0:own], in_=self.ps[pg][:, 0:own])
                S_.op("dve", [rwk[b]], [rm8[b]], "max", out=m8[b][:], in_=wk[b][:])
                S_.op("dve", [rwk[b], rm8[b]], [rbs[b]], "tensor_scalar", out=bs[b][:], in0=wk[b][:],
                      scalar1=m8[b][:, 2:3], scalar2=NEG, op0=ALU.is_lt, op1=ALU.mult)
                yield
                S_.op("pe", [rbs[b], self.rc], [self.psr[7]], "transpose", out=self.ps[7][0:16, 0:128],
                      in_=bs[b][:], identity=self.identf[:])
                S_.op("act", [self.psr[7], rbT0], [rbT[hh][qt]], "copy", out=bT[hh][0:16, qt * 128:(qt + 1) * 128],
                      in_=self.ps[7][0:16, 0:128])
            round_robin((bias_body(hh, qt, i) for i, (hh, qt) in
                         enumerate((hh, qt) for hh in range(2) for qt in range(8, NT))), 2)
            ap_ = AttnPipe(self, es, 128)
            for hh in range(2):
                h0 = 64 * hh
                for qt in range(NT):
                    own = qt // 2
                    items = []
                    for kt in range(qt + 1):
                        n = kt // 2
                        ex = []
                        rds = [rK, rV]
                        if n < own and qt >= 8:
                            ex = [(e16[:, n, :], bT[hh][:, qt * 128:(qt + 1) * 128])]
                            rds = rds + [re, rbT[hh][qt]]
                        elif kt == qt:
                            ex = [(self.identb[:], self.masks[:, 0, :])]
                        items.append((KT[:, kt * 128:(kt + 1) * 128], V[:, kt, hh, :], ex, rds))
                    ob = 3 + (qt % 2)

                    def post(ob=ob, qt=qt, hh=hh):
                        b = qt % 2
                        S_.op("dve", [self.psr[ob]], [rrd[b]], "reciprocal", out=rd[b][:], in_=self.ps[ob][:, 64:65])
                        S_.op("dve", [self.psr[ob], rrd[b]], [rO], "tensor_scalar", out=O[:, qt, hh * 64:(hh + 1) * 64],
                              in0=self.ps[ob][:, 0:64], scalar1=rd[b][:, 0:1], scalar2=None, op0=ALU.mult)
                    ap_.qtile(items, Qh[hh][:, qt * 128:(qt + 1) * 128], 1, ob, [(0, 65)], [rQ], post)
            ap_.flush()
            S_.dma("pool", [rO], [], self.scr["o"][:, pr * 128:(pr + 1) * 128].rearrange("(t p) c -> p t c", p=128), O[:])
            S_.barrier()


Prog.phase_moba = _moba


def _nsa(self):
    S_ = self.S_
    l = self.l
    with ExitStack() as es:
        KsT = self.sb(es, "KsT", [128, S], BF16)
        KwT = self.sb(es, "KwT", [128, S], BF16)
        Vs = self.sb(es, "Vs", [128, NT, 2, 65], BF16)
        Vw = self.sb(es, "Vw", [128, NT, 2, 65], BF16)
        cmpm = self.sb(es, "cmpm", [128, 2, S], BF16)
        nadd = self.sb(es, "nadd", [128, NT, 64], F32)
        nmin = self.sb(es, "nmin", [128, NT, 64], F32)
        e64 = self.sb(es, "e64", [128, 32, 128], BF16)
        ovl = self.sb(es, "ovl", [128, 2, 64], BF16)
        KcT = self.sb(es, "KcT", [128, 256], BF16)
        VcA = self.sb(es, "VcA", [128, 2, 2, 129], BF16)
        Oall = self.sb(es, "Oall", [128, NT, 384], BF16)
        ec = ExitStack()
        KcP = self.sb(ec, "KcP", [128, S], BF16)
        VcP = self.sb(ec, "VcP", [128, S], BF16)
        w1 = {"k": self.sb(ec, "w1k", [128, 32, 128], BF16), "v": self.sb(ec, "w1v", [128, 32, 128], BF16)}
        pe_ = {"k": self.sb(ec, "pek", [128, 32], BF16), "v": self.sb(ec, "pev", [128, 32], BF16)}
        w2kp = self.sb(ec, "w2kp", [128, 2, 128], BF16)
        w2v = self.sb(ec, "w2v", [128, 64], BF16)
        hid = [self.sb(ec, f"hid{i}", [128, 256], BF16) for i in range(2)]
        peb = [self.sb(ec, f"peb{i}", [128, 1], F32) for i in range(2)]
        rld, rw, rV, rKc, rVc, rO = Res(), Res(), Res(), Res(), Res(), Res()
        for t_, fb in ((KcP, FB_KC), (VcP, FB_VC), (KsT, FB_KS), (KwT, FB_KW)):
            S_.dma("sp", [], [rld], t_[:], self.scr["ft"][fb])
        S_.op("pool", [], [rld], "memset", e64[64:128, :, :], 0.0)
        S_.dma("sp", [], [rld], e64[0:64, :, :], self.C["esel64"])
        for t_, n_ in ((cmpm, "cmpmask"), (nadd, "nsa_add"), (nmin, "nsa_min"), (ovl, "overlap")):
            S_.dma("sp", [], [rld], t_[:], self.C[n_])
        for vt, c0 in ((Vs, 256), (Vw, 384)):
            S_.op("pool", [], [rV], "memset", vt[:, :, :, 64:65], 1.0)
            for g in range(2):
                S_.dma("sp", [], [rV], vt[:, :, g, 0:64],
                       self.scr["tv"][:, c0 + g * 64:c0 + (g + 1) * 64].rearrange("(t p) d -> p t d", p=128))
        S_.op("pool", [], [rw], "memset", w2kp[:], 0.0)
        for kind in ("k", "v"):
            for g in range(2):
                S_.dma("pool", [], [rw], w1[kind][64 * g:64 * g + 64, :, :],
                       self.I["cmp_w1_" + kind][l].rearrange("(l d) h -> d l h", d=64))
                S_.dma("pool", [], [rw], pe_[kind][64 * g:64 * g + 64, :], self.I["cmp_peT_" + kind][l])
        for g in range(2):
            S_.dma("pool", [], [rw], w2kp[:, g, 64 * g:64 * g + 64], self.I["cmp_w2_k"][l])
        S_.dma("pool", [], [rw], w2v[:], self.I["cmp_w2_v"][l])
        S_.op("pool", [], [rVc], "memset", VcA[:, :, :, 64:65], 1.0)
        for nt in range(2):
            for g in range(2):
                S_.op("pool", [rld], [rVc], "tensor_copy", out=VcA[:, nt, g, 65:129], in_=ovl[:, nt, :])
        rhid, rpeb = [Res(), Res()], [Res(), Res()]
        kk = 0
        for kind, src in (("k", KcP), ("v", VcP)):
            s16 = src[:].rearrange("p (n s) -> p n s", s=16)
            for g in range(2):
                h0 = 64 * g
                b = kk % 2
                kk += 1
                for li in range(32):
                    S_.op("pe", [rw], [self.psr[5]], "matmul", self.ps[5][:, 0:1], lhsT=w1[kind][h0:h0 + 64, li, :],
                          rhs=pe_[kind][h0:h0 + 64, li:li + 1], start=(li == 0), stop=(li == 31))
                S_.op("dve", [self.psr[5]], [rpeb[b]], "tensor_copy", out=peb[b][:], in_=self.ps[5][:, 0:1])
                for li in range(32):
                    rhs = s16[h0:h0 + 64, 0:255, li] if li < 16 else s16[h0:h0 + 64, 1:256, li - 16]
                    S_.op("pe", [rw, rld], [self.psr[6]], "matmul", self.ps[6][:, 0:255], lhsT=w1[kind][h0:h0 + 64, li, :],
                          rhs=rhs, start=(li == 0), stop=(li == 31))
                S_.op("dve", [], [rhid[b]], "memset", hid[b][:, 255:256], 0.0)
                S_.op("act", [self.psr[6], rpeb[b]], [rhid[b]], "activation", out=hid[b][:, 0:255], in_=self.ps[6][:, 0:255],
                      func=AF.Silu, bias=peb[b][:, 0:1], scale=1.0)
                if kind == "k":
                    S_.op("pe", [rw, rhid[b]], [self.psr[7]], "matmul", self.ps[7][:, 0:256], lhsT=w2kp[:, g, :],
                          rhs=hid[b][:], start=(g == 0), stop=(g == 1))
                    if g == 1:
                        S_.op("act", [self.psr[7]], [rKc], "copy", out=KcT[:], in_=self.ps[7][:, 0:256])
                else:
                    for nt in range(2):
                        S_.op("pe", [rw, rhid[b]], [self.psr[7]], "matmul", self.ps[7][:, nt * 64:(nt + 1) * 64],
                              lhsT=hid[b][:, nt * 128:(nt + 1) * 128], rhs=w2v[:], start=True, stop=True)
                    S_.op("act", [self.psr[7]], [rVc], "copy", out=VcA[:, :, g, 0:64],
                          in_=self.ps[7][:, 0:128].rearrange("p (n d) -> p n d", d=64))
        S_.barrier()
        ec.close()
        QG = [self.sb(es, f"QG{g}", [128, 3, S], BF16) for g in range(2)]
        S_.op("pool", [], [rld], "memset", QG[0][64:128, :, :], 0.0)
        S_.op("pool", [], [rld], "memset", QG[1][0:64, :, :], 0.0)
        for r in range(3):
            S_.dma("sp", [], [rld], QG[0][0:64, r, :], self.scr["ft"][FB_BQ + r][0:64, :])
            S_.dma("sp", [], [rld], QG[1][64:128, r, :], self.scr["ft"][FB_BQ + r][64:128, :])
        import os
        if self.debug:
            S_.dma("pool", [rKc], [], self.scr["dbg1"], KcT[:])
            S_.dma("pool", [rVc], [], self.scr["dbg2"], VcA[:].rearrange("p a b c -> p (a b c)"))
        if os.environ.get("NSA_STOP") == "1":
            S_.barrier()
            return
        bT = [[self.sb(es, f"nbT{g}{b}", [128, 128], BF16) for b in range(2)] for g in range(2)]
        rbT = [[Res(), Res()], [Res(), Res()]]
        for g in range(2):
            for b in range(2):
                S_.op("pool", [], [rbT[g][b]], "memset", bT[g][b][:], 0.0)
        Ob = [self.sb(es, f"Ob{b}", [128, 6, 64], F32) for b in range(2)]
        rOb = [Res(), Res()]
        NS = 4
        rdn = [self.sb(es, f"rdn{i}", [128, 3], F32) for i in range(NS)]
        sc3 = [self.sb(es, f"sc3{i}", [128, 3], F32) for i in range(NS)]
        imp = [self.sb(es, f"imp{i}", [128, 64], F32) for i in range(2)]
        imp2 = [self.sb(es, f"imp2{i}", [128, 64], F32) for i in range(2)]
        m8a = [self.sb(es, f"m8a{i}", [128, 8], F32) for i in range(2)]
        m8b = [self.sb(es, f"m8b{i}", [128, 8], F32) for i in range(2)]
        bsf = [self.sb(es, f"bsf{i}", [128, 64], F32) for i in range(2)]
        rsm = [Res() for _ in range(NS)]
        rimp = [Res(), Res()]
        ap_ = AttnPipe(self, es, 384)
        cnt = {"o": 0, "s": 0, "i": 0}

        def gw(qt, g, br):
            return self.BGs[:, qt, :].rearrange("p (h k) -> p h k", k=3)[:, 3 * g:3 * g + 3, br]

        def post_common(ob, wd, qt, g, br, first):
            i = cnt["s"] % NS
            cnt["s"] += 1
            b = qt % 2
            den = self.ps[ob][:, 0:3 * wd].rearrange("p (r w) -> p r w", w=wd)[:, :, 64]
            S_.op("dve", [self.psr[ob]], [rsm[i]], "tensor_scalar", out=rdn[i][:], in0=den, scalar1=1e-30, scalar2=None,
                  op0=ALU.max)
            S_.op("dve", [rsm[i]], [rsm[i]], "reciprocal", out=rdn[i][:], in_=rdn[i][:])
            S_.op("dve", [rsm[i], self.rBG], [rsm[i]], "tensor_tensor", out=sc3[i][:], in0=rdn[i][:], in1=gw(qt, g, br),
                  op=ALU.mult)
            for r in range(3):
                src = self.ps[ob][:, r * wd:r * wd + 64]
                if first:
                    S_.op("dve", [self.psr[ob], rsm[i]], [rOb[b]], "tensor_scalar", out=Ob[b][:, 3 * g + r, :], in0=src,
                          scalar1=sc3[i][:, r:r + 1], scalar2=None, op0=ALU.mult)
                else:
                    S_.op("dve", [self.psr[ob], rsm[i]], [rOb[b]], "scalar_tensor_tensor", out=Ob[b][:, 3 * g + r, :],
                          in0=src, scalar=sc3[i][:, r:r + 1], in1=Ob[b][:, 3 * g + r, :], op0=ALU.mult, op1=ALU.add)
            return i

        nqt = int(os.environ.get("NSA_NQT", NT))
        brs = os.environ.get("NSA_BR", "csw")
        deferred = []

        def emit_cmp(qt):
            b = qt % 2
            qs = slice(qt * 128, (qt + 1) * 128)
            for g in range(2):
                h0 = 64 * g
                qrhs = QG[g][:, :, qs]
                items = []
                for nt in ([0, 1] if qt >= 16 else [0]):
                    ex = []
                    if nt == 1 or qt < 17:
                        ex = [(self.identb[:], cmpm[:, nt, qs])]
                    items.append((KcT[:, nt * 128:(nt + 1) * 128], VcA[:, nt, g, :], ex, [rKc, rVc, rld]))
                ob = 3 + cnt["o"] % 3
                cnt["o"] += 1

                def post_cmp(ob=ob, qt=qt, g=g, b=b):
                    i = post_common(ob, 129, qt, g, 0, True)
                    j = cnt["i"] % 2
                    cnt["i"] += 1
                    for r in range(3):
                        src = self.ps[ob][:, r * 129 + 65:r * 129 + 129]
                        if r == 0:
                            S_.op("dve", [self.psr[ob], rsm[i]], [rimp[j]], "tensor_scalar", out=imp[j][:], in0=src,
                                  scalar1=rdn[i][:, 0:1], scalar2=None, op0=ALU.mult)
                        else:
                            S_.op("dve", [self.psr[ob], rsm[i]], [rimp[j]], "scalar_tensor_tensor", out=imp[j][:], in0=src,
                                  scalar=rdn[i][:, r:r + 1], in1=imp[j][:], op0=ALU.mult, op1=ALU.add)
                    S_.op("dve", [rld], [rimp[j]], "tensor_tensor", out=imp[j][:], in0=imp[j][:], in1=nadd[:, qt, :], op=ALU.add)
                    S_.op("dve", [rld], [rimp[j]], "tensor_tensor", out=imp[j][:], in0=imp[j][:], in1=nmin[:, qt, :], op=ALU.min)
                    S_.op("dve", [rimp[j]], [rimp[j]], "max", out=m8a[j][:], in_=imp[j][:])
                    S_.op("dve", [rimp[j]], [rimp[j]], "match_replace", out=imp2[j][:], in_to_replace=m8a[j][:],
                          in_values=imp[j][:], imm_value=-BIG)
                    S_.op("dve", [rimp[j]], [rimp[j]], "max", out=m8b[j][:], in_=imp2[j][:])
                    S_.op("dve", [rimp[j]], [rimp[j]], "tensor_scalar", out=bsf[j][:], in0=imp[j][:], scalar1=m8b[j][:, 7:8],
                          scalar2=NEG, op0=ALU.is_lt, op1=ALU.mult)
                    def tr(j=j, g=g, b=b):
                        S_.op("pe", [rimp[j], self.rc], [self.psr[7]], "transpose", out=self.ps[7][0:64, 0:128], in_=bsf[j][:],
                              identity=self.identf[:])
                        S_.op("act", [self.psr[7]], [rbT[g][b]], "copy", out=bT[g][b][0:64, :], in_=self.ps[7][0:64, 0:128])
                    deferred.append(tr)
                ap_.qtile(items, qrhs, 3, ob, [(r * 129, 129) for r in range(3)], [rld], post_cmp)

        def emit_sw(qt):
            b = qt % 2
            qs = slice(qt * 128, (qt + 1) * 128)
            for br, KT_, V_ in ((1, KsT, Vs), (2, KwT, Vw)):
                if "csw"[br] not in brs:
                    continue
                for g in range(2):
                    h0 = 64 * g
                    qrhs = QG[g][:, :, qs]
                    items = []
                    k0 = 0 if br == 1 else max(0, qt - 4)
                    for kt in range(k0, qt + 1):
                        ex = []
                        rds = [rld, rV]
                        if br == 1:
                            ex.append((e64[:, kt, :], bT[g][b][:]))
                            rds = rds + [rbT[g][b]]
                        if kt == qt:
                            ex.append((self.identb[:], self.masks[:, 0, :]))
                        elif br == 2 and kt == qt - 4:
                            ex.append((self.identb[:], self.masks[:, 1, :]))
                        items.append((KT_[:, kt * 128:(kt + 1) * 128], V_[:, kt, g, :], ex, rds))
                    ob = 3 + cnt["o"] % 3
                    cnt["o"] += 1

                    def post_sw(ob=ob, qt=qt, g=g, br=br, b=b):
                        post_common(ob, 65, qt, g, br, False)
                        if br == 2 and g == 1:
                            S_.op("pool", [rOb[b]], [rO], "tensor_copy", out=Oall[:, qt, :],
                                  in_=Ob[b][:].rearrange("p h d -> p (h d)"))
                    ap_.qtile(items, qrhs, 3, ob, [(r * 65, 65) for r in range(3)], [rld], post_sw)

        for qt in range(nqt + 1):
            if qt < nqt:
                emit_cmp(qt)
            if qt >= 1:
                emit_sw(qt - 1)
            else:
                ap_.flush()
            while deferred:
                deferred.pop(0)()
        ap_.flush()
        S_.dma("pool", [rO], [], self.scr["o"][:, 256:640].rearrange("(t p) c -> p t c", p=128), Oall[:])
        S_.barrier()


Prog.phase_nsa = _nsa


def _dil(self):
    S_ = self.S_
    with ExitStack() as es:
        QD = [self.sb(es, f"QD{i}", [128, 3, S], BF16) for i in range(2)]
        KTc = self.sb(es, "KTc", [128, 3, S], BF16)
        Vg = [self.sb(es, f"Vg{gi}", [128, NT, 2, 65], BF16) for gi in range(3)]
        rld, rV = Res(), Res()
        S_.op("pool", [], [rld], "memset", QD[0][64:128, :, :], 0.0)
        S_.op("pool", [], [rld], "memset", QD[1][0:64, :, :], 0.0)
        for gi in range(3):
            S_.dma("sp", [], [rld], QD[0][0:64, gi, :], self.scr["ft"][FB_CQ + gi][0:64, :])
            S_.dma("sp", [], [rld], QD[1][64:128, gi, :], self.scr["ft"][FB_CQ + gi][64:128, :])
            S_.dma("sp", [], [rld], KTc[:, gi, :], self.scr["ft"][FB_CK + gi])
        for gi, dil in enumerate((1, 4, 16)):
            ntile = NT // dil
            S_.op("pool", [], [rV], "memset", Vg[gi][:, :, :, 64:65], 1.0)
            for r in range(dil):
                for hs in range(2):
                    c0 = 512 + gi * 128 + hs * 64
                    src = self.scr["tv"][:, c0:c0 + 64].rearrange("(l d) c -> d l c", d=dil)[r]
                    S_.dma("sp", [], [rV], Vg[gi][:, r * ntile:(r + 1) * ntile, hs, 0:64],
                           src.rearrange("(j p) c -> p j c", p=128))
        ocs = [self.sb(es, f"ocs{i}", [128, 65], F32) for i in range(4)]
        rocs = [Res() for _ in range(4)]
        ap_ = AttnPipe(self, es, 128)
        cnt = {"o": 0}
        for gi, dil in enumerate((1, 4, 16)):
            ntile = NT // dil
            ocv = self.scr["oc"][gi].rearrange("(l d) c -> d l c", d=dil)
            for hs in range(2):
                h0 = 64 * hs
                qv = QD[hs][:, gi, :].rearrange("p (l d) -> p d l", d=dil)
                kv = KTc[:, gi, :].rearrange("p (l d) -> p d l", d=dil)
                for r in range(dil):
                    for j in range(ntile):
                        items = []
                        for kt in ([j - 1, j] if j >= 1 else [j]):
                            mk = 0 if kt == j else 2
                            items.append((kv[:, r, kt * 128:(kt + 1) * 128], Vg[gi][:, r * ntile + kt, hs, :],
                                          [(self.identb[:], self.masks[:, mk, :])], [rld, rV]))
                        ob = 3 + cnt["o"] % 3
                        cnt["o"] += 1

                        def post(ob=ob, r=r, j=j, hs=hs, ocv=ocv, i=cnt["o"] % 4):
                            S_.op("dve", [self.psr[ob]], [rocs[i]], "tensor_copy", out=ocs[i][:], in_=self.ps[ob][:, 0:65])
                            S_.dma("pool", [rocs[i]], [], ocv[r][j * 128:(j + 1) * 128, hs * 65:(hs + 1) * 65], ocs[i][:])
                        ap_.qtile(items, qv[:, r, j * 128:(j + 1) * 128], 1, ob, [(0, 65)], [rld], post)
        ap_.flush()
        S_.barrier()
        Oc = self.sb(es, "Oc", [128, NT, 128], BF16)
        rOc = Res()
        acc = [[self.sb(es, f"acc{i}{gi}", [128, 8, 130], F32) for gi in range(3)] for i in range(2)]
        racc = [Res(), Res()]
        rdc = [self.sb(es, f"rdc{i}", [128, 8, 2], F32) for i in range(2)]
        for ch in range(4):
            i = ch % 2
            for gi in range(3):
                S_.dma("sp", [], [racc[i]], acc[i][gi][:],
                       self.scr["oc"][gi][ch * 1024:(ch + 1) * 1024, :].rearrange("(t p) c -> p t c", p=128))
            S_.op("dve", [racc[i]], [racc[i]], "tensor_tensor", out=acc[i][0][:], in0=acc[i][0][:], in1=acc[i][1][:], op=ALU.add)
            S_.op("dve", [racc[i]], [racc[i]], "tensor_tensor", out=acc[i][0][:], in0=acc[i][0][:], in1=acc[i][2][:], op=ALU.add)
            for hs in range(2):
                S_.op("dve", [racc[i]], [racc[i]], "reciprocal", out=rdc[i][:, :, hs:hs + 1],
                      in_=acc[i][0][:, :, hs * 65 + 64:hs * 65 + 65])
            for t in range(8):
                for hs in range(2):
                    S_.op("dve", [racc[i]], [rOc], "tensor_scalar", out=Oc[:, ch * 8 + t, hs * 64:(hs + 1) * 64],
                          in0=acc[i][0][:, t, hs * 65:hs * 65 + 64], scalar1=rdc[i][:, t, hs:hs + 1], scalar2=None,
                          op0=ALU.mult)
        S_.dma("pool", [rOc], [], self.scr["o"][:, 640:768].rearrange("(t p) c -> p t c", p=128), Oc[:])
        S_.barrier()


Prog.phase_dil = _dil


def _merge(self, xin, xout):
    S_ = self.S_
    l = self.l
    with ExitStack() as es:
        Wa = self.sb(es, "Wa", [128, 6, D], BF16)
        Wo = self.sb(es, "Wo", [128, 8, D], BF16)
        Wr = self.sb(es, "Wr", [128, 8, NEXP], BF16)
        rW, rB = Res(), Res()
        for nm, c0, nck in (("w_branch_a", 0, 2), ("w_branch_b", 2, 3), ("w_branch_c", 5, 1)):
            S_.dma("pool", [], [rW], Wa[:, c0:c0 + nck, :], self.I[nm][l].rearrange("(c p) n -> p c n", p=128))
        S_.dma("pool", [], [rW], Wo[:], self.I["w_out"][l].rearrange("(c p) n -> p c n", p=128))
        S_.dma("pool", [], [rW], Wr[:], self.I["router_w"][l].rearrange("(c p) n -> p c n", p=128))
        g1b = self.bload(es, "g1b", self.scr["mod"][l:l + 1, 2 * D:3 * D], rB)
        l1g = self.bload(es, "l1g", self.I["ln1_g"][l:l + 1, :], rB)
        l1b = self.bload(es, "l1b", self.I["ln1_b"][l:l + 1, :], rB)
        sc2 = self.bload(es, "sc2", self.scr["mod"][l:l + 1, 4 * D:5 * D], rB)
        sh2 = self.bload(es, "sh2", self.scr["mod"][l:l + 1, 3 * D:4 * D], rB)
        rbb = self.sb(es, "rbb", [128, NEXP], F32)
        S_.dma("sp", [], [rB], rbb[:], self.I["router_b"][l:l + 1, :].partition_broadcast(128))

        def dbl(name, shape, dt):
            return [self.sb(es, f"{name}{i}", shape, dt) for i in range(2)], [Res(), Res()]
        ot, rot = dbl("ot", [128, 768], BF16)
        gt, rgt = dbl("gt", [128, 3072], BF16)
        xt, rxt = dbl("mxt", [128, D], F32)
        oT, roT = dbl("oT", [128, 6, 128], BF16)
        m1, rm1 = dbl("m1", [128, 512], F32)
        m2, rm2 = dbl("m2", [128, 512], F32)
        m3, rm3 = dbl("m3", [128, 512], F32)
        mb, rmb = dbl("mb", [128, D], BF16)
        mT, rmT = dbl("mT", [128, 8, 128], BF16)
        yt, ryt = dbl("yt", [128, D], F32)
        zt, rzt = dbl("zt", [128, D], F32)
        x1, rx1 = dbl("x1", [128, D], F32)
        xn2, rxn2 = dbl("xn2", [128, D], F32)
        hb2, rhb2 = dbl("hb2", [128, D], BF16)
        hT2, rhT2 = dbl("hT2", [128, 8, 128], BF16)
        st1, rst1 = dbl("st1", [128, 16], F32)
        st2, rst2 = dbl("st2", [128, 16], F32)
        lg, rlg = dbl("lg", [128, NEXP], F32)
        ex, rex = dbl("ex", [128, NEXP], F32)
        m8, rm8 = dbl("rm8", [128, 8], F32)
        sm, rsm = dbl("rsm", [128, 4], F32)
        def tile_body(t):
            b = t % 2
            rows = slice(t * 128, (t + 1) * 128)
            S_.dma("sp", [], [rot[b]], ot[b][:], self.scr["o"][rows, :])
            S_.dma("sp", [], [rgt[b]], gt[b][:], self.scr["mg"][rows, :])
            S_.dma("sp", [], [rxt[b]], xt[b][:], xin[rows, :])
            pT = 6 + b
            pst = self.ps[pT][:].bitcast(BF16)
            for c in range(6):
                S_.op("pe", [rot[b], self.rc], [self.psr[pT]], "transpose", out=pst[:, c * 128:(c + 1) * 128],
                      in_=ot[b][:, c * 128:(c + 1) * 128], identity=self.identb[:])
            S_.op("act", [self.psr[pT]], [roT[b]], "copy", out=oT[b][:], in_=pst[:, 0:768].rearrange("p (c n) -> p c n", c=6))
            yield
            for half in range(2):
                hs_ = slice(half * 512, (half + 1) * 512)
                pbs = (0, 1, 2) if half == 0 else (3, 4, 5)
                for bi, (c0, nck) in enumerate(((0, 2), (2, 3), (5, 1))):
                    for c in range(nck):
                        S_.op("pe", [roT[b], rW], [self.psr[pbs[bi]]], "matmul", self.ps[pbs[bi]][:, :],
                              lhsT=oT[b][:, c0 + c, :], rhs=Wa[:, c0 + c, hs_], start=(c == 0), stop=(c == nck - 1))
                for bi, (mm_, rmm) in enumerate(((m1, rm1), (m2, rm2), (m3, rm3))):
                    S_.op("dve", [self.psr[pbs[bi]], rgt[b]], [rmm[b]], "tensor_tensor", out=mm_[b][:],
                          in0=self.ps[pbs[bi]][:, :], in1=gt[b][:, bi * 1024 + half * 512:bi * 1024 + (half + 1) * 512],
                          op=ALU.mult)
                S_.op("pool", [rm1[b], rm2[b]], [rm1[b]], "tensor_tensor", out=m1[b][:], in0=m1[b][:], in1=m2[b][:], op=ALU.add)
                S_.op("pool", [rm1[b], rm3[b]], [rmb[b]], "tensor_tensor", out=mb[b][:, hs_], in0=m1[b][:], in1=m3[b][:],
                      op=ALU.add)
                yield
            for c in range(8):
                S_.op("pe", [rmb[b], self.rc], [self.psr[pT]], "transpose", out=pst[:, c * 128:(c + 1) * 128],
                      in_=mb[b][:, c * 128:(c + 1) * 128], identity=self.identb[:])
            S_.op("act", [self.psr[pT]], [rmT[b]], "copy", out=mT[b][:], in_=pst.rearrange("p (c n) -> p c n", c=8))
            yield
            for half in range(2):
                hs_ = slice(half * 512, (half + 1) * 512)
                pb_ = half
                for c in range(8):
                    S_.op("pe", [rmT[b], rW], [self.psr[pb_]], "matmul", self.ps[pb_][:, :], lhsT=mT[b][:, c, :],
                          rhs=Wo[:, c, hs_], start=(c == 0), stop=(c == 7))
                S_.op("dve", [self.psr[pb_], rB], [ryt[b]], "tensor_tensor", out=yt[b][:, hs_], in0=self.ps[pb_][:, :],
                      in1=g1b[:, hs_], op=ALU.mult)
            yield
            S_.op("dve", [ryt[b], rxt[b]], [rzt[b]], "scalar_tensor_tensor", out=zt[b][:], in0=xt[b][:], scalar=ALPHA,
                  in1=yt[b][:], op0=ALU.mult, op1=ALU.add)
            self.ln_tile(zt[b], rzt[b], None, None, st1[b], rst1[b], split=True)
            yield
            self.ln_tile2(st1[b], rst1[b])
            S_.op("act", [rzt[b], rst1[b]], [rzt[b]], "activation", out=zt[b][:], in_=zt[b][:], func=AF.Identity,
                  bias=st1[b][:, 1:2], scale=st1[b][:, 0:1])
            S_.op("pool", [rzt[b], rB], [rzt[b]], "tensor_tensor", out=zt[b][:], in0=zt[b][:], in1=l1g[:], op=ALU.mult)
            yield
            S_.op("dve", [rzt[b], rB], [rx1[b]], "tensor_tensor", out=x1[b][:], in0=zt[b][:], in1=l1b[:], op=ALU.add)
            S_.dma("pool", [rx1[b]], [], xout[rows, :], x1[b][:])
            self.ln_tile(x1[b], rx1[b], None, None, st2[b], rst2[b], split=True)
            yield
            self.ln_tile2(st2[b], rst2[b])
            S_.op("act", [rx1[b], rst2[b]], [rxn2[b]], "activation", out=xn2[b][:], in_=x1[b][:], func=AF.Identity,
                  bias=st2[b][:, 1:2], scale=st2[b][:, 0:1])
            S_.op("pool", [rxn2[b], rB], [rxn2[b]], "tensor_tensor", out=xn2[b][:], in0=xn2[b][:], in1=sc2[:], op=ALU.mult)
            yield
            S_.op("dve", [rxn2[b], rB], [rhb2[b]], "tensor_tensor", out=hb2[b][:], in0=xn2[b][:], in1=sh2[:], op=ALU.add)
            for c in range(8):
                S_.op("pe", [rhb2[b], self.rc], [self.psr[pT]], "transpose", out=pst[:, c * 128:(c + 1) * 128],
                      in_=hb2[b][:, c * 128:(c + 1) * 128], identity=self.identb[:])
            S_.op("act", [self.psr[pT]], [rhT2[b]], "copy", out=hT2[b][:], in_=pst.rearrange("p (c n) -> p c n", c=8))
            S_.dma("pool", [rhb2[b]], [], self.scr["h2"][rows, :], hb2[b][:])
            yield
            for c in range(8):
                S_.op("pe", [rhT2[b], rW], [self.psr[2]], "matmul", self.ps[2][:, 0:NEXP], lhsT=hT2[b][:, c, :],
                      rhs=Wr[:, c, :], start=(c == 0), stop=(c == 7))
            S_.op("dve", [self.psr[2], rB], [rlg[b]], "tensor_tensor", out=lg[b][:], in0=self.ps[2][:, 0:NEXP], in1=rbb[:],
                  op=ALU.add)
            S_.op("dve", [rlg[b]], [rm8[b]], "max", out=m8[b][:], in_=lg[b][:])
            S_.op("dve", [rm8[b]], [rsm[b]], "tensor_scalar", out=sm[b][:, 0:1], in0=m8[b][:, 0:1], scalar1=-1.0, scalar2=None,
                  op0=ALU.mult)
            S_.op("act", [rlg[b], rsm[b]], [rex[b]], "activation", out=ex[b][:], in_=lg[b][:], func=AF.Exp,
                  bias=sm[b][:, 0:1], scale=1.0)
            yield
            S_.op("dve", [rlg[b], rm8[b], rex[b]], [rex[b]], "scalar_tensor_tensor", out=ex[b][:], in0=lg[b][:],
                  scalar=m8[b][:, 3:4], in1=ex[b][:], op0=ALU.is_ge, op1=ALU.mult)
            S_.op("dve", [rlg[b], rm8[b]], [self.rS], "tensor_scalar", out=self.Sall[:, t, :], in0=lg[b][:],
                  scalar1=m8[b][:, 3:4], scalar2=None, op0=ALU.is_ge)
            S_.op("dve", [rex[b]], [rsm[b]], "tensor_reduce", out=sm[b][:, 1:2], in_=ex[b][:], axis=AX.X, op=ALU.add)
            S_.op("dve", [rsm[b]], [rsm[b]], "reciprocal", out=sm[b][:, 2:3], in_=sm[b][:, 1:2])
            S_.op("dve", [rex[b], rsm[b]], [self.rG], "tensor_scalar", out=self.Gall[:, t, :], in0=ex[b][:],
                  scalar1=sm[b][:, 2:3], scalar2=None, op0=ALU.mult)
        round_robin((tile_body(t) for t in range(NT)), 2)
        S_.barrier()


Prog.phase_merge = _merge


def _moe(self):
    S_ = self.S_
    l = self.l
    TS = 1024
    with ExitStack() as es:
        wgu = [self.sb(es, f"wgu{i}", [128, 8, 2 * D], BF16) for i in range(2)]
        wdn = [self.sb(es, f"wdn{i}", [128, 8, D], BF16) for i in range(2)]
        rwg, rwd = [Res(), Res()], [Res(), Res()]
        bgu = self.sb(es, "bgu", [128, NEXP * 16], F32)
        bdn = self.sb(es, "bdn", [NEXP, D], F32)
        h2 = self.sb(es, "h2", [128, 8, TS], BF16)
        yacc = self.sb(es, "yacc", [128, 8, D], F32)
        GT = self.sb(es, "GT", [NEXP, 8, 128], F32)
        actT = [self.sb(es, f"actT{i}", [128, 8, 512], BF16) for i in range(2)]
        xg = [self.sb(es, f"xg{i}", [128, 512], F32) for i in range(2)]
        sg = [self.sb(es, f"sg{i}", [128, 512], F32) for i in range(2)]
        xl = [self.sb(es, f"xl{i}", [128, 512], F32) for i in range(2)]
        rB, rh2, rGT = Res(), Res(), Res()
        ry = [Res() for _ in range(8)]
        ract = [Res(), Res()]
        rxg, rsg, rxl = [Res(), Res()], [Res(), Res()], [Res(), Res()]
        S_.dma("sp", [], [rB], bgu[:], self.I["b_gu_l"][l])
        S_.dma("sp", [], [rB], bdn[:], self.I["b_down"][l])
        bguv = bgu[:].rearrange("p (e j two) -> p e j two", e=NEXP, two=2)

        def load_wg(e, wb):
            src = self.I["w_gate_up"][l, e].rearrange("(c p) n -> p c n", p=128)
            for hlf in range(2):
                S_.dma("pool", [], [rwg[wb]], wgu[wb][:, hlf * 4:(hlf + 1) * 4, :], src[:, hlf * 4:(hlf + 1) * 4, :])

        def load_wd(e, wb):
            S_.dma("pool", [], [rwd[wb]], wdn[wb][:], self.I["w_down"][l, e].rearrange("(c p) n -> p c n", p=128))
        kq = 0
        ky = 0
        kw = 0
        pend = None
        load_wg(0, 0)
        load_wd(0, 0)
        for ts in range(S // TS):
            S_.dma("sp", [], [rh2], h2[:], self.scr["h2t"][:, :, ts * TS:(ts + 1) * TS])
            for tt in range(8):
                tg = ts * 8 + tt
                pb = 4 + ky % 4
                ky += 1
                S_.op("pe", [self.rG, self.rc], [self.psr[pb]], "transpose", out=self.ps[pb][0:NEXP, 0:128],
                      in_=self.Gall[:, tg, :], identity=self.identf[:])
                S_.op("act", [self.psr[pb]], [rGT], "copy", out=GT[:, tt, :], in_=self.ps[pb][0:NEXP, 0:128])
                for half in range(2):
                    pb = 4 + ky % 4
                    ky += 1
                    S_.op("pe", [rGT, rB], [self.psr[pb]], "matmul", self.ps[pb][:, :], lhsT=GT[:, tt, :],
                          rhs=bdn[:, half * 512:(half + 1) * 512], start=True, stop=True)
                    S_.op("act", [self.psr[pb]], [ry[tt]], "copy", out=yacc[:, tt, half * 512:(half + 1) * 512],
                          in_=self.ps[pb][:, :])
            for e in range(NEXP):
                wb = kw % 2
                kw += 1
                more = not (ts == S // TS - 1 and e == NEXP - 1)
                if more:
                    load_wg((e + 1) % NEXP, (wb + 1) % 2)
                wv = wgu[wb][:].rearrange("p c (j two) -> p c two j", two=2)
                for ch in range(2):
                    ab = (kq // 8) % 2
                    for jc in range(8):
                        q2 = kq % 2
                        pg, pl = (0, 1) if (kq % 2 == 0) else (2, 3)
                        kq += 1
                        for two, pp in ((0, pg), (1, pl)):
                            for c in range(8):
                                S_.op("pe", [rwg[wb], rh2], [self.psr[pp]], "matmul", self.ps[pp][:, :],
                                      lhsT=wv[:, c, two, jc * 128:(jc + 1) * 128], rhs=h2[:, c, ch * 512:(ch + 1) * 512],
                                      start=(c == 0), stop=(c == 7))
                        S_.op("dve", [self.psr[pg], rB], [rxg[q2]], "tensor_scalar", out=xg[q2][:], in0=self.ps[pg][:, :],
                              scalar1=bguv[:, e, jc, 0:1], scalar2=7.0, op0=ALU.add, op1=ALU.min)
                        S_.op("act", [rxg[q2]], [rsg[q2]], "activation", out=sg[q2][:], in_=xg[q2][:], func=AF.Sigmoid,
                              scale=1.702)
                        S_.op("dve", [self.psr[pl], rB], [rxl[q2]], "tensor_scalar", out=xl[q2][:], in0=self.ps[pl][:, :],
                              scalar1=bguv[:, e, jc, 1:2], scalar2=7.0, op0=ALU.add, op1=ALU.min)
                        S_.op("dve", [rxl[q2]], [rxl[q2]], "tensor_scalar", out=xl[q2][:], in0=xl[q2][:], scalar1=-7.0,
                              scalar2=1.0, op0=ALU.max, op1=ALU.add)
                        S_.op("pool", [rxg[q2], rsg[q2]], [rxg[q2]], "tensor_tensor", out=xg[q2][:], in0=xg[q2][:],
                              in1=sg[q2][:], op=ALU.mult)
                        S_.op("dve", [rxg[q2], rxl[q2]], [ract[ab]], "tensor_tensor", out=actT[ab][:, jc, :], in0=xg[q2][:],
                              in1=xl[q2][:], op=ALU.mult)
                    if pend is not None:
                        pend()
                    if ch == 0 and more:
                        load_wd((e + 1) % NEXP, (wb + 1) % 2)

                    def down(e=e, ch=ch, ab=ab, wb=wb, ts=ts):
                        nonlocal ky
                        for t4 in range(4):
                            tt = ch * 4 + t4
                            for half in range(2):
                                pb = 4 + ky % 4
                                ky += 1
                                for jc in range(8):
                                    S_.op("pe", [ract[ab], rwd[wb]], [self.psr[pb]], "matmul", self.ps[pb][:, :],
                                          lhsT=actT[ab][:, jc, t4 * 128:(t4 + 1) * 128],
                                          rhs=wdn[wb][:, jc, half * 512:(half + 1) * 512], start=(jc == 0), stop=(jc == 7))
                                ysl = yacc[:, tt, half * 512:(half + 1) * 512]
                                S_.op("dve", [self.psr[pb], self.rG], [ry[tt]], "scalar_tensor_tensor", out=ysl,
                                      in0=self.ps[pb][:, :], scalar=self.Gall[:, ts * 8 + tt, e:e + 1], in1=ysl,
                                      op0=ALU.mult, op1=ALU.add)
                    pend = down
            pend()
            pend = None
            S_.dma("sp", ry, [], self.scr["ys"][ts * TS:(ts + 1) * TS, :].rearrange("(t p) d -> p t d", p=128), yacc[:])
        S_.barrier()


def _ln2(self, xin, xout):
    S_ = self.S_
    l = self.l
    with ExitStack() as es:
        rB = Res()
        g2b = self.bload(es, "g2b", self.scr["mod"][l:l + 1, 5 * D:6 * D], rB)
        l2g = self.bload(es, "l2g", self.I["ln2_g"][l:l + 1, :], rB)
        l2b = self.bload(es, "l2b", self.I["ln2_b"][l:l + 1, :], rB)
        xt = [self.sb(es, f"fx{i}", [128, D], F32) for i in range(2)]
        yt = [self.sb(es, f"fy{i}", [128, D], F32) for i in range(2)]
        ot = [self.sb(es, f"fo{i}", [128, D], F32) for i in range(2)]
        st = [self.sb(es, f"fs{i}", [128, 16], F32) for i in range(2)]
        rx, ry, ro, rs = [Res(), Res()], [Res(), Res()], [Res(), Res()], [Res(), Res()]
        for t in range(NT):
            b = t % 2
            rows = slice(t * 128, (t + 1) * 128)
            S_.dma("sp", [], [rx[b]], xt[b][:], xin[rows, :])
            S_.dma("sp", [], [ry[b]], yt[b][:], self.scr["ys"][rows, :])
            S_.op("pool", [ry[b], rB], [ry[b]], "tensor_tensor", out=yt[b][:], in0=yt[b][:], in1=g2b[:], op=ALU.mult)
            S_.op("dve", [rx[b], ry[b]], [ry[b]], "scalar_tensor_tensor", out=yt[b][:], in0=xt[b][:], scalar=ALPHA,
                  in1=yt[b][:], op0=ALU.mult, op1=ALU.add)
            self.ln_tile(yt[b], ry[b], None, None, st[b], rs[b])
            S_.op("act", [ry[b], rs[b]], [ry[b]], "activation", out=yt[b][:], in_=yt[b][:], func=AF.Identity,
                  bias=st[b][:, 1:2], scale=st[b][:, 0:1])
            S_.op("pool", [ry[b], rB], [ry[b]], "tensor_tensor", out=yt[b][:], in0=yt[b][:], in1=l2g[:], op=ALU.mult)
            S_.op("dve", [ry[b], rB], [ro[b]], "tensor_tensor", out=ot[b][:], in0=yt[b][:], in1=l2b[:], op=ALU.add)
            S_.dma("pool", [ro[b]], [], xout[rows, :], ot[b][:])
        S_.barrier()


Prog.phase_moe = _moe
Prog.phase_ln2 = _ln2


_CACHE = {}


def kernel(**inputs):
    consts = make_consts()
    if "nc" not in _CACHE:
        _CACHE["nc"] = Prog(consts, debug=False).build()
    nc = _CACHE["nc"]
    shared = host_shared(inputs)
    in_maps = [host_inputs(inputs, consts, b, shared) for b in range(NCORES)]
    res = run_bass_kernel_spmd(nc, in_maps, core_ids=list(range(NCORES)))
    out = np.stack([np.asarray(r["out"], dtype=np.float32) for r in res.results], axis=0)
    return out


def _route(self):
    S_ = self.S_
    C0 = 40000.0
    with ExitStack() as es:
        tri = self.sb(es, "tri", [128, 128], BF16)
        ones = self.sb(es, "ones", [128, 128], BF16)
        pidx = self.sb(es, "pidx", [128, 1], F32)
        Sb = self.sb(es, "Sb", [128, NT, NEXP], BF16)
        Rk = self.sb(es, "Rk", [128, NT, NEXP], F32)
        cnt = self.sb(es, "cnt", [128, NEXP], F32)
        nb = self.sb(es, "nb", [128, NEXP], F32)
        pad = self.sb(es, "pad", [128, NEXP], F32)
        pst = self.sb(es, "pst", [128, NEXP], F32)
        a = [self.sb(es, f"sc{i}", [128, NEXP], F32) for i in range(2)]
        m8 = self.sb(es, "rm8", [128, NT, 8], F32)
        d4f = self.sb(es, "d4f", [128, NT, 4], F32)
        tmp = [self.sb(es, f"rtmp{i}", [128, NEXP], F32) for i in range(2)]
        ebf = self.sb(es, "ebf", [128, NBLK], F32)
        rc_, rSb, rRk, rcnt, rsc, rm8, rtmp, reb = Res(), Res(), Res(), Res(), Res(), Res(), [Res(), Res()], Res()
        S_.dma("sp", [], [rc_], tri[:], self.C["tri"])
        S_.dma("sp", [], [rc_], pidx[:], self.C["pidx"])
        S_.op("pool", [], [rc_], "memset", ones[:], 1.0)
        S_.op("dve", [self.rS], [rSb], "tensor_copy", out=Sb[:], in_=self.Sall[:])
        for tt in range(NT):
            pb = tt % 2
            S_.op("pe", [rSb, rc_], [self.psr[pb]], "matmul", self.ps[pb][:, 0:NEXP], lhsT=tri[:], rhs=Sb[:, tt, :],
                  start=True, stop=(tt == 0))
            for t2 in range(tt):
                S_.op("pe", [rSb, rc_], [self.psr[pb]], "matmul", self.ps[pb][:, 0:NEXP], lhsT=ones[:], rhs=Sb[:, t2, :],
                      start=False, stop=(t2 == tt - 1))
            S_.op("act", [self.psr[pb]], [rRk], "copy", out=Rk[:, tt, :], in_=self.ps[pb][:, 0:NEXP])
        for tt in range(NT):
            S_.op("pe", [rSb, rc_], [self.psr[2]], "matmul", self.ps[2][:, 0:NEXP], lhsT=ones[:], rhs=Sb[:, tt, :],
                  start=(tt == 0), stop=(tt == NT - 1))
        S_.op("act", [self.psr[2]], [rcnt], "copy", out=cnt[:], in_=self.ps[2][:, 0:NEXP])
        S_.op("dve", [rcnt], [rsc], "tensor_scalar", out=nb[:], in0=cnt[:], scalar1=0.0, scalar2=None, op0=ALU.is_gt)
        for j in range(1, S // RB):
            S_.op("dve", [rcnt, rsc], [rsc], "scalar_tensor_tensor", out=nb[:], in0=cnt[:], scalar=float(RB * j), in1=nb[:],
                  op0=ALU.is_gt, op1=ALU.add)
        S_.op("dve", [rsc], [rsc], "tensor_scalar", out=pad[:], in0=nb[:], scalar1=float(RB), scalar2=None, op0=ALU.mult)
        S_.op("dve", [rsc], [rsc], "tensor_copy", out=a[0][:], in_=pad[:])
        cur = 0
        for sft in (1, 2, 4, 8, 16):
            nx = 1 - cur
            S_.op("dve", [rsc], [rsc], "tensor_copy", out=a[nx][:, 0:sft], in_=a[cur][:, 0:sft])
            S_.op("dve", [rsc], [rsc], "tensor_tensor", out=a[nx][:, sft:NEXP], in0=a[cur][:, sft:NEXP],
                  in1=a[cur][:, 0:NEXP - sft], op=ALU.add)
            cur = nx
        pend = a[cur]
        S_.op("dve", [rsc], [rsc], "tensor_tensor", out=pst[:], in0=pend[:], in1=pad[:], op=ALU.subtract)
        for tt in range(NT):
            S_.op("dve", [rRk, rsc], [rRk], "tensor_tensor", out=Rk[:, tt, :], in0=Rk[:, tt, :], in1=pst[:], op=ALU.add)
        Rf = Rk[:].rearrange("p t e -> p (t e)")
        Sf = self.Sall[:].rearrange("p t e -> p (t e)")
        S_.op("dve", [rRk], [rRk], "tensor_scalar", out=Rf, in0=Rf, scalar1=-1.0, scalar2=C0 + 1.0, op0=ALU.mult, op1=ALU.add)
        S_.op("dve", [rRk, self.rS], [rRk], "tensor_tensor", out=Rf, in0=Rf, in1=Sf, op=ALU.mult)
        S_.op("dve", [rRk], [rRk], "tensor_scalar", out=Rf, in0=Rf, scalar1=-1.0, scalar2=None, op0=ALU.add)
        for tt in range(NT):
            S_.op("dve", [rRk], [rm8], "max", out=m8[:, tt, :], in_=Rk[:, tt, :])
        S_.op("dve", [rm8], [rm8], "tensor_scalar", out=d4f[:], in0=m8[:, :, 0:4], scalar1=-1.0, scalar2=C0, op0=ALU.mult,
              op1=ALU.add)
        S_.op("dve", [rm8], [self.rDi], "tensor_copy", out=self.Di[:], in_=d4f[:])
        kk = 0
        for tt in range(NT):
            for k in range(4):
                i = kk % 2
                kk += 1
                S_.op("dve", [rRk, rm8, self.rG], [rtmp[i]], "scalar_tensor_tensor", out=tmp[i][:], in0=Rk[:, tt, :],
                      scalar=m8[:, tt, k:k + 1], in1=self.Gall[:, tt, :], op0=ALU.is_equal, op1=ALU.mult)
                S_.op("dve", [rtmp[i]], [self.rg4], "tensor_reduce", out=self.g4[:, tt, k:k + 1], in_=tmp[i][:], axis=AX.X,
                      op=ALU.add)
        for b in range(NBLK):
            i = kk % 2
            kk += 1
            S_.op("dve", [rsc], [rtmp[i]], "tensor_scalar", out=tmp[i][:], in0=pend[:], scalar1=float(RB * b), scalar2=None,
                  op0=ALU.is_le)
            S_.op("dve", [rtmp[i]], [reb], "tensor_reduce", out=ebf[:, b:b + 1], in_=tmp[i][:], axis=AX.X, op=ALU.add)
        S_.op("dve", [reb], [reb], "tensor_scalar", out=ebf[:], in0=ebf[:], scalar1=float(NEXP - 1), scalar2=128.0,
              op0=ALU.min, op1=ALU.mult)
        S_.op("dve", [reb, rc_], [reb], "tensor_scalar", out=ebf[:], in0=ebf[:], scalar1=pidx[:, 0:1],
              scalar2=float(self.l * NEXP * 128), op0=ALU.add, op1=ALU.add)
        S_.op("dve", [reb], [self.rIw], "tensor_copy", out=self.Iw[:], in_=ebf[:])
        ht = [self.sb(es, f"ht{i}", [128, D], BF16) for i in range(3)]
        rht = [Res() for _ in range(3)]
        for tt in range(NT):
            i = tt % 3
            S_.dma("sp", [], [rht[i]], ht[i][:], self.scr["h2"][tt * 128:(tt + 1) * 128, :])
            for k in range(4):
                S_.idma([rht[i], self.rDi], [], out=self.scr["xs2"][:, :],
                        out_offset=bass.IndirectOffsetOnAxis(ap=self.Di[:, tt, k:k + 1], axis=0), in_=ht[i][:, :],
                        in_offset=None)
        S_.barrier()


def _ffn(self):
    S_ = self.S_
    l = self.l
    wtab = self.I["w_gu_l"].rearrange("l r c -> (l r) c")
    dtab = self.I["w_dn_l"].rearrange("l r c -> (l r) c")
    btab = self.I["b_gu_l"].rearrange("l r c -> (l r) c")
    with ExitStack() as es:
        wgu = [self.sb(es, f"gwgu{i}", [128, 8 * 2 * D], BF16) for i in range(2)]
        wdn = [self.sb(es, f"gwdn{i}", [128, 8 * D], BF16) for i in range(2)]
        bgb = [self.sb(es, f"bgb{i}", [128, 16], F32) for i in range(2)]
        rw = [Res(), Res()]
        rwd = [Res(), Res()]
        xtok = [self.sb(es, f"xtok{i}", [128, 4, D], BF16) for i in range(2)]
        rxt = [Res(), Res()]
        hT = [self.sb(es, f"ghT{i}", [128, 8, RB], BF16) for i in range(2)]
        rhT = [Res(), Res()]
        actT = [self.sb(es, f"gact{i}", [128, 8, RB], BF16) for i in range(2)]
        ract = [Res(), Res()]
        xg = [self.sb(es, f"gxg{i}", [128, 512], F32) for i in range(2)]
        sg = [self.sb(es, f"gsg{i}", [128, 512], F32) for i in range(2)]
        xl = [self.sb(es, f"gxl{i}", [128, 512], F32) for i in range(2)]
        rxg, rsg, rxl = [Res(), Res()], [Res(), Res()], [Res(), Res()]
        yb = [self.sb(es, f"gyb{i}", [128, D], F32) for i in range(4)]
        ryb = [Res() for _ in range(4)]

        def load_w(b):
            wb = b % 2
            ix = bass.IndirectOffsetOnAxis(ap=self.Iw[:, b:b + 1], axis=0)
            S_.idma([self.rIw], [rw[wb]], out=wgu[wb][:, :], out_offset=None, in_=wtab[:, :], in_offset=ix)
            S_.idma([self.rIw], [rw[wb]], out=bgb[wb][:, :], out_offset=None, in_=btab[:, :], in_offset=ix)

        def load_wd(b):
            wb = b % 2
            ix = bass.IndirectOffsetOnAxis(ap=self.Iw[:, b:b + 1], axis=0)
            S_.idma([self.rIw], [rwd[wb]], out=wdn[wb][:, :], out_offset=None, in_=dtab[:, :], in_offset=ix)

        def load_x(b):
            S_.dma("sp", [], [rxt[b % 2]], xtok[b % 2][:],
                   self.scr["xs2"][b * RB:(b + 1) * RB, :].rearrange("(t p) d -> p t d", p=128))
        kq = 0
        ky = 0
        pend = None
        fin = None
        load_w(0)
        load_wd(0)
        load_x(0)
        for b in range(NBLK):
            wb = b % 2
            if b + 1 < NBLK:
                load_w(b + 1)
                load_x(b + 1)
            for t4 in range(4):
                pT = 6 + (t4 % 2)
                pst = self.ps[pT][:].bitcast(BF16)
                for c in range(8):
                    S_.op("pe", [rxt[wb], self.rc], [self.psr[pT]], "transpose", out=pst[:, c * 128:(c + 1) * 128],
                          in_=xtok[wb][:, t4, c * 128:(c + 1) * 128], identity=self.identb[:])
                S_.op("act", [self.psr[pT]], [rhT[wb]], "copy", out=hT[wb][:, :, t4 * 128:(t4 + 1) * 128],
                      in_=pst.rearrange("p (c n) -> p c n", c=8))
            wv = wgu[wb][:].rearrange("p (c j two) -> p c two j", c=8, two=2)
            for jc in range(8):
                q2 = kq % 2
                pg, pl = (0, 1) if (kq % 2 == 0) else (2, 3)
                kq += 1
                for two, pp in ((0, pg), (1, pl)):
                    for c in range(8):
                        S_.op("pe", [rw[wb], rhT[wb]], [self.psr[pp]], "matmul", self.ps[pp][:, :],
                              lhsT=wv[:, c, two, jc * 128:(jc + 1) * 128], rhs=hT[wb][:, c, :], start=(c == 0), stop=(c == 7))
                S_.op("dve", [self.psr[pg], rw[wb]], [rxg[q2]], "tensor_scalar", out=xg[q2][:], in0=self.ps[pg][:, :],
                      scalar1=bgb[wb][:, 2 * jc:2 * jc + 1], scalar2=7.0, op0=ALU.add, op1=ALU.min)
                S_.op("act", [rxg[q2]], [rsg[q2]], "activation", out=sg[q2][:], in_=xg[q2][:], func=AF.Sigmoid, scale=1.702)
                S_.op("dve", [self.psr[pl], rw[wb]], [rxl[q2]], "tensor_scalar", out=xl[q2][:], in0=self.ps[pl][:, :],
                      scalar1=bgb[wb][:, 2 * jc + 1:2 * jc + 2], scalar2=7.0, op0=ALU.add, op1=ALU.min)
                S_.op("dve", [rxl[q2]], [rxl[q2]], "tensor_scalar", out=xl[q2][:], in0=xl[q2][:], scalar1=-7.0, scalar2=1.0,
                      op0=ALU.max, op1=ALU.add)
                if fin is not None:
                    fin()

                def fin(q2=q2, jc=jc, wb=wb):
                    S_.op("dve", [rxg[q2], rsg[q2]], [rxg[q2]], "tensor_tensor", out=xg[q2][:], in0=xg[q2][:], in1=sg[q2][:],
                          op=ALU.mult)
                    S_.op("dve", [rxg[q2], rxl[q2]], [ract[wb]], "tensor_tensor", out=actT[wb][:, jc, :], in0=xg[q2][:],
                          in1=xl[q2][:], op=ALU.mult)
            fin()
            fin = None
            if pend is not None:
                pend()
            if b + 1 < NBLK:
                load_wd(b + 1)

            def down(b=b, wb=wb):
                nonlocal ky
                wd = wdn[wb][:].rearrange("p (c n) -> p c n", c=8)
                for t4 in range(4):
                    yi = ky % 4
                    ky += 1
                    for half in range(2):
                        pb = 4 + half
                        for jc in range(8):
                            S_.op("pe", [ract[wb], rwd[wb]], [self.psr[pb]], "matmul", self.ps[pb][:, :],
                                  lhsT=actT[wb][:, jc, t4 * 128:(t4 + 1) * 128], rhs=wd[:, jc, half * 512:(half + 1) * 512],
                                  start=(jc == 0), stop=(jc == 7))
                        S_.op("act", [self.psr[pb]], [ryb[yi]], "copy", out=yb[yi][:, half * 512:(half + 1) * 512],
                              in_=self.ps[pb][:, :])
                    r0 = b * RB + t4 * 128
                    S_.dma("sp", [ryb[yi]], [], self.scr["ys2"][r0:r0 + 128, :], yb[yi][:])
            pend = down
        pend()
        S_.barrier()


def _ln2g(self, xin, xout):
    S_ = self.S_
    l = self.l
    with ExitStack() as es:
        rB = Res()
        g2b = self.bload(es, "g2b", self.scr["mod"][l:l + 1, 5 * D:6 * D], rB)
        l2g = self.bload(es, "l2g", self.I["ln2_g"][l:l + 1, :], rB)
        l2b = self.bload(es, "l2b", self.I["ln2_b"][l:l + 1, :], rB)
        bdn = self.sb(es, "bdn", [NEXP, D], F32)
        S_.dma("sp", [], [rB], bdn[:], self.I["b_down"][l])
        GT = [self.sb(es, f"cGT{i}", [NEXP, 128], F32) for i in range(2)]
        xt = [self.sb(es, f"fx{i}", [128, D], F32) for i in range(2)]
        yt = [self.sb(es, f"fy{i}", [128, D], F32) for i in range(2)]
        ot = [self.sb(es, f"fo{i}", [128, D], F32) for i in range(2)]
        yg = [[self.sb(es, f"yg{i}{k}", [128, D], F32) for k in range(4)] for i in range(2)]
        st = [self.sb(es, f"fs{i}", [128, 16], F32) for i in range(2)]
        rx, ry, ro, rs, rGT = [Res(), Res()], [Res(), Res()], [Res(), Res()], [Res(), Res()], [Res(), Res()]
        ryg = [[Res() for _ in range(4)] for _ in range(2)]
        def tile_body(t):
            b = t % 2
            rows = slice(t * 128, (t + 1) * 128)
            S_.dma("sp", [], [rx[b]], xt[b][:], xin[rows, :])
            for k in range(4):
                S_.idma([self.rDi], [ryg[b][k]], out=yg[b][k][:, :], out_offset=None, in_=self.scr["ys2"][:, :],
                        in_offset=bass.IndirectOffsetOnAxis(ap=self.Di[:, t, k:k + 1], axis=0))
            pT = 6 + b
            S_.op("pe", [self.rG, self.rc], [self.psr[pT]], "transpose", out=self.ps[pT][0:NEXP, 0:128],
                  in_=self.Gall[:, t, :], identity=self.identf[:])
            S_.op("act", [self.psr[pT]], [rGT[b]], "copy", out=GT[b][:], in_=self.ps[pT][0:NEXP, 0:128])
            for half in range(2):
                pb = 2 * b + half
                hs_ = slice(half * 512, (half + 1) * 512)
                S_.op("pe", [rGT[b], rB], [self.psr[pb]], "matmul", self.ps[pb][:, :], lhsT=GT[b][:], rhs=bdn[:, hs_],
                      start=True, stop=True)
                S_.op("dve", [self.psr[pb], ryg[b][0], self.rg4], [ry[b]], "scalar_tensor_tensor", out=yt[b][:, hs_],
                      in0=yg[b][0][:, hs_], scalar=self.g4[:, t, 0:1], in1=self.ps[pb][:, :], op0=ALU.mult, op1=ALU.add)
            yield
            for k in range(1, 4):
                S_.op("dve", [ry[b], ryg[b][k], self.rg4], [ry[b]], "scalar_tensor_tensor", out=yt[b][:], in0=yg[b][k][:],
                      scalar=self.g4[:, t, k:k + 1], in1=yt[b][:], op0=ALU.mult, op1=ALU.add)
            S_.op("pool", [ry[b], rB], [ry[b]], "tensor_tensor", out=yt[b][:], in0=yt[b][:], in1=g2b[:], op=ALU.mult)
            yield
            S_.op("dve", [rx[b], ry[b]], [ry[b]], "scalar_tensor_tensor", out=yt[b][:], in0=xt[b][:], scalar=ALPHA,
                  in1=yt[b][:], op0=ALU.mult, op1=ALU.add)
            self.ln_tile(yt[b], ry[b], None, None, st[b], rs[b], split=True)
            yield
            self.ln_tile2(st[b], rs[b])
            S_.op("act", [ry[b], rs[b]], [ry[b]], "activation", out=yt[b][:], in_=yt[b][:], func=AF.Identity,
                  bias=st[b][:, 1:2], scale=st[b][:, 0:1])
            S_.op("pool", [ry[b], rB], [ry[b]], "tensor_tensor", out=yt[b][:], in0=yt[b][:], in1=l2g[:], op=ALU.mult)
            yield
            S_.op("dve", [ry[b], rB], [ro[b]], "tensor_tensor", out=ot[b][:], in0=yt[b][:], in1=l2b[:], op=ALU.add)
            S_.dma("sp", [ro[b]], [], xout[rows, :], ot[b][:])
        round_robin((tile_body(t) for t in range(NT)), 2)
        S_.barrier()


Prog.phase_route = _route
Prog.phase_ffn = _ffn
Prog.phase_ln2 = _ln2g
```
